# Optimizing a Trainium2 kernel written in Bass

```python
import jax, jax.numpy as jnp
from jax import lax
import numpy as np

D_MODEL = 2048
BATCH = 2
SEQ = 16384
DEPTH = 2

N_EVEN = (DEPTH + 1) // 2
N_ODD = DEPTH // 2
PLE_DIM = 256
CHUNK = 64
EPS = 1e-6
A_HEADS = 8
A_DK = 128
A_DV = 128
B_HEADS = 8
B_DK = 128
B_DV = 128
CONV_WIDTH = 4
A_QK_W = A_HEADS * A_DK
A_V_W = A_HEADS * A_DV
B_QK_W = B_HEADS * B_DK
B_V_W = B_HEADS * B_DV
EVEN_MIX_W = A_V_W + B_V_W
EVEN_PROJ_W = 2 * A_QK_W + 2 * A_V_W + 2 * B_QK_W + 2 * B_V_W + 2 * B_HEADS
C_WIDTH = D_MODEL
C_BLOCKS = 8
C_BLOCK_W = C_WIDTH // C_BLOCKS
RGLRU_C = 8.0
FFN_DIM = ((8 * D_MODEL // 3 + 255) // 256) * 256
N_EXPERTS = 8
TOP_K = 2
EXPERT_DIM = FFN_DIM
MOE_BLOCK = 512

kernel_name = "hgrn2_gdn_rglru_moe_hybrid"


def rmsnorm(x, g):
    xf = x.astype(jnp.float32)
    y = xf * lax.rsqrt(jnp.mean(xf * xf, axis=-1, keepdims=True) + EPS)
    return (y * g.astype(jnp.float32)).astype(x.dtype)


def l2norm(x):
    return x * lax.rsqrt(jnp.sum(x * x, axis=-1, keepdims=True) + EPS)


def causal_dwconv(x, w):
    k, c = w.shape
    return lax.conv_general_dilated(x, w[:, None, :].astype(x.dtype), (1,), [(k - 1, 0)],
                                    dimension_numbers=("NWC", "WIO", "NWC"), feature_group_count=c)


def to_chunks(t):
    b, s = t.shape[:2]
    return jnp.moveaxis(t.reshape((b, s // CHUNK, CHUNK) + t.shape[2:]), 3, 1)


def from_chunks(t):
    t = jnp.moveaxis(t, 1, 3)
    return t.reshape((t.shape[0], t.shape[1] * t.shape[2]) + t.shape[3:])


def hgrn2_chunked(q, k, v, logf):
    qc, kc, vc, gc = (to_chunks(t) for t in (q, k, v, logf))
    bcum = jnp.cumsum(gc, axis=3)
    causal = jnp.tril(jnp.ones((CHUNK, CHUNK), bool))

    def step(state, inp):
        qi, ki, vi, bi = inp
        rel = jnp.where(causal[:, :, None], bi[:, :, :, None, :] - bi[:, :, None, :, :], -jnp.inf)
        scores = jnp.einsum("bhtd,bhsd,bhtsd->bhts", qi, ki, jnp.exp(rel))
        blast = bi[:, :, -1:, :]
        o = (jnp.einsum("bhts,bhsv->bhtv", scores, vi)
             + jnp.einsum("bhtd,bhdv->bhtv", qi * jnp.exp(bi), state))
        state = (jnp.exp(blast[:, :, 0, :])[..., None] * state
                 + jnp.einsum("bhsd,bhsv->bhdv", ki * jnp.exp(blast - bi), vi))
        return state, o

    state0 = jnp.zeros(qc.shape[:2] + (qc.shape[-1], vc.shape[-1]), jnp.float32)
    xs = tuple(jnp.moveaxis(t, 2, 0) for t in (qc, kc, vc, bcum))
    _, o = lax.scan(step, state0, xs)
    return from_chunks(jnp.moveaxis(o, 0, 2))


def gated_delta_chunked(q, k, v, g, beta):
    qc, kc, vc = (to_chunks(t) for t in (q, k, v))
    gc, bc = to_chunks(g), to_chunks(beta)
    gam = jnp.cumsum(gc, axis=-1)
    causal = jnp.tril(jnp.ones((CHUNK, CHUNK), bool))
    strict = jnp.tril(jnp.ones((CHUNK, CHUNK), bool), -1)
    decay = jnp.exp(jnp.where(causal, gam[..., :, None] - gam[..., None, :], -jnp.inf))
    kbeta = kc * bc[..., None]
    a_mat = jnp.where(strict, jnp.einsum("bhntd,bhnsd->bhnts", kbeta, kc) * decay, 0.0)
    lower = a_mat + jnp.eye(CHUNK, dtype=jnp.float32)
    u = lax.linalg.triangular_solve(lower, vc * bc[..., None], left_side=True, lower=True,
                                    unit_diagonal=True)
    w = lax.linalg.triangular_solve(lower, kbeta * jnp.exp(gam)[..., None], left_side=True,
                                    lower=True, unit_diagonal=True)
    qk = jnp.einsum("bhntd,bhnsd->bhnts", qc, kc) * decay

    def step(state, inp):
        qi, ki, ui, wi, qki, gi = inp
        v_new = ui - jnp.einsum("bhtd,bhdv->bhtv", wi, state)
        o = (jnp.einsum("bhtd,bhdv->bhtv", qi * jnp.exp(gi)[..., None], state)
             + jnp.einsum("bhts,bhsv->bhtv", qki, v_new))
        glast = gi[..., -1:]
        state = (jnp.exp(glast)[..., None] * state
                 + jnp.einsum("bhsd,bhsv->bhdv", ki * jnp.exp(glast - gi)[..., None], v_new))
        return state, o

    state0 = jnp.zeros(qc.shape[:2] + (qc.shape[-1], vc.shape[-1]), jnp.float32)
    xs = tuple(jnp.moveaxis(t, 2, 0) for t in (qc, kc, u, w, qk, gam))
    _, o = lax.scan(step, state0, xs)
    return from_chunks(jnp.moveaxis(o, 0, 2))


def even_mixer(u, w_in, lb, conv_w, a_log, dt_bias, gn_a, gn_b, w_out):
    bsz, s, _ = u.shape
    f32 = jnp.float32
    proj = (u @ w_in).astype(f32)
    cuts = [int(c) for c in np.cumsum([A_QK_W, A_QK_W, A_V_W, A_V_W, 2 * B_QK_W + B_V_W, B_V_W, B_HEADS])]
    qa, fa, ia, ga, qkv_b, zb, ab, bb = jnp.split(proj, cuts, axis=-1)

    def heads(t, h):
        return t.reshape(bsz, s, h, -1)

    lbf = lb.astype(f32)
    forget = lbf + (1.0 - lbf) * jax.nn.sigmoid(fa)
    o_a = hgrn2_chunked(heads(qa, A_HEADS) * A_DK ** -0.5, heads(1.0 - forget, A_HEADS),
                        heads(ia, A_HEADS), heads(jnp.log(forget), A_HEADS))
    o_a = rmsnorm(o_a, gn_a) * jax.nn.silu(heads(ga, A_HEADS))

    qkv_b = jax.nn.silu(causal_dwconv(qkv_b, conv_w.astype(f32)))
    qb, kb, vb = jnp.split(qkv_b, [B_QK_W, 2 * B_QK_W], axis=-1)
    qb = l2norm(heads(qb, B_HEADS)) * B_DK ** -0.5
    kb = l2norm(heads(kb, B_HEADS))
    beta = jax.nn.sigmoid(bb)
    g = -jnp.exp(a_log.astype(f32)) * jax.nn.softplus(ab + dt_bias.astype(f32))
    o_b = gated_delta_chunked(qb, kb, heads(vb, B_HEADS), g, beta)
    o_b = rmsnorm(o_b, gn_b) * jax.nn.silu(heads(zb, B_HEADS))

    mixed = jnp.concatenate([o_a.reshape(bsz, s, A_V_W), o_b.reshape(bsz, s, B_V_W)], axis=-1)
    return mixed.astype(u.dtype) @ w_out


def _lin_rec_combine(left, right):
    a_l, b_l = left
    a_r, b_r = right
    return a_l * a_r, a_r * b_l + b_r


def odd_mixer(u, w_in, conv_w, conv_b, w_r, b_r, w_i, b_i, lam, w_out):
    bsz, s, _ = u.shape
    f32 = jnp.float32
    y_branch, xr = jnp.split((u @ w_in).astype(f32), [C_WIDTH], axis=-1)
    y_branch = jax.nn.gelu(y_branch)
    xr = causal_dwconv(xr, conv_w.astype(f32)) + conv_b.astype(f32)
    xb = xr.reshape(bsz, s, C_BLOCKS, C_BLOCK_W)
    r = jax.nn.sigmoid(jnp.einsum("bsnc,ncd->bsnd", xb, w_r.astype(f32)).reshape(bsz, s, C_WIDTH)
                       + b_r.astype(f32))
    gi = jax.nn.sigmoid(jnp.einsum("bsnc,ncd->bsnd", xb, w_i.astype(f32)).reshape(bsz, s, C_WIDTH)
                        + b_i.astype(f32))
    log_a = -RGLRU_C * r * jax.nn.softplus(-lam.astype(f32))
    a = jnp.exp(log_a)
    mult = jnp.sqrt(jnp.maximum(-jnp.expm1(2.0 * log_a), 0.0))
    mult = jnp.where((jnp.arange(s) == 0)[None, :, None], 1.0, mult)
    _, h = lax.associative_scan(_lin_rec_combine, (a, mult * gi * xr), axis=1)
    return (h * y_branch).astype(u.dtype) @ w_out


def swiglu(u, wg, wu, wd):
    return (jax.nn.silu(u @ wg) * (u @ wu)) @ wd


def moe_swiglu(u, w_router, w_gate, w_up, w_down):
    bsz, s, d = u.shape
    t = bsz * s
    xs = u.reshape(t, d)
    logits = (xs @ w_router).astype(jnp.float32)
    top_logits, top_idx = lax.top_k(logits, TOP_K)
    gates = jax.nn.softmax(top_logits, axis=-1)
    e_flat = top_idx.reshape(-1)
    tok_flat = jnp.repeat(jnp.arange(t, dtype=jnp.int32), TOP_K)
    g_flat = gates.reshape(-1)
    order = jnp.argsort(e_flat)
    e_sorted, tok_sorted, g_sorted = e_flat[order], tok_flat[order], g_flat[order]
    counts = jnp.bincount(e_flat, length=N_EXPERTS)
    padded = ((counts + MOE_BLOCK - 1) // MOE_BLOCK) * MOE_BLOCK
    start = jnp.cumsum(counts) - counts
    pend = jnp.cumsum(padded)
    pstart = pend - padded
    dest = pstart[e_sorted] + jnp.arange(t * TOP_K) - start[e_sorted]
    n_blocks = -(-(t * TOP_K) // MOE_BLOCK) + N_EXPERTS
    rows = n_blocks * MOE_BLOCK
    row_tok = jnp.full((rows,), t, jnp.int32).at[dest].set(tok_sorted)
    row_gate = jnp.zeros((rows,), jnp.float32).at[dest].set(g_sorted)
    block_expert = jnp.minimum(
        jnp.searchsorted(pend, jnp.arange(n_blocks) * MOE_BLOCK, side="right"), N_EXPERTS - 1)
    xs_pad = jnp.concatenate([xs, jnp.zeros((1, d), xs.dtype)], axis=0)

    def expert_block(args):
        toks, gts, e = args
        xb = xs_pad[toks]
        hb = jax.nn.silu(xb @ w_gate[e]) * (xb @ w_up[e])
        return (hb @ w_down[e]) * gts[:, None].astype(xb.dtype)

    yb = lax.map(expert_block, (row_tok.reshape(n_blocks, MOE_BLOCK),
                                row_gate.reshape(n_blocks, MOE_BLOCK), block_expert))
    y = jnp.zeros((t + 1, d), yb.dtype).at[row_tok].add(yb.reshape(rows, d))[:t]
    return y.reshape(bsz, s, d)


def setup_inputs(seed: int = 0) -> dict:
    key = jax.random.key(seed)
    ks = iter(jax.random.split(key, 48))
    f32 = jnp.float32

    def nrm(shape, scale):
        return jax.random.normal(next(ks), shape, f32) * scale

    def gain(shape):
        return 1.0 + 0.1 * jax.random.normal(next(ks), shape, f32)

    def unif(shape, lo, hi):
        return jax.random.uniform(next(ks), shape, f32, lo, hi)

    x = nrm((BATCH, SEQ, D_MODEL), 1.0)
    p = nrm((DEPTH, BATCH, SEQ, PLE_DIM), 1.0)
    ln_mix = gain((DEPTH, D_MODEL))
    ln_ffn = gain((DEPTH, D_MODEL))
    ln_ple = gain((DEPTH, D_MODEL))
    ln_final = gain((D_MODEL,))
    lb_table = gain((DEPTH + 1, A_QK_W))
    ab_w_in = nrm((N_EVEN, D_MODEL, EVEN_PROJ_W), D_MODEL ** -0.5)
    ab_conv = nrm((N_EVEN, CONV_WIDTH, 2 * B_QK_W + B_V_W), CONV_WIDTH ** -0.5)
    b_a_log = jnp.log(unif((N_EVEN, B_HEADS), 1.0, 16.0))
    dt = jnp.exp(unif((N_EVEN, B_HEADS), float(np.log(1e-3)), float(np.log(1e-1))))
    b_dt_bias = dt + jnp.log(-jnp.expm1(-dt))
    a_gnorm = gain((N_EVEN, A_DV))
    b_gnorm = gain((N_EVEN, B_DV))
    ab_w_out = nrm((N_EVEN, EVEN_MIX_W, D_MODEL), EVEN_MIX_W ** -0.5)
    c_w_in = nrm((N_ODD, D_MODEL, 2 * C_WIDTH), D_MODEL ** -0.5)
    c_conv_w = nrm((N_ODD, CONV_WIDTH, C_WIDTH), CONV_WIDTH ** -0.5)
    c_conv_b = nrm((N_ODD, C_WIDTH), 0.01)
    c_w_r = nrm((N_ODD, C_BLOCKS, C_BLOCK_W, C_BLOCK_W), C_BLOCK_W ** -0.5)
    c_b_r = nrm((N_ODD, C_WIDTH), 0.1)
    c_w_i = nrm((N_ODD, C_BLOCKS, C_BLOCK_W, C_BLOCK_W), C_BLOCK_W ** -0.5)
    c_b_i = nrm((N_ODD, C_WIDTH), 0.1)
    a0 = unif((N_ODD, C_WIDTH), 0.9, 0.999)
    sg = a0 ** (1.0 / RGLRU_C)
    c_lambda = jnp.log(sg) - jnp.log1p(-sg)
    c_w_out = nrm((N_ODD, C_WIDTH, D_MODEL), C_WIDTH ** -0.5)
    ffn_w_gate = nrm((N_EVEN, D_MODEL, FFN_DIM), D_MODEL ** -0.5)
    ffn_w_up = nrm((N_EVEN, D_MODEL, FFN_DIM), D_MODEL ** -0.5)
    ffn_w_down = nrm((N_EVEN, FFN_DIM, D_MODEL), FFN_DIM ** -0.5)
    moe_router = nrm((N_ODD, D_MODEL, N_EXPERTS), D_MODEL ** -0.5)
    moe_w_gate = nrm((N_ODD, N_EXPERTS, D_MODEL, EXPERT_DIM), D_MODEL ** -0.5)
    moe_w_up = nrm((N_ODD, N_EXPERTS, D_MODEL, EXPERT_DIM), D_MODEL ** -0.5)
    moe_w_down = nrm((N_ODD, N_EXPERTS, EXPERT_DIM, D_MODEL), EXPERT_DIM ** -0.5)
    ple_w_proj = nrm((DEPTH, PLE_DIM, D_MODEL), PLE_DIM ** -0.5)
    ple_w_gate = nrm((DEPTH, D_MODEL, D_MODEL), D_MODEL ** -0.5)
    return {"x": x, "p": p, "ln_mix": ln_mix, "ln_ffn": ln_ffn, "ln_ple": ln_ple,
            "ln_final": ln_final, "lb_table": lb_table, "ab_w_in": ab_w_in, "ab_conv": ab_conv,
            "b_a_log": b_a_log, "b_dt_bias": b_dt_bias, "a_gnorm": a_gnorm, "b_gnorm": b_gnorm,
            "ab_w_out": ab_w_out, "c_w_in": c_w_in, "c_conv_w": c_conv_w, "c_conv_b": c_conv_b,
            "c_w_r": c_w_r, "c_b_r": c_b_r, "c_w_i": c_w_i, "c_b_i": c_b_i, "c_lambda": c_lambda,
            "c_w_out": c_w_out, "ffn_w_gate": ffn_w_gate, "ffn_w_up": ffn_w_up,
            "ffn_w_down": ffn_w_down, "moe_router": moe_router, "moe_w_gate": moe_w_gate,
            "moe_w_up": moe_w_up, "moe_w_down": moe_w_down, "ple_w_proj": ple_w_proj,
            "ple_w_gate": ple_w_gate}


def reference(x, p, ln_mix, ln_ffn, ln_ple, ln_final, lb_table, ab_w_in, ab_conv, b_a_log,
              b_dt_bias, a_gnorm, b_gnorm, ab_w_out, c_w_in, c_conv_w, c_conv_b, c_w_r, c_b_r,
              c_w_i, c_b_i, c_lambda, c_w_out, ffn_w_gate, ffn_w_up, ffn_w_down, moe_router,
              moe_w_gate, moe_w_up, moe_w_down, ple_w_proj, ple_w_gate):
    lb_all = jnp.cumsum(jax.nn.softmax(lb_table.astype(jnp.float32), axis=0), axis=0)
    h = x
    for layer in range(DEPTH):
        j = layer // 2
        u = rmsnorm(h, ln_mix[layer])
        if layer % 2 == 0:
            h = h + even_mixer(u, ab_w_in[j], lb_all[layer], ab_conv[j], b_a_log[j], b_dt_bias[j],
                               a_gnorm[j], b_gnorm[j], ab_w_out[j])
            u = rmsnorm(h, ln_ffn[layer])
            h = h + swiglu(u, ffn_w_gate[j], ffn_w_up[j], ffn_w_down[j])
        else:
            h = h + odd_mixer(u, c_w_in[j], c_conv_w[j], c_conv_b[j], c_w_r[j], c_b_r[j], c_w_i[j],
                              c_b_i[j], c_lambda[j], c_w_out[j])
            u = rmsnorm(h, ln_ffn[layer])
            h = h + moe_swiglu(u, moe_router[j], moe_w_gate[j], moe_w_up[j], moe_w_down[j])
        gate = jax.nn.sigmoid(rmsnorm(h, ln_ple[layer]) @ ple_w_gate[layer])
        h = h + gate * (p[layer] @ ple_w_proj[layer])
    return rmsnorm(h, ln_final)
```

```python
import contextlib
import numpy as np
import concourse.bass as bass
import concourse.mybir as mybir
from concourse.bass_utils import run_bass_kernel_spmd

F32 = mybir.dt.float32
BF16 = mybir.dt.bfloat16
ALU = mybir.AluOpType
AF = mybir.ActivationFunctionType
AX = mybir.AxisListType


class Sched:
    ENG = ("pe", "act", "dve", "pool", "sp")

    def __init__(self, nc, selfsync=True):
        self.nc = nc
        self.stack = contextlib.ExitStack()
        self.ops = {e: [] for e in self.ENG}
        self.sems = {}
        self.cnt = {}
        self.waited = {e: {} for e in self.ENG}
        self.state = {}
        self.selfsync = selfsync
        self.nwaits = 0
        for e in ("pe", "act", "dve", "pool"):
            self._sem("e_" + e)

    def _sem(self, name):
        if name not in self.sems:
            self.sems[name] = self.stack.enter_context(self.nc.semaphore(name))
            self.cnt[name] = 0
        return self.sems[name]

    def sbuf(self, name, shape, dtype):
        return self.stack.enter_context(self.nc.sbuf_tensor(name, list(shape), dtype))

    def psum(self, name, shape, dtype=F32):
        return self.stack.enter_context(self.nc.psum_tensor(name, list(shape), dtype))

    def _st(self, t, r):
        d = self.state.setdefault(t, {})
        if r not in d:
            d[r] = [None, {}]
        return d[r]

    def _overl(self, t, r):
        d = self.state.get(t, {})
        if r is None:
            return list(d.values())
        return [d[k] for k in (r, None) if k in d]

    def _deps(self, reads, writes):
        ev = {}

        def add(e):
            if e is not None and ev.get(e[0], 0) < e[1]:
                ev[e[0]] = e[1]
        for (t, r) in reads:
            for st in self._overl(t, r):
                add(st[0])
        for (t, r) in writes:
            for st in self._overl(t, r):
                add(st[0])
                for s, v in st[1].items():
                    add((s, v))
        return ev

    def _commit(self, reads, writes, e):
        for (t, r) in reads:
            st = self._st(t, r)
            if st[1].get(e[0], 0) < e[1]:
                st[1][e[0]] = e[1]
        for (t, r) in writes:
            if r is None:
                self.state[t] = {None: [e, {}]}
            else:
                st = self._st(t, r)
                st[0] = e
                st[1] = {}

    def _waits(self, eng, ev):
        w = []
        for s, v in ev.items():
            if s == "e_" + eng and (eng == "pe" or not self.selfsync):
                continue
            if self.waited[eng].get(s, 0) < v:
                self.waited[eng][s] = v
                w.append((s, v))
        self.nwaits += len(w)
        return w

    @staticmethod
    def _norm(keys):
        out = []
        for k in keys:
            if isinstance(k, tuple):
                if k[0].startswith("PS"):
                    out.append((k[0], None))
                else:
                    out.append((k[0], k[1]))
            else:
                out.append((k, None))
        return out

    def op(self, eng, fn, reads=(), writes=()):
        reads, writes = self._norm(reads), self._norm(writes)
        w = self._waits(eng, self._deps(reads, writes))
        s = "e_" + eng
        self.cnt[s] += 1
        e = (s, self.cnt[s])
        self.ops[eng].append((w, fn, [(s, 1)]))
        self._commit(reads, writes, e)
        return e

    def dma(self, q, pairs, chan, reads=(), writes=()):
        reads, writes = self._norm(reads), self._norm(writes)
        w = self._waits(q, self._deps(reads, writes))
        s = "d_" + chan
        self._sem(s)
        for i, (o, a) in enumerate(pairs):
            self.cnt[s] += 16
            self.ops[q].append((w if i == 0 else [], ("dma", o, a), [(s, 16)]))
        e = (s, self.cnt[s])
        self._commit(reads, writes, e)
        return e

    def finish(self, eng, events):
        ev = {}
        for (s, v) in events:
            ev[s] = max(ev.get(s, 0), v)
        self.ops[eng].append((list(ev.items()), None, []))

    def emit(self):
        nc = self.nc
        sems = self.sems

        def run(eng, lst):
            for (w, fn, incs) in lst:
                for (s, v) in w:
                    eng.wait_ge(sems[s], v)
                if fn is None:
                    continue
                if isinstance(fn, tuple):
                    ins = eng.dma_start(out=fn[1], in_=fn[2])
                else:
                    ins = fn(eng)
                for (s, n) in incs:
                    ins.then_inc(sems[s], n)
        with nc.Block() as block:
            @block.tensor
            def _(e):
                run(e, self.ops["pe"])

            @block.scalar
            def _(e):
                run(e, self.ops["act"])

            @block.vector
            def _(e):
                run(e, self.ops["dve"])

            @block.gpsimd
            def _(e):
                run(e, self.ops["pool"])

            @block.sync
            def _(e):
                run(e, self.ops["sp"])
        self.stack.close()


D = 2048
KC = 16
TT = 512
FF = 5632
FC = FF // 128
PLE = 256
EPS = 1e-6


class DenseCore:
    def __init__(self, s):
        self.s = s
        s_ = s
        self.H = s_.sbuf("H", [128, KC, TT], F32)
        self.MT = s_.sbuf("MT", [128, KC, TT], BF16)
        self.UT = s_.sbuf("UT", [128, KC, TT], BF16)
        self.HT = s_.sbuf("HT", [128, FC, TT], BF16)
        self.PT = s_.sbuf("PT", [128, 2, TT], BF16)
        self.WB = [s_.sbuf(f"WB{i}", [128, KC, 256], BF16) for i in range(4)]
        self.WD = [s_.sbuf(f"WD{i}", [128, 11, 512], BF16) for i in range(2)]
        self.WP = [s_.sbuf(f"WP{i}", [128, 2, 256], BF16) for i in range(2)]
        self.RSTD = s_.sbuf("RSTD", [128, TT], F32)
        self.SG = [s_.sbuf(f"SG{i}", [128, TT], F32) for i in range(2)]
        self.TMP = [s_.sbuf(f"TMP{i}", [128, TT], F32) for i in range(2)]
        self.ONES = s_.sbuf("ONES", [128, 128], BF16)
        self.PB = s_.psum("PB", [128, 8, TT])
        s_.op("pool", lambda e: e.memset(self.ONES[:], 1.0), writes=["ONES"])
        self.wb_i = 0
        self.wd_i = 0
        self.wp_i = 0
        self.pb_i = 0
        self.sg_i = 0

    def bank(self):
        b = self.pb_i % 8
        self.pb_i += 1
        return b

    def load_vec(self, name, ap):
        t = self.s.sbuf(name, list(ap.shape), F32)
        self.s.dma("sp", [(t[:], ap)], "c_" + name, writes=[name])
        return t

    def load_tile(self, hT_ap, tok):
        self.s.dma("sp", [(self.H[:], hT_ap[:, tok].rearrange("(kc p) t -> p kc t", p=128))], "h", writes=["H"])

    def load_bf(self, dst, name, src_ap, tok, chan):
        self.s.dma("pool", [(dst[:], src_ap[:, tok].rearrange("(kc p) t -> p kc t", p=128))], chan, writes=[name])

    def store_tile(self, out_ap, tok):
        return self.s.dma("sp", [(out_ap[:, tok].rearrange("(kc p) t -> p kc t", p=128), self.H[:])], "st", reads=["H"])

    def rmsnorm(self, Gname, G):
        s = self.s
        H, SQ, UT, RSTD, ONES, PB = self.H, self.MT, self.UT, self.RSTD, self.ONES, self.PB
        s.op("act", lambda e: e.activation(out=SQ[:], in_=H[:], func=AF.Square), reads=["H"], writes=["MT"])
        b = self.bank()
        for kc in range(KC):
            s.op("pe", lambda e, kc=kc: e.matmul(PB[:, b, :], lhsT=ONES[:], rhs=SQ[:, kc, :], start=(kc == 0), stop=(kc == KC - 1)),
                 reads=["ONES", "MT"], writes=[("PB", b)])
        s.op("act", lambda e: e.activation(out=RSTD[:], in_=PB[:, b, :], func=AF.Sqrt, bias=EPS, scale=1.0 / D),
             reads=[("PB", b)], writes=["RSTD"])
        s.op("dve", lambda e: e.reciprocal(out=RSTD[:], in_=RSTD[:]), reads=["RSTD"], writes=["RSTD"])
        for kc in range(KC):
            s.op("dve", lambda e, kc=kc: e.scalar_tensor_tensor(out=UT[:, kc, :], in0=H[:, kc, :], scalar=G[:, kc:kc + 1],
                                                              in1=RSTD[:], op0=ALU.mult, op1=ALU.mult),
                 reads=[("H", kc), Gname, "RSTD"], writes=[("UT", kc)])

    def load_wb(self, w_ap, cb, chan):
        i = self.wb_i % 4
        self.wb_i += 1
        self.s.dma("pool", [(self.WB[i][:], w_ap[:, cb * 256:(cb + 1) * 256].rearrange("(kc p) c -> p kc c", p=128))],
                   f"wb{i}", writes=[f"WB{i}"])
        return i

    def mm2048(self, bank, wi, j, rhs, rhsname):
        s = self.s
        W, PB = self.WB[wi], self.PB
        for kc in range(KC):
            s.op("pe", lambda e, kc=kc: e.matmul(PB[:, bank, :], lhsT=W[:, kc, j * 128:(j + 1) * 128], rhs=rhs[:, kc, :],
                                               start=(kc == 0), stop=(kc == KC - 1)),
                 reads=[f"WB{wi}", (rhsname, kc)], writes=[("PB", bank)])

    def proj_add(self, w_ap, rhs, rhsname):
        s = self.s
        H, PB = self.H, self.PB
        for cb in range(D // 256):
            wi = self.load_wb(w_ap, cb, "w")
            for j in range(2):
                dc = cb * 2 + j
                b = self.bank()
                self.mm2048(b, wi, j, rhs, rhsname)
                s.op("dve", lambda e, dc=dc, b=b: e.tensor_tensor(out=H[:, dc, :], in0=H[:, dc, :], in1=PB[:, b, :], op=ALU.add),
                     reads=[("H", dc), ("PB", b)], writes=[("H", dc)])

    def ffn(self, wg_ap, wu_ap, wd_ap, gate_bc=None, gate_name=None):
        s = self.s
        H, PB, HT, UT = self.H, self.PB, self.HT, self.UT
        for cb in range(FF // 256):
            wg = self.load_wb(wg_ap, cb, "w")
            wu = self.load_wb(wu_ap, cb, "w")
            for j in range(2):
                fc = cb * 2 + j
                ba, bb = self.bank(), self.bank()
                self.mm2048(ba, wg, j, UT, "UT")
                self.mm2048(bb, wu, j, UT, "UT")
                sg = self.SG[self.sg_i % 2]
                sgn = f"SG{self.sg_i % 2}"
                self.sg_i += 1
                s.op("act", lambda e, sg=sg, ba=ba: e.activation(out=sg[:], in_=PB[:, ba, :], func=AF.Silu),
                     reads=[("PB", ba)], writes=[sgn])
                s.op("dve", lambda e, sg=sg, bb=bb, fc=fc: e.tensor_tensor(out=HT[:, fc, :], in0=sg[:], in1=PB[:, bb, :], op=ALU.mult),
                     reads=[sgn, ("PB", bb)], writes=[("HT", fc)])
        for cb in range(D // 512):
            banks = [self.bank() for _ in range(4)]
            for fg in range(FC // 11):
                i = self.wd_i % 2
                self.wd_i += 1
                WD = self.WD[i]
                s.dma("pool", [(WD[:], wd_ap[fg * 11 * 128:(fg + 1) * 11 * 128, cb * 512:(cb + 1) * 512]
                                .rearrange("(fc p) c -> p fc c", p=128))], f"wd{i}", writes=[f"WD{i}"])
                for jf in range(11):
                    fc = fg * 11 + jf
                    for d4 in range(4):
                        s.op("pe", lambda e, WD=WD, jf=jf, d4=d4, fc=fc, b=banks[d4]: e.matmul(
                            PB[:, b, :], lhsT=WD[:, jf, d4 * 128:(d4 + 1) * 128], rhs=HT[:, fc, :],
                            start=(fc == 0), stop=(fc == FC - 1)),
                            reads=[f"WD{i}", ("HT", fc)], writes=[("PB", banks[d4])])
            for d4 in range(4):
                dc = cb * 4 + d4
                b = banks[d4]
                if gate_bc is None:
                    s.op("dve", lambda e, dc=dc, b=b: e.tensor_tensor(out=H[:, dc, :], in0=H[:, dc, :], in1=PB[:, b, :], op=ALU.add),
                         reads=[("H", dc), ("PB", b)], writes=[("H", dc)])
                else:
                    tmp = self.TMP[d4 % 2]
                    tn = f"TMP{d4 % 2}"
                    s.op("dve", lambda e, tmp=tmp, b=b: e.tensor_tensor(out=tmp[:], in0=PB[:, b, :], in1=gate_bc, op=ALU.mult),
                         reads=[("PB", b), gate_name], writes=[tn])
                    s.op("dve", lambda e, tmp=tmp, dc=dc: e.tensor_tensor(out=H[:, dc, :], in0=H[:, dc, :], in1=tmp[:], op=ALU.add),
                         reads=[("H", dc), tn], writes=[("H", dc)])

    def ple(self, wpg_ap, wpp_ap):
        s = self.s
        H, PB, UT, PT = self.H, self.PB, self.UT, self.PT
        for cb in range(D // 256):
            wi = self.load_wb(wpg_ap, cb, "w")
            ip = self.wp_i % 2
            self.wp_i += 1
            WP = self.WP[ip]
            s.dma("pool", [(WP[:], wpp_ap[:, cb * 256:(cb + 1) * 256].rearrange("(kc p) c -> p kc c", p=128))],
                  f"wp{ip}", writes=[f"WP{ip}"])
            for j in range(2):
                dc = cb * 2 + j
                ba, bb = self.bank(), self.bank()
                self.mm2048(ba, wi, j, UT, "UT")
                for kc in range(2):
                    s.op("pe", lambda e, kc=kc, WP=WP, j=j, bb=bb: e.matmul(PB[:, bb, :], lhsT=WP[:, kc, j * 128:(j + 1) * 128],
                                                                         rhs=PT[:, kc, :], start=(kc == 0), stop=(kc == 1)),
                         reads=[f"WP{ip}", "PT"], writes=[("PB", bb)])
                sg = self.SG[self.sg_i % 2]
                sgn = f"SG{self.sg_i % 2}"
                self.sg_i += 1
                s.op("act", lambda e, sg=sg, ba=ba: e.activation(out=sg[:], in_=PB[:, ba, :], func=AF.Sigmoid),
                     reads=[("PB", ba)], writes=[sgn])
                tmp = self.TMP[j]
                tn = f"TMP{j}"
                s.op("dve", lambda e, tmp=tmp, sg=sg, bb=bb: e.tensor_tensor(out=tmp[:], in0=sg[:], in1=PB[:, bb, :], op=ALU.mult),
                     reads=[sgn, ("PB", bb)], writes=[tn])
                s.op("dve", lambda e, tmp=tmp, dc=dc: e.tensor_tensor(out=H[:, dc, :], in0=H[:, dc, :], in1=tmp[:], op=ALU.add),
                     reads=[("H", dc), tn], writes=[("H", dc)])


def vec_layout(v):
    return np.ascontiguousarray(np.asarray(v, np.float32).reshape(-1, 128).T)


def build_k2(NT):
    nc = bass.Bass("TRN2", target_bir_lowering=False)
    dt = lambda n, sh, kind="ExternalInput": nc.dram_tensor(n, sh, F32, kind=kind).ap()
    hT = dt("hT", [D, NT]); mT = dt("mT", [D, NT]); pT = dt("pT", [PLE, NT])
    w_out = dt("w_out", [D, D]); wg = dt("wg", [D, FF]); wu = dt("wu", [D, FF]); wd = dt("wd", [FF, D])
    wpg = dt("wpg", [D, D]); wpp = dt("wpp", [PLE, D])
    g_ffn = dt("g_ffn", [128, KC]); g_ple = dt("g_ple", [128, KC])
    oT = dt("oT", [D, NT], "ExternalOutput")
    s = Sched(nc)
    c = DenseCore(s)
    Gf = c.load_vec("Gf", g_ffn)
    Gp = c.load_vec("Gp", g_ple)
    evs = []
    for it in range(NT // TT):
        tok = slice(it * TT, (it + 1) * TT)
        c.load_tile(hT, tok)
        c.load_bf(c.MT, "MT", mT, tok, "m")
        c.load_bf(c.PT, "PT", pT, tok, "p")
        c.proj_add(w_out, c.MT, "MT")
        c.rmsnorm("Gf", Gf)
        c.ffn(wg, wu, wd)
        c.rmsnorm("Gp", Gp)
        c.ple(wpg, wpp)
        evs.append(c.store_tile(oT, tok))
    s.finish("sp", evs[-1:])
    s.emit()
    return nc


class MoECore(DenseCore):
    def __init__(self, s, ident_ap, sel_ap, wr_ap):
        super().__init__(s)
        sb = s.sbuf
        self.IDF = sb("IDF", [128, 128], F32)
        self.SEL = sb("SEL", [8, 8, 128], F32)
        self.WR32 = sb("WR32", [128, KC, 8], F32)
        self.WRH = sb("WRH", [128, KC, 8], BF16)
        self.WRL = sb("WRL", [128, KC, 8], BF16)
        self.LG = sb("LG", [128, 4, 8], F32)
        self.M8 = sb("M8", [128, 4, 8], F32)
        self.GT = sb("GT", [128, 4, 8], F32)
        self.SM = sb("SM", [128, 4, 4], F32)
        self.GTT = sb("GTT", [8, TT], F32)
        self.GB = [sb(f"GB{i}", [128, TT], F32) for i in range(2)]
        s.dma("sp", [(self.IDF[:], ident_ap)], "c_id", writes=["IDF"])
        s.dma("sp", [(self.SEL[:], sel_ap)], "c_sel", writes=["SEL"])
        s.dma("sp", [(self.WR32[:], wr_ap)], "c_wr", writes=["WR32"])
        s.op("act", lambda e: e.activation(out=self.WRH[:], in_=self.WR32[:], func=AF.Copy), reads=["WR32"], writes=["WRH"])
        s.op("dve", lambda e: e.tensor_tensor(out=self.WRL[:], in0=self.WR32[:], in1=self.WRH[:], op=ALU.subtract),
             reads=["WR32", "WRH"], writes=["WRL"])
        self.gb_i = 0

    def rmsnorm_hilo(self, Gname, G):
        s = self.s
        H, SQ, UT, RSTD, ONES, PB, HT = self.H, self.MT, self.UT, self.RSTD, self.ONES, self.PB, self.HT
        s.op("act", lambda e: e.activation(out=SQ[:], in_=H[:], func=AF.Square), reads=["H"], writes=["MT"])
        b = self.bank()
        for kc in range(KC):
            s.op("pe", lambda e, kc=kc: e.matmul(PB[:, b, :], lhsT=ONES[:], rhs=SQ[:, kc, :], start=(kc == 0), stop=(kc == KC - 1)),
                 reads=["ONES", "MT"], writes=[("PB", b)])
        s.op("act", lambda e: e.activation(out=RSTD[:], in_=PB[:, b, :], func=AF.Sqrt, bias=EPS, scale=1.0 / D),
             reads=[("PB", b)], writes=["RSTD"])
        s.op("dve", lambda e: e.reciprocal(out=RSTD[:], in_=RSTD[:]), reads=["RSTD"], writes=["RSTD"])
        for kc in range(KC):
            tmp = self.TMP[kc % 2]
            tn = f"TMP{kc % 2}"
            s.op("dve", lambda e, kc=kc, tmp=tmp: e.scalar_tensor_tensor(out=tmp[:], in0=H[:, kc, :], scalar=G[:, kc:kc + 1],
                                                                       in1=RSTD[:], op0=ALU.mult, op1=ALU.mult),
                 reads=[("H", kc), Gname, "RSTD"], writes=[tn])
            s.op("act", lambda e, kc=kc, tmp=tmp: e.activation(out=UT[:, kc, :], in_=tmp[:], func=AF.Copy), reads=[tn], writes=[("UT", kc)])
            s.op("dve", lambda e, kc=kc, tmp=tmp: e.tensor_tensor(out=HT[:, kc, :], in0=tmp[:], in1=UT[:, kc, :], op=ALU.subtract),
                 reads=[tn, ("UT", kc)], writes=[("HT", kc)])

    def router(self):
        s = self.s
        UT, HT, PB, LG, M8, GT, SM, GTT, IDF = self.UT, self.HT, self.PB, self.LG, self.M8, self.GT, self.SM, self.GTT, self.IDF
        WRH, WRL = self.WRH, self.WRL
        b = self.bank()
        for q in range(4):
            ts_ = slice(q * 128, (q + 1) * 128)
            n = 0
            for (a, an, w, wn) in ((UT, "UT", WRH, "WRH"), (HT, "HT", WRH, "WRH"), (UT, "UT", WRL, "WRL")):
                for kc in range(KC):
                    s.op("pe", lambda e, a=a, w=w, kc=kc, ts_=ts_, q=q, n=n: e.matmul(
                        PB[:, b, q * 8:(q + 1) * 8], lhsT=a[:, kc, ts_], rhs=w[:, kc, :], start=(n == 0), stop=(n == 3 * KC - 1)),
                        reads=[(an, kc), wn], writes=[("PB", b)])
                    n += 1
        s.op("act", lambda e: e.activation(out=LG[:], in_=PB[:, b, 0:32].rearrange("p (q e) -> p q e", q=4), func=AF.Copy),
             reads=[("PB", b)], writes=["LG"])
        for q in range(4):
            s.op("dve", lambda e, q=q: e.max(out=M8[:, q, :], in_=LG[:, q, :]), reads=["LG"], writes=[("M8", q)])
            s.op("dve", lambda e, q=q: e.tensor_scalar(out=GT[:, q, :], in0=LG[:, q, :], scalar1=M8[:, q, 1:2], scalar2=None, op0=ALU.is_ge),
                 reads=["LG", ("M8", q)], writes=[("GT", q)])
            s.op("dve", lambda e, q=q: e.tensor_scalar_mul(out=SM[:, q, 0:1], in0=M8[:, q, 0:1], scalar1=-1.0), reads=[("M8", q)], writes=[("SM", q)])
            s.op("act", lambda e, q=q: e.activation(out=LG[:, q, :], in_=LG[:, q, :], func=AF.Exp, bias=SM[:, q, 0:1]),
                 reads=["LG", ("SM", q)], writes=["LG"])
            s.op("dve", lambda e, q=q: e.tensor_tensor(out=GT[:, q, :], in0=GT[:, q, :], in1=LG[:, q, :], op=ALU.mult),
                 reads=["LG", ("GT", q)], writes=[("GT", q)])
            s.op("dve", lambda e, q=q: e.reduce_sum(out=SM[:, q, 1:2], in_=GT[:, q, :], axis=AX.X), reads=[("GT", q)], writes=[("SM", q)])
            s.op("dve", lambda e, q=q: e.reciprocal(out=SM[:, q, 1:2], in_=SM[:, q, 1:2]), reads=[("SM", q)], writes=[("SM", q)])
            s.op("dve", lambda e, q=q: e.tensor_scalar(out=GT[:, q, :], in0=GT[:, q, :], scalar1=SM[:, q, 1:2], scalar2=None, op0=ALU.mult),
                 reads=[("GT", q), ("SM", q)], writes=[("GT", q)])
        b2 = self.bank()
        for q in range(4):
            s.op("pe", lambda e, q=q: e.transpose(PB[0:8, b2, q * 128:(q + 1) * 128], GT[:, q, :], IDF[:]), reads=[("GT", q), "IDF"],
                 writes=[("PB", b2)])
        s.op("act", lambda e: e.activation(out=GTT[:], in_=PB[0:8, b2, :], func=AF.Copy), reads=[("PB", b2)], writes=["GTT"])

    def gate_bc(self, ex):
        s = self.s
        i = self.gb_i % 2
        self.gb_i += 1
        GB, PB = self.GB[i], self.PB
        b = self.bank()
        s.op("pe", lambda e: e.matmul(PB[:, b, :], lhsT=self.SEL[:, ex, :], rhs=self.GTT[:], start=True, stop=True),
             reads=["SEL", "GTT"], writes=[("PB", b)])
        s.op("act", lambda e: e.activation(out=GB[:], in_=PB[:, b, :], func=AF.Copy), reads=[("PB", b)], writes=[f"GB{i}"])
        return GB[:], f"GB{i}"

    def final_norm(self, Gname, G):
        s = self.s
        H, SQ, RSTD, ONES, PB = self.H, self.MT, self.RSTD, self.ONES, self.PB
        s.op("act", lambda e: e.activation(out=SQ[:], in_=H[:], func=AF.Square), reads=["H"], writes=["MT"])
        b = self.bank()
        for kc in range(KC):
            s.op("pe", lambda e, kc=kc: e.matmul(PB[:, b, :], lhsT=ONES[:], rhs=SQ[:, kc, :], start=(kc == 0), stop=(kc == KC - 1)),
                 reads=["ONES", "MT"], writes=[("PB", b)])
        s.op("act", lambda e: e.activation(out=RSTD[:], in_=PB[:, b, :], func=AF.Sqrt, bias=EPS, scale=1.0 / D),
             reads=[("PB", b)], writes=["RSTD"])
        s.op("dve", lambda e: e.reciprocal(out=RSTD[:], in_=RSTD[:]), reads=["RSTD"], writes=["RSTD"])
        for kc in range(KC):
            s.op("dve", lambda e, kc=kc: e.scalar_tensor_tensor(out=H[:, kc, :], in0=H[:, kc, :], scalar=G[:, kc:kc + 1],
                                                              in1=RSTD[:], op0=ALU.mult, op1=ALU.mult),
                 reads=[("H", kc), Gname, "RSTD"], writes=[("H", kc)])


def k4_consts():
    sel = np.zeros((8, 8, 128), np.float32)
    for e in range(8):
        sel[e, e, :] = 1.0
    return {"ident": np.eye(128, dtype=np.float32), "sel": sel}


def build_k4(NT, NEXP=8):
    nc = bass.Bass("TRN2", target_bir_lowering=False)
    dt = lambda n, sh, kind="ExternalInput": nc.dram_tensor(n, sh, F32, kind=kind).ap()
    hT = dt("hT", [D, NT]); mT = dt("mT", [D, NT]); pT = dt("pT", [PLE, NT])
    w_out = dt("w_out", [D, D])
    wg = dt("wg", [NEXP, D, FF]); wu = dt("wu", [NEXP, D, FF]); wd = dt("wd", [NEXP, FF, D])
    wr = dt("wr", [128, KC, 8])
    wpg = dt("wpg", [D, D]); wpp = dt("wpp", [PLE, D])
    g_ffn = dt("g_ffn", [128, KC]); g_ple = dt("g_ple", [128, KC]); g_fin = dt("g_fin", [128, KC])
    ident = dt("ident", [128, 128]); sel = dt("sel", [8, 8, 128])
    oT = dt("oT", [D, NT], "ExternalOutput")
    s = Sched(nc)
    c = MoECore(s, ident, sel, wr)
    Gf = c.load_vec("Gf", g_ffn)
    Gp = c.load_vec("Gp", g_ple)
    Gl = c.load_vec("Gl", g_fin)
    evs = []
    for it in range(NT // TT):
        tok = slice(it * TT, (it + 1) * TT)
        c.load_tile(hT, tok)
        c.load_bf(c.MT, "MT", mT, tok, "m")
        c.load_bf(c.PT, "PT", pT, tok, "p")
        c.proj_add(w_out, c.MT, "MT")
        c.rmsnorm_hilo("Gf", Gf)
        c.router()
        for ex in range(NEXP):
            gb, gbn = c.gate_bc(ex)
            c.ffn(wg[ex], wu[ex], wd[ex], gate_bc=gb, gate_name=gbn)
        c.rmsnorm("Gp", Gp)
        c.ple(wpg, wpp)
        c.final_norm("Gl", Gl)
        evs.append(c.store_tile(oT, tok))
    s.finish("sp", evs[-1:])
    s.emit()
    return nc


D = 2048
KC = 16
TT = 512
NCH = 8
C = 64
EPS = 1e-6
DK = 128
QSCALE = DK ** -0.5


def k1_consts():
    ident = np.eye(128, dtype=np.float32)
    s_idx = np.arange(C)[:, None]
    t_idx = np.arange(C)[None, :]
    m_u = (s_idx <= t_idx).astype(np.float32)
    m_su = (s_idx < t_idx).astype(np.float32)
    m_sl = (s_idx > t_idx).astype(np.float32)
    masks = np.stack([m_su, m_sl, m_u], 1)
    rst = np.ones((128, TT), np.float32)
    rst[:, ::C] = 0.0
    return {"ident": ident, "masks": np.ascontiguousarray(masks), "rst": rst}


def build_k1(NTOK, SEQ):
    nc = bass.Bass("TRN2", target_bir_lowering=False)
    dt = lambda n, sh, kind="ExternalInput": nc.dram_tensor(n, sh, F32, kind=kind).ap()
    xT = dt("xT", [D, NTOK])
    g_mix = dt("g_mix", [128, KC])
    w_fm = dt("w_fm", [D, 640])
    w_tok = dt("w_tok", [D, 384])
    w_ab = dt("w_ab", [D, 2])
    lbt = dt("lbt", [128, 3])
    convw = dt("convw", [128, 3, 4])
    sc2 = dt("sc2", [1, 2])
    gn = dt("gn", [C, 2, 128])
    ident_d = dt("ident", [128, 128])
    masks_d = dt("masks", [C, 3, C])
    rst_d = dt("rst", [128, TT])
    o = dt("o", [NTOK, 256], "ExternalOutput")

    s = Sched(nc)
    sb = s.sbuf
    H = sb("H", [128, KC, TT], F32)
    UT = sb("UT", [128, KC, TT], BF16)
    SQ = UT
    WFM = sb("WFM", [128, KC, 640], BF16)
    WTOK = sb("WTOK", [128, KC, 384], BF16)
    WAB = sb("WAB", [128, KC, 2], BF16)
    G = sb("G", [128, KC], F32)
    LBT = sb("LBT", [128, 3], F32)
    LB = sb("LB", [128, 4], F32)
    CW = sb("CW", [128, 3, 4], F32)
    SC2 = sb("SC2", [1, 2], F32)
    NEA = sb("NEA", [1, 1], F32)
    GN = sb("GN", [C, 2, 128], F32)
    IDF = sb("IDF", [128, 128], F32)
    IDB = sb("IDB", [128, 128], BF16)
    MASKS = sb("MASKS", [C, 3, C], F32)
    RST = sb("RST", [128, TT], F32)
    ONESB = sb("ONESB", [128, 128], BF16)
    ONER = sb("ONER", [1, 128], F32)
    RSTD = sb("RSTD", [128, TT], F32)
    TOK = sb("TOK", [C, NCH, 384], F32)
    SGT = sb("SGT", [C, NCH, 256], F32)
    AQ = sb("AQ", [128, TT], F32)
    FF_ = sb("FF", [128, TT], F32)
    LOGF = sb("LOGF", [128, TT], F32)
    KA = sb("KA", [128, TT], F32)
    BC = sb("BC", [128, TT], F32)
    EBP = sb("EBP", [128, TT], F32)
    ENB = sb("ENB", [128, TT], F32)
    E2 = sb("E2", [128, TT], F32)
    QE = sb("QE", [128, TT], BF16)
    KE = sb("KE", [128, TT], BF16)
    KE2T = sb("KE2T", [128, TT], BF16)
    KE2A = sb("KE2A", [C, NCH, 128], BF16)
    VA = sb("VA", [C, NCH, 128], BF16)
    STA = sb("STA", [C, NCH, C], BF16)
    SA = sb("SA", [128, 128], F32)
    SAB = sb("SAB", [128, 128], BF16)
    XR = sb("XR", [128, 3, TT + 3], F32)
    XC = sb("XC", [128, 3, TT], F32)
    CS = XC
    SQ2 = sb("SQ2", [128, 2, TT], BF16)
    RS2 = sb("RS2", [128, 2, TT], F32)
    QN = sb("QN", [128, TT], F32)
    KN = sb("KN", [128, TT], F32)
    KNB = sb("KNB", [128, TT], BF16)
    CVB = sb("CVB", [128, TT], BF16)
    EGB = sb("EGB", [128, TT], F32)
    QEB = sb("QEB", [128, TT], BF16)
    ROW = sb("ROW", [1, 8, TT], F32)
    R_SP, R_GAM, R_L, R_NGAM, R_EG, R_EGP, R_EGL, R_BETA = range(8)
    R_G = R_SP
    R_GAMP = R_L
    EE = sb("EE", [C, 3, C], F32)
    M_ = [sb(f"M{i}", [C, C], F32) for i in range(2)]
    N_ = [sb(f"N{i}", [C, C], F32) for i in range(2)]
    R_ = [sb(f"R{i}", [C, C], F32) for i in range(2)]
    TTB = sb("TTB", [C, C], BF16)
    QKT = sb("QKT", [C, C], BF16)
    COL = sb("COL", [C, 3], F32)
    XK = sb("XK", [C, 128], BF16)
    BV = sb("BV", [C, 128], BF16)
    KE2B = sb("KE2B", [C, 128], BF16)
    WTB = sb("WTB", [128, C], BF16)
    USB = sb("USB", [C, 128], F32)
    VN = sb("VN", [C, 128], BF16)
    SB_ = sb("SB", [128, 128], F32)
    SBB = sb("SBB", [128, 128], BF16)
    OUTT = sb("OUTT", [C, NCH, 256], F32)
    SQO = sb("SQO", [C, 128], F32)
    SSO = sb("SSO", [C, 2], F32)
    TMPO = sb("TMPO", [C, 128], F32)

    PS = [s.psum(f"PS{i}", [128, 512], F32) for i in range(5)] + [s.psum("PS5", [128, 1024], BF16)] + \
         [s.psum(f"PS{i}", [128, 512], F32) for i in (6, 7)]

    op = s.op
    s.dma("sp", [(G[:], g_mix)], "c0", writes=["G"])
    s.dma("sp", [(LBT[:], lbt)], "c1", writes=["LBT"])
    s.dma("sp", [(CW[:], convw)], "c2", writes=["CW"])
    s.dma("sp", [(SC2[:], sc2)], "c3", writes=["SC2"])
    s.dma("sp", [(GN[:], gn)], "c4", writes=["GN"])
    s.dma("sp", [(IDF[:], ident_d)], "c5", writes=["IDF"])
    s.dma("sp", [(MASKS[:], masks_d)], "c6", writes=["MASKS"])
    s.dma("sp", [(RST[:], rst_d)], "c7", writes=["RST"])
    s.dma("pool", [(IDB[:], ident_d)], "c8", writes=["IDB"])
    s.dma("pool", [(WFM[:], w_fm.rearrange("(kc p) c -> p kc c", p=128))], "c9", writes=["WFM"])
    s.dma("pool", [(WTOK[:], w_tok.rearrange("(kc p) c -> p kc c", p=128))], "c10", writes=["WTOK"])
    s.dma("pool", [(WAB[:], w_ab.rearrange("(kc p) c -> p kc c", p=128))], "c11", writes=["WAB"])
    op("pool", lambda e: e.memset(ONESB[:], 1.0), writes=["ONESB"])
    op("pool", lambda e: e.memset(ONER[:], 1.0), writes=["ONER"])
    op("act", lambda e: e.activation(out=LBT[:], in_=LBT[:], func=AF.Exp), reads=["LBT"], writes=["LBT"])
    op("dve", lambda e: e.reduce_sum(out=LB[:, 2:3], in_=LBT[:], axis=AX.X), reads=["LBT"], writes=["LB"])
    op("dve", lambda e: e.reciprocal(out=LB[:, 2:3], in_=LB[:, 2:3]), reads=["LB"], writes=["LB"])
    op("dve", lambda e: e.tensor_tensor(out=LB[:, 0:1], in0=LBT[:, 0:1], in1=LB[:, 2:3], op=ALU.mult), reads=["LB", "LBT"], writes=["LB"])
    op("dve", lambda e: e.tensor_scalar(out=LB[:, 1:2], in0=LB[:, 0:1], scalar1=-1.0, scalar2=1.0, op0=ALU.mult, op1=ALU.add),
       reads=["LB"], writes=["LB"])
    op("act", lambda e: e.activation(out=NEA[:], in_=SC2[:, 0:1], func=AF.Exp), reads=["SC2"], writes=["NEA"])
    op("dve", lambda e: e.tensor_scalar_mul(out=NEA[:], in0=NEA[:], scalar1=-1.0), reads=["NEA"], writes=["NEA"])

    ntile = NTOK // TT
    tpb = SEQ // TT
    big_i = [0]

    def big():
        b = big_i[0] % 2
        big_i[0] += 1
        return b

    evs = []
    for it in range(ntile):
        first = (it % tpb == 0)
        tok0 = it * TT
        s.dma("sp", [(H[:], xT[:, tok0:tok0 + TT].rearrange("(kc p) t -> p kc t", p=128))], "h", writes=["H"])
        op("act", lambda e: e.activation(out=SQ[:], in_=H[:], func=AF.Square), reads=["H"], writes=["UT"])
        b = big()
        for kc in range(KC):
            op("pe", lambda e, kc=kc, b=b: e.matmul(PS[b][:], lhsT=ONESB[:], rhs=SQ[:, kc, :], start=(kc == 0), stop=(kc == KC - 1)),
               reads=["ONESB", "UT"], writes=[f"PS{b}"])
        op("act", lambda e, b=b: e.activation(out=RSTD[:], in_=PS[b][:], func=AF.Sqrt, bias=EPS, scale=1.0 / D),
           reads=[f"PS{b}"], writes=["RSTD"])
        op("dve", lambda e: e.reciprocal(out=RSTD[:], in_=RSTD[:]), reads=["RSTD"], writes=["RSTD"])
        for kc in range(KC):
            op("dve", lambda e, kc=kc: e.scalar_tensor_tensor(out=UT[:, kc, :], in0=H[:, kc, :], scalar=G[:, kc:kc + 1],
                                                            in1=RSTD[:], op0=ALU.mult, op1=ALU.mult),
               reads=["H", "G", "RSTD"], writes=[("UT", kc)])
        if first:
            op("pool", lambda e: e.memset(XR[:, :, 0:3], 0.0), writes=["XR"])
            op("pool", lambda e: e.memset(SA[:], 0.0), writes=["SA"])
            op("pool", lambda e: e.memset(SAB[:], 0.0), writes=["SAB"])
            op("pool", lambda e: e.memset(SB_[:], 0.0), writes=["SB"])
            op("pool", lambda e: e.memset(SBB[:], 0.0), writes=["SBB"])

        def fm_proj(col, M, b):
            for kc in range(KC):
                op("pe", lambda e, kc=kc: e.matmul(PS[b][0:M, :], lhsT=(WFM[:, kc, col * 128:(col + 1) * 128] if M == 128 else WAB[:, kc, col:col + 1]),
                                                 rhs=UT[:, kc, :], start=(kc == 0), stop=(kc == KC - 1)),
                   reads=["WFM", "WAB", ("UT", kc)], writes=[f"PS{b}"])
        b = big(); fm_proj(0, 128, b)
        op("act", lambda e, b=b: e.activation(out=AQ[:], in_=PS[b][:], func=AF.Copy), reads=[f"PS{b}"], writes=["AQ"])
        b = big(); fm_proj(1, 128, b)
        op("act", lambda e, b=b: e.activation(out=FF_[:], in_=PS[b][:], func=AF.Sigmoid), reads=[f"PS{b}"], writes=["FF"])
        for i in range(3):
            b = big(); fm_proj(2 + i, 128, b)
            op("act", lambda e, b=b, i=i: e.activation(out=XR[:, i, 3:TT + 3], in_=PS[b][:], func=AF.Copy), reads=[f"PS{b}"], writes=["XR"])
        b = big(); fm_proj(0, 1, b)
        op("act", lambda e, b=b: e.activation(out=ROW[:, R_SP, :], in_=PS[b][0:1, :], func=AF.Exp, bias=SC2[:, 1:2]),
           reads=[f"PS{b}", "SC2"], writes=[("ROW", R_SP)])
        op("act", lambda e: e.activation(out=ROW[:, R_SP, :], in_=ROW[:, R_SP, :], func=AF.Ln, bias=1.0),
           reads=[("ROW", R_SP)], writes=[("ROW", R_SP)])
        op("dve", lambda e: e.tensor_scalar(out=ROW[:, R_G, :], in0=ROW[:, R_SP, :], scalar1=NEA[:, 0:1], scalar2=None, op0=ALU.mult),
           reads=[("ROW", R_SP), "NEA"], writes=[("ROW", R_G)])
        b = big(); fm_proj(1, 1, b)
        op("act", lambda e, b=b: e.activation(out=ROW[:, R_BETA, :], in_=PS[b][0:1, :], func=AF.Sigmoid),
           reads=[f"PS{b}"], writes=[("ROW", R_BETA)])
        op("act", lambda e, b=b: e.activation(out=ROW[:, R_L, :], in_=PS[b][0:1, :], func=AF.Exp, scale=-1.0),
           reads=[f"PS{b}"], writes=[("ROW", R_L)])
        op("act", lambda e: e.activation(out=ROW[:, R_L, :], in_=ROW[:, R_L, :], func=AF.Ln, bias=1.0),
           reads=[("ROW", R_L)], writes=[("ROW", R_L)])
        for j in range(NCH):
            pb = 2 + (j % 2)
            for kc in range(KC):
                op("pe", lambda e, kc=kc, j=j, pb=pb: e.matmul(PS[pb][0:C, 0:384], lhsT=UT[:, kc, j * C:(j + 1) * C], rhs=WTOK[:, kc, :],
                                                            start=(kc == 0), stop=(kc == KC - 1)),
                   reads=["WTOK", ("UT", kc)], writes=[f"PS{pb}"])
            op("act", lambda e, j=j, pb=pb: e.activation(out=TOK[:, j, :], in_=PS[pb][0:C, 0:384], func=AF.Copy),
               reads=[f"PS{pb}"], writes=[("TOK", j)])
        op("act", lambda e: e.activation(out=SGT[:], in_=TOK[:, :, 128:384], func=AF.Silu), reads=["TOK"], writes=["SGT"])
        op("pool", lambda e: e.tensor_copy(out=VA[:], in_=TOK[:, :, 0:128]), reads=["TOK"], writes=["VA"])

        op("dve", lambda e: e.tensor_scalar(out=FF_[:], in0=FF_[:], scalar1=LB[:, 1:2], scalar2=LB[:, 0:1], op0=ALU.mult, op1=ALU.add),
           reads=["FF", "LB"], writes=["FF"])
        op("act", lambda e: e.activation(out=LOGF[:], in_=FF_[:], func=AF.Ln), reads=["FF"], writes=["LOGF"])
        op("dve", lambda e: e.tensor_scalar(out=KA[:], in0=FF_[:], scalar1=-1.0, scalar2=1.0, op0=ALU.mult, op1=ALU.add),
           reads=["FF"], writes=["KA"])
        op("dve", lambda e: e.tensor_tensor_scan(out=BC[:], data0=RST[:], data1=LOGF[:], initial=0.0, op0=ALU.mult, op1=ALU.add),
           reads=["RST", "LOGF"], writes=["BC"])
        op("act", lambda e: e.activation(out=EBP[:], in_=BC[:], func=AF.Exp), reads=["BC"], writes=["EBP"])
        op("act", lambda e: e.activation(out=ENB[:], in_=BC[:], func=AF.Exp, scale=-1.0), reads=["BC"], writes=["ENB"])
        for j in range(NCH):
            op("act", lambda e, j=j: e.activation(out=E2[:, j * C:(j + 1) * C], in_=BC[:, j * C:(j + 1) * C], func=AF.Exp, scale=-1.0,
                                                bias=BC[:, (j + 1) * C - 1:(j + 1) * C]), reads=["BC"], writes=["E2"])
        op("dve", lambda e: e.scalar_tensor_tensor(out=QE[:], in0=AQ[:], scalar=QSCALE, in1=EBP[:], op0=ALU.mult, op1=ALU.mult),
           reads=["AQ", "EBP"], writes=["QE"])
        op("dve", lambda e: e.tensor_tensor(out=KE[:], in0=KA[:], in1=ENB[:], op=ALU.mult), reads=["KA", "ENB"], writes=["KE"])
        op("dve", lambda e: e.tensor_tensor(out=KE2T[:], in0=KA[:], in1=E2[:], op=ALU.mult), reads=["KA", "E2"], writes=["KE2T"])

        for i in range(3):
            op("dve", lambda e, i=i: e.tensor_scalar(out=XC[:, i, :], in0=XR[:, i, 3:TT + 3], scalar1=CW[:, i, 3:4], scalar2=None, op0=ALU.mult),
               reads=["XR", "CW"], writes=[("XC", i)])
            for k in range(3):
                op("dve", lambda e, i=i, k=k: e.scalar_tensor_tensor(out=XC[:, i, :], in0=XR[:, i, k:TT + k], scalar=CW[:, i, k:k + 1],
                                                                    in1=XC[:, i, :], op0=ALU.mult, op1=ALU.add),
                   reads=["XR", "CW", ("XC", i)], writes=[("XC", i)])
        op("pool", lambda e: e.tensor_copy(out=XR[:, :, 0:3], in_=XR[:, :, TT:TT + 3]), reads=["XR", "XC"], writes=["XR"])
        op("act", lambda e: e.activation(out=CS[:], in_=XC[:], func=AF.Silu), reads=["XC"], writes=["XC"])
        op("act", lambda e: e.activation(out=SQ2[:], in_=CS[:, 0:2, :], func=AF.Square), reads=["XC"], writes=["SQ2"])
        for i in range(2):
            b = big()
            op("pe", lambda e, i=i, b=b: e.matmul(PS[b][:], lhsT=ONESB[:], rhs=SQ2[:, i, :], start=True, stop=True),
               reads=["ONESB", "SQ2"], writes=[f"PS{b}"])
            op("act", lambda e, i=i, b=b: e.activation(out=RS2[:, i, :], in_=PS[b][:], func=AF.Sqrt, bias=EPS), reads=[f"PS{b}"], writes=[("RS2", i)])
        op("dve", lambda e: e.reciprocal(out=RS2[:], in_=RS2[:]), reads=["RS2"], writes=["RS2"])
        op("dve", lambda e: e.scalar_tensor_tensor(out=QN[:], in0=CS[:, 0, :], scalar=QSCALE, in1=RS2[:, 0, :], op0=ALU.mult, op1=ALU.mult),
           reads=["XC", "RS2"], writes=["QN"])
        op("dve", lambda e: e.tensor_tensor(out=KN[:], in0=CS[:, 1, :], in1=RS2[:, 1, :], op=ALU.mult), reads=["XC", "RS2"], writes=["KN"])
        op("act", lambda e: e.activation(out=KNB[:], in_=KN[:], func=AF.Copy), reads=["KN"], writes=["KNB"])
        op("act", lambda e: e.activation(out=CVB[:], in_=CS[:, 2, :], func=AF.Copy), reads=["XC"], writes=["CVB"])
        op("dve", lambda e: e.tensor_tensor_scan(out=ROW[:, R_GAM, :], data0=RST[0:1, :], data1=ROW[:, R_G, :], initial=0.0,
                                                 op0=ALU.mult, op1=ALU.add), reads=["RST", ("ROW", R_G)], writes=[("ROW", R_GAM)])
        op("dve", lambda e: e.tensor_tensor(out=ROW[:, R_GAMP, :], in0=ROW[:, R_GAM, :], in1=ROW[:, R_L, :], op=ALU.subtract),
           reads=[("ROW", R_GAM), ("ROW", R_L)], writes=[("ROW", R_GAMP)])
        op("dve", lambda e: e.tensor_scalar_mul(out=ROW[:, R_NGAM, :], in0=ROW[:, R_GAM, :], scalar1=-1.0),
           reads=[("ROW", R_GAM)], writes=[("ROW", R_NGAM)])
        op("act", lambda e: e.activation(out=ROW[:, R_EG, :], in_=ROW[:, R_GAM, :], func=AF.Exp), reads=[("ROW", R_GAM)], writes=[("ROW", R_EG)])
        op("act", lambda e: e.activation(out=ROW[:, R_EGP, :], in_=ROW[:, R_GAMP, :], func=AF.Exp), reads=[("ROW", R_GAMP)], writes=[("ROW", R_EGP)])
        for j in range(NCH):
            op("act", lambda e, j=j: e.activation(out=ROW[:, R_EGL, j * C:(j + 1) * C], in_=ROW[:, R_GAM, j * C:(j + 1) * C], func=AF.Exp,
                                                scale=-1.0, bias=ROW[:, R_GAM, (j + 1) * C - 1:(j + 1) * C]),
               reads=[("ROW", R_GAM)], writes=[("ROW", R_EGL)])
        b = big()
        op("pe", lambda e, b=b: e.matmul(PS[b][:], lhsT=ONER[0:1, :], rhs=ROW[:, R_EG, :], start=True, stop=True),
           reads=["ONER", ("ROW", R_EG)], writes=[f"PS{b}"])
        op("act", lambda e, b=b: e.activation(out=EGB[:], in_=PS[b][:], func=AF.Copy), reads=[f"PS{b}"], writes=["EGB"])
        op("dve", lambda e: e.tensor_tensor(out=QEB[:], in0=QN[:], in1=EGB[:], op=ALU.mult), reads=["QN", "EGB"], writes=["QEB"])

        P4, P5, P6, P7 = PS[4], PS[5], PS[6], PS[7]
        for j in range(NCH):
            cs = slice(j * C, (j + 1) * C)
            last = slice((j + 1) * C - 1, (j + 1) * C)
            op("pe", lambda e, cs=cs: e.transpose(P5[0:C, 0:128], KE2T[:, cs], IDB[:]), reads=["KE2T", "IDB"], writes=[("PS5", 0)])
            op("act", lambda e, j=j: e.activation(out=KE2A[:, j, :], in_=P5[0:C, 0:128], func=AF.Copy), reads=[("PS5", 0)], writes=[("KE2A", j)])
            op("pe", lambda e, cs=cs: e.matmul(P6[0:C, 192:256], lhsT=KE[:, cs], rhs=QE[:, cs], start=True, stop=True),
               reads=["KE", "QE"], writes=[("PS6", "sc")])
            op("dve", lambda e, j=j: e.tensor_tensor(out=STA[:, j, :], in0=P6[0:C, 192:256], in1=MASKS[:, 2, :], op=ALU.mult),
               reads=[("PS6", "sc"), "MASKS"], writes=[("STA", j)])
            op("pe", lambda e, j=j: e.matmul(P6[:, 256:384], lhsT=KE2A[:, j, :], rhs=VA[:, j, :], start=True, stop=True),
               reads=[("KE2A", j), "VA"], writes=[("PS6", "kva")])
            op("pe", lambda e, j=j: e.matmul(P7[0:C, 384:512], lhsT=STA[:, j, :], rhs=VA[:, j, :], start=True, stop=False),
               reads=[("STA", j), "VA"], writes=[("PS7", "oa")])
            op("pe", lambda e, cs=cs: e.matmul(P7[0:C, 384:512], lhsT=QE[:, cs], rhs=SAB[:], start=False, stop=True),
               reads=["QE", "SAB"], writes=[("PS7", "oa")])
            op("dve", lambda e, last=last: e.scalar_tensor_tensor(out=SA[:], in0=SA[:], scalar=EBP[:, last], in1=P6[:, 256:384],
                                                                 op0=ALU.mult, op1=ALU.add),
               reads=["SA", "EBP", ("PS6", "kva")], writes=["SA"])
            op("act", lambda e: e.activation(out=SAB[:], in_=SA[:], func=AF.Copy), reads=["SA"], writes=["SAB"])

            def out_norm(ps_ap, pskey, gi, j=j):
                op("act", lambda e: e.activation(out=SQO[:], in_=ps_ap, func=AF.Square), reads=[pskey], writes=["SQO"])
                op("dve", lambda e: e.reduce_sum(out=SSO[:, 0:1], in_=SQO[:], axis=AX.X), reads=["SQO"], writes=["SSO"])
                op("act", lambda e: e.activation(out=SSO[:, 1:2], in_=SSO[:, 0:1], func=AF.Sqrt, bias=EPS, scale=1.0 / 128),
                   reads=["SSO"], writes=["SSO"])
                op("dve", lambda e: e.reciprocal(out=SSO[:, 1:2], in_=SSO[:, 1:2]), reads=["SSO"], writes=["SSO"])
                op("dve", lambda e: e.scalar_tensor_tensor(out=TMPO[:], in0=ps_ap, scalar=SSO[:, 1:2], in1=GN[:, gi, :],
                                                           op0=ALU.mult, op1=ALU.mult), reads=[pskey, "SSO", "GN"], writes=["TMPO"])
                op("dve", lambda e: e.tensor_tensor(out=OUTT[:, j, gi * 128:(gi + 1) * 128], in0=TMPO[:], in1=SGT[:, j, gi * 128:(gi + 1) * 128],
                                                    op=ALU.mult), reads=["TMPO", "SGT"], writes=[("OUTT", j)])
            out_norm(P7[0:C, 384:512], ("PS7", "oa"), 0)

            op("pe", lambda e, cs=cs: e.matmul(P4[0:C, 0:64], lhsT=KN[:, cs], rhs=KN[:, cs], start=True, stop=True),
               reads=["KN"], writes=[("PS4", "kk")])
            op("pe", lambda e, cs=cs: e.matmul(P4[0:C, 64:128], lhsT=KN[:, cs], rhs=QN[:, cs], start=True, stop=True),
               reads=["KN", "QN"], writes=[("PS4", "qk")])
            op("pe", lambda e, cs=cs: e.matmul(P4[0:C, 128:192], lhsT=ONER[0:1, 0:C], rhs=ROW[:, R_GAMP, cs], start=True, stop=False),
               reads=["ONER", ("ROW", R_GAMP)], writes=[("PS4", "d")])
            op("pe", lambda e, cs=cs: e.matmul(P4[0:C, 128:192], lhsT=ROW[:, R_NGAM, cs], rhs=ONER[0:1, 0:C], start=False, stop=True),
               reads=["ONER", ("ROW", R_NGAM)], writes=[("PS4", "d")])
            op("pe", lambda e, cs=cs: e.matmul(P4[0:C, 192:256], lhsT=ROW[:, R_GAMP, cs], rhs=ONER[0:1, 0:C], start=True, stop=False),
               reads=["ONER", ("ROW", R_GAMP)], writes=[("PS4", "d")])
            op("pe", lambda e, cs=cs: e.matmul(P4[0:C, 192:256], lhsT=ONER[0:1, 0:C], rhs=ROW[:, R_NGAM, cs], start=False, stop=True),
               reads=["ONER", ("ROW", R_NGAM)], writes=[("PS4", "d")])
            op("pe", lambda e, cs=cs: e.matmul(P4[0:C, 256:320], lhsT=ONER[0:1, 0:C], rhs=ROW[:, R_GAM, cs], start=True, stop=False),
               reads=["ONER", ("ROW", R_GAM)], writes=[("PS4", "d")])
            op("pe", lambda e, cs=cs: e.matmul(P4[0:C, 256:320], lhsT=ROW[:, R_NGAM, cs], rhs=ONER[0:1, 0:C], start=False, stop=True),
               reads=["ONER", ("ROW", R_NGAM)], writes=[("PS4", "d")])
            op("dve", lambda e: e.tensor_scalar_min(out=EE[:], in0=P4[0:C, 128:320].rearrange("p (a b) -> p a b", a=3), scalar1=0.0),
               reads=[("PS4", "d")], writes=["EE"])
            op("act", lambda e: e.activation(out=EE[:], in_=EE[:], func=AF.Exp), reads=["EE"], writes=["EE"])
            op("dve", lambda e: e.tensor_tensor(out=EE[:], in0=EE[:], in1=MASKS[:], op=ALU.mult), reads=["EE", "MASKS"], writes=["EE"])
            op("dve", lambda e: e.scalar_tensor_tensor(out=M_[0][:], in0=P4[0:C, 0:64], scalar=-1.0, in1=EE[:, 0, :], op0=ALU.mult, op1=ALU.mult),
               reads=[("PS4", "kk"), "EE"], writes=["M0"])
            op("dve", lambda e: e.scalar_tensor_tensor(out=N_[0][:], in0=P4[0:C, 0:64], scalar=-1.0, in1=EE[:, 1, :], op0=ALU.mult, op1=ALU.mult),
               reads=[("PS4", "kk"), "EE"], writes=["N0"])
            op("dve", lambda e: e.tensor_tensor(out=QKT[:], in0=P4[0:C, 64:128], in1=EE[:, 2, :], op=ALU.mult),
               reads=[("PS4", "qk"), "EE"], writes=["QKT"])
            op("dve", lambda e: e.tensor_tensor(out=R_[0][:], in0=M_[0][:], in1=IDF[0:C, 0:C], op=ALU.add), reads=["M0", "IDF"], writes=["R0"])
            cur = 0
            for lvl in range(1, 6):
                nx = 1 - cur
                lastl = (lvl == 5)
                if not lastl:
                    op("pe", lambda e, cur=cur: e.matmul(P4[0:C, 320:384], lhsT=N_[cur][:], rhs=M_[cur][:], start=True, stop=True),
                       reads=[f"N{cur}", f"M{cur}"], writes=[("PS4", "m")])
                op("pe", lambda e, cur=cur: e.matmul(P4[0:C, 384:448], lhsT=M_[cur][:], rhs=N_[cur][:], start=True, stop=True),
                   reads=[f"N{cur}", f"M{cur}"], writes=[("PS4", "n")])
                if not lastl:
                    op("act", lambda e, nx=nx: e.activation(out=M_[nx][:], in_=P4[0:C, 320:384], func=AF.Copy), reads=[("PS4", "m")], writes=[f"M{nx}"])
                op("act", lambda e, nx=nx: e.activation(out=N_[nx][:], in_=P4[0:C, 384:448], func=AF.Copy), reads=[("PS4", "n")], writes=[f"N{nx}"])
                op("pe", lambda e, cur=cur, nx=nx: e.matmul(P4[0:C, 448:512], lhsT=N_[nx][:], rhs=R_[cur][:], start=True, stop=True),
                   reads=[f"N{nx}", f"R{cur}"], writes=[("PS4", "r")])
                if lastl:
                    op("dve", lambda e, cur=cur: e.tensor_tensor(out=TTB[:], in0=R_[cur][:], in1=P4[0:C, 448:512], op=ALU.add),
                       reads=[f"R{cur}", ("PS4", "r")], writes=["TTB"])
                else:
                    op("dve", lambda e, cur=cur, nx=nx: e.tensor_tensor(out=R_[nx][:], in0=R_[cur][:], in1=P4[0:C, 448:512], op=ALU.add),
                       reads=[f"R{cur}", ("PS4", "r")], writes=[f"R{nx}"])
                cur = nx
            op("pe", lambda e, cs=cs: e.transpose(P5[0:C, 128:256], KNB[:, cs], IDB[:]), reads=["KNB", "IDB"], writes=[("PS5", 1)])
            op("pe", lambda e, cs=cs: e.transpose(P5[0:C, 256:384], CVB[:, cs], IDB[:]), reads=["CVB", "IDB"], writes=[("PS5", 2)])
            for ci, rr in enumerate((R_EGP, R_BETA, R_EGL)):
                op("pe", lambda e, cs=cs, ci=ci, rr=rr: e.matmul(P6[0:C, 384 + ci:385 + ci], lhsT=ROW[:, rr, cs], rhs=ONER[0:1, 0:1], start=True, stop=True),
                   reads=[("ROW", rr), "ONER"], writes=[("PS6", "col")])
            op("act", lambda e: e.activation(out=COL[:], in_=P6[0:C, 384:387], func=AF.Copy), reads=[("PS6", "col")], writes=["COL"])
            op("dve", lambda e: e.tensor_scalar(out=XK[:], in0=P5[0:C, 128:256], scalar1=COL[:, 0:1], scalar2=None, op0=ALU.mult),
               reads=[("PS5", 1), "COL"], writes=["XK"])
            op("dve", lambda e: e.tensor_scalar(out=KE2B[:], in0=P5[0:C, 128:256], scalar1=COL[:, 2:3], scalar2=None, op0=ALU.mult),
               reads=[("PS5", 1), "COL"], writes=["KE2B"])
            op("dve", lambda e: e.tensor_scalar(out=BV[:], in0=P5[0:C, 256:384], scalar1=COL[:, 1:2], scalar2=None, op0=ALU.mult),
               reads=[("PS5", 2), "COL"], writes=["BV"])
            op("pe", lambda e: e.matmul(P6[:, 0:64], lhsT=XK[:], rhs=TTB[:], start=True, stop=True), reads=["XK", "TTB"], writes=[("PS6", "wt")])
            op("act", lambda e: e.activation(out=WTB[:], in_=P6[:, 0:64], func=AF.Copy), reads=[("PS6", "wt")], writes=["WTB"])
            op("pe", lambda e: e.matmul(P6[0:C, 64:192], lhsT=TTB[:], rhs=BV[:], start=True, stop=True), reads=["BV", "TTB"], writes=[("PS6", "u")])
            op("act", lambda e: e.activation(out=USB[:], in_=P6[0:C, 64:192], func=AF.Copy), reads=[("PS6", "u")], writes=["USB"])
            op("pe", lambda e: e.matmul(P7[0:C, 0:128], lhsT=WTB[:], rhs=SBB[:], start=True, stop=True), reads=["WTB", "SBB"], writes=[("PS7", "ws")])
            op("dve", lambda e: e.tensor_tensor(out=VN[:], in0=USB[:], in1=P7[0:C, 0:128], op=ALU.subtract), reads=["USB", ("PS7", "ws")], writes=["VN"])
            op("pe", lambda e, cs=cs: e.matmul(P7[0:C, 128:256], lhsT=QEB[:, cs], rhs=SBB[:], start=True, stop=False),
               reads=["QEB", "SBB"], writes=[("PS7", "ob")])
            op("pe", lambda e: e.matmul(P7[0:C, 128:256], lhsT=QKT[:], rhs=VN[:], start=False, stop=True), reads=["QKT", "VN"], writes=[("PS7", "ob")])
            op("pe", lambda e: e.matmul(P7[:, 256:384], lhsT=KE2B[:], rhs=VN[:], start=True, stop=True), reads=["KE2B", "VN"], writes=[("PS7", "kvb")])
            op("dve", lambda e, last=last: e.scalar_tensor_tensor(out=SB_[:], in0=SB_[:], scalar=EGB[:, last], in1=P7[:, 256:384],
                                                                 op0=ALU.mult, op1=ALU.add), reads=["SB", "EGB", ("PS7", "kvb")], writes=["SB"])
            op("act", lambda e: e.activation(out=SBB[:], in_=SB_[:], func=AF.Copy), reads=["SB"], writes=["SBB"])
            out_norm(P7[0:C, 128:256], ("PS7", "ob"), 1)
        evs.append(s.dma("sp", [(o[tok0:tok0 + TT, :].rearrange("(j p) c -> p j c", p=C), OUTT[:])], "st", reads=["OUTT"]))
    s.finish("sp", evs[-1:])
    s.emit()
    return nc


D = 2048
KC = 16
TT = 512
EPS = 1e-6
CB = 256


def build_k3(NTOK, SEQ):
    nc = bass.Bass("TRN2", target_bir_lowering=False)
    dt = lambda n, sh, kind="ExternalInput": nc.dram_tensor(n, sh, F32, kind=kind).ap()
    hT = dt("hT", [D, NTOK])
    g_mix = dt("g_mix", [128, KC])
    w_y = dt("w_y", [D, CB]); w_x = dt("w_x", [D, CB])
    w_r = dt("w_r", [CB, CB]); w_i = dt("w_i", [CB, CB])
    cvec = dt("cvec", [128, 2, 8])
    oT = dt("oT", [CB, NTOK], "ExternalOutput")
    s = Sched(nc)
    H = [s.sbuf(f"H{i}", [128, KC, TT], F32) for i in range(2)]
    SQ = s.sbuf("SQ", [128, KC, TT], BF16)
    UT = s.sbuf("UT", [128, KC, TT], BF16)
    WY = s.sbuf("WY", [128, KC, CB], BF16)
    WX = s.sbuf("WX", [128, KC, CB], BF16)
    WR = s.sbuf("WR", [128, 2, CB], BF16)
    WI = s.sbuf("WI", [128, 2, CB], BF16)
    CV = s.sbuf("CV", [128, 2, 8], F32)
    C8 = s.sbuf("C8", [128, 2], F32)
    G = s.sbuf("G", [128, KC], F32)
    ONES = s.sbuf("ONES", [128, 128], BF16)
    RSTD = s.sbuf("RSTD", [128, TT], F32)
    YF = s.sbuf("YF", [128, 2, TT], F32)
    T1 = s.sbuf("T1", [128, 2, TT], F32)
    GY = s.sbuf("GY", [128, 2, TT], F32)
    XR = s.sbuf("XR", [128, 2, TT + 3], F32)
    XC = s.sbuf("XC", [128, 2, TT], F32)
    XCB = s.sbuf("XCB", [128, 2, TT], BF16)
    RG = s.sbuf("RG", [128, 2, TT], F32)
    IG = s.sbuf("IG", [128, 2, TT], F32)
    AA = s.sbuf("AA", [128, 2, TT], F32)
    MM = s.sbuf("MM", [128, 2, TT], F32)
    BB = s.sbuf("BB", [128, 2, TT], F32)
    HS = [s.sbuf(f"HS{i}", [128, 2, TT], F32) for i in range(2)]
    OUT = [s.sbuf(f"OUT{i}", [128, 2, TT], F32) for i in range(2)]
    PB = s.psum("PB", [128, 8, TT])
    pbi = [0]

    def bank():
        b = pbi[0] % 8
        pbi[0] += 1
        return b

    s.op("pool", lambda e: e.memset(ONES[:], 1.0), writes=["ONES"])
    s.dma("sp", [(G[:], g_mix)], "c0", writes=["G"])
    s.dma("sp", [(CV[:], cvec)], "c1", writes=["CV"])
    s.dma("pool", [(WY[:], w_y.rearrange("(kc p) c -> p kc c", p=128))], "c2", writes=["WY"])
    s.dma("pool", [(WX[:], w_x.rearrange("(kc p) c -> p kc c", p=128))], "c3", writes=["WX"])
    s.dma("pool", [(WR[:], w_r.rearrange("(kc p) c -> p kc c", p=128))], "c4", writes=["WR"])
    s.dma("pool", [(WI[:], w_i.rearrange("(kc p) c -> p kc c", p=128))], "c5", writes=["WI"])
    s.op("act", lambda e: e.activation(out=C8[:], in_=CV[:, :, 7], func=AF.Exp, scale=-1.0), reads=["CV"], writes=["C8"])
    s.op("act", lambda e: e.activation(out=C8[:], in_=C8[:], func=AF.Ln, bias=1.0), reads=["C8"], writes=["C8"])
    s.op("dve", lambda e: e.tensor_scalar_mul(out=C8[:], in0=C8[:], scalar1=-8.0), reads=["C8"], writes=["C8"])

    ntile = NTOK // TT
    tpb = SEQ // TT
    evs = []
    s.dma("sp", [(H[0][:], hT[:, 0:TT].rearrange("(kc p) t -> p kc t", p=128))], "h0", writes=["H0"])
    for it in range(ntile):
        Hc, Hn = H[it % 2], f"H{it % 2}"
        if it + 1 < ntile:
            nx = (it + 1) % 2
            s.dma("sp", [(H[nx][:], hT[:, (it + 1) * TT:(it + 2) * TT].rearrange("(kc p) t -> p kc t", p=128))], f"h{nx}",
                  writes=[f"H{nx}"])
        first = (it % tpb == 0)
        HSc, HSn = HS[it % 2], f"HS{it % 2}"
        HSp = HS[(it + 1) % 2]
        HSpn = f"HS{(it + 1) % 2}"
        O, On = OUT[it % 2], f"OUT{it % 2}"
        s.op("act", lambda e, Hc=Hc: e.activation(out=SQ[:], in_=Hc[:], func=AF.Square), reads=[Hn], writes=["SQ"])
        b = bank()
        for kc in range(KC):
            s.op("pe", lambda e, kc=kc, b=b: e.matmul(PB[:, b, :], lhsT=ONES[:], rhs=SQ[:, kc, :], start=(kc == 0), stop=(kc == KC - 1)),
                 reads=["ONES", "SQ"], writes=[("PB", b)])
        s.op("act", lambda e, b=b: e.activation(out=RSTD[:], in_=PB[:, b, :], func=AF.Sqrt, bias=EPS, scale=1.0 / D),
             reads=[("PB", b)], writes=["RSTD"])
        s.op("dve", lambda e: e.reciprocal(out=RSTD[:], in_=RSTD[:]), reads=["RSTD"], writes=["RSTD"])
        for kc in range(KC):
            s.op("dve", lambda e, kc=kc, Hc=Hc: e.scalar_tensor_tensor(out=UT[:, kc, :], in0=Hc[:, kc, :], scalar=G[:, kc:kc + 1],
                                                                    in1=RSTD[:], op0=ALU.mult, op1=ALU.mult),
                 reads=[Hn, "G", "RSTD"], writes=[("UT", kc)])
        by = [bank(), bank()]
        bx = [bank(), bank()]
        for c in range(2):
            for kc in range(KC):
                s.op("pe", lambda e, kc=kc, c=c: e.matmul(PB[:, by[c], :], lhsT=WY[:, kc, c * 128:(c + 1) * 128], rhs=UT[:, kc, :],
                                                       start=(kc == 0), stop=(kc == KC - 1)),
                     reads=["WY", ("UT", kc)], writes=[("PB", by[c])])
            for kc in range(KC):
                s.op("pe", lambda e, kc=kc, c=c: e.matmul(PB[:, bx[c], :], lhsT=WX[:, kc, c * 128:(c + 1) * 128], rhs=UT[:, kc, :],
                                                       start=(kc == 0), stop=(kc == KC - 1)),
                     reads=["WX", ("UT", kc)], writes=[("PB", bx[c])])
        if first:
            s.op("pool", lambda e: e.memset(XR[:, :, 0:3], 0.0), writes=["XR"])
        for c in range(2):
            s.op("act", lambda e, c=c: e.activation(out=YF[:, c, :], in_=PB[:, by[c], :], func=AF.Copy),
                 reads=[("PB", by[c])], writes=[("YF", c)])
            s.op("act", lambda e, c=c: e.activation(out=XR[:, c, 3:TT + 3], in_=PB[:, bx[c], :], func=AF.Copy),
                 reads=[("PB", bx[c])], writes=["XR"])
        s.op("dve", lambda e: e.tensor_tensor(out=T1[:], in0=YF[:], in1=YF[:], op=ALU.mult), reads=["YF"], writes=["T1"])
        s.op("dve", lambda e: e.tensor_scalar(out=T1[:], in0=T1[:], scalar1=0.044715, scalar2=1.0, op0=ALU.mult, op1=ALU.add),
             reads=["T1"], writes=["T1"])
        s.op("dve", lambda e: e.tensor_tensor(out=T1[:], in0=T1[:], in1=YF[:], op=ALU.mult), reads=["T1", "YF"], writes=["T1"])
        s.op("act", lambda e: e.activation(out=T1[:], in_=T1[:], func=AF.Sigmoid, scale=1.5957691216), reads=["T1"], writes=["T1"])
        s.op("dve", lambda e: e.tensor_tensor(out=GY[:], in0=T1[:], in1=YF[:], op=ALU.mult), reads=["T1", "YF"], writes=["GY"])
        for c in range(2):
            s.op("dve", lambda e, c=c: e.tensor_scalar(out=XC[:, c, :], in0=XR[:, c, 3:TT + 3], scalar1=CV[:, c, 3:4], scalar2=CV[:, c, 4:5],
                                                      op0=ALU.mult, op1=ALU.add), reads=["XR", "CV"], writes=[("XC", c)])
            for k in range(3):
                s.op("dve", lambda e, c=c, k=k: e.scalar_tensor_tensor(out=XC[:, c, :], in0=XR[:, c, k:TT + k], scalar=CV[:, c, k:k + 1],
                                                                      in1=XC[:, c, :], op0=ALU.mult, op1=ALU.add),
                     reads=["XR", "CV", ("XC", c)], writes=[("XC", c)])
        s.op("act", lambda e: e.activation(out=XCB[:], in_=XC[:], func=AF.Copy), reads=["XC"], writes=["XCB"])
        s.op("pool", lambda e: e.tensor_copy(out=XR[:, :, 0:3], in_=XR[:, :, TT:TT + 3]), reads=["XR"], writes=["XR"])
        br = [bank(), bank()]
        bi = [bank(), bank()]
        for c in range(2):
            for kc in range(2):
                s.op("pe", lambda e, kc=kc, c=c: e.matmul(PB[:, br[c], :], lhsT=WR[:, kc, c * 128:(c + 1) * 128], rhs=XCB[:, kc, :],
                                                       start=(kc == 0), stop=(kc == 1)), reads=["WR", "XCB"], writes=[("PB", br[c])])
            for kc in range(2):
                s.op("pe", lambda e, kc=kc, c=c: e.matmul(PB[:, bi[c], :], lhsT=WI[:, kc, c * 128:(c + 1) * 128], rhs=XCB[:, kc, :],
                                                       start=(kc == 0), stop=(kc == 1)), reads=["WI", "XCB"], writes=[("PB", bi[c])])
        for c in range(2):
            s.op("act", lambda e, c=c: e.activation(out=RG[:, c, :], in_=PB[:, br[c], :], func=AF.Sigmoid, bias=CV[:, c, 5:6]),
                 reads=[("PB", br[c]), "CV"], writes=[("RG", c)])
            s.op("act", lambda e, c=c: e.activation(out=IG[:, c, :], in_=PB[:, bi[c], :], func=AF.Sigmoid, bias=CV[:, c, 6:7]),
                 reads=[("PB", bi[c]), "CV"], writes=[("IG", c)])
        for c in range(2):
            s.op("act", lambda e, c=c: e.activation(out=AA[:, c, :], in_=RG[:, c, :], func=AF.Exp, scale=C8[:, c:c + 1]),
                 reads=[("RG", c), "C8"], writes=[("AA", c)])
        s.op("dve", lambda e: e.tensor_tensor(out=MM[:], in0=AA[:], in1=AA[:], op=ALU.mult), reads=["AA"], writes=["MM"])
        s.op("dve", lambda e: e.tensor_scalar(out=MM[:], in0=MM[:], scalar1=-1.0, scalar2=1.0, op0=ALU.mult, op1=ALU.add),
             reads=["MM"], writes=["MM"])
        s.op("dve", lambda e: e.tensor_scalar_max(out=MM[:], in0=MM[:], scalar1=0.0), reads=["MM"], writes=["MM"])
        s.op("act", lambda e: e.activation(out=MM[:], in_=MM[:], func=AF.Sqrt), reads=["MM"], writes=["MM"])
        if first:
            s.op("dve", lambda e: e.memset(MM[:, :, 0:1], 1.0), reads=["MM"], writes=["MM"])
        s.op("dve", lambda e: e.tensor_tensor(out=BB[:], in0=IG[:], in1=XC[:], op=ALU.mult), reads=["IG", "XC"], writes=["BB"])
        s.op("dve", lambda e: e.tensor_tensor(out=BB[:], in0=BB[:], in1=MM[:], op=ALU.mult), reads=["BB", "MM"], writes=["BB"])
        for c in range(2):
            init = 0.0 if first else HSp[:, c, TT - 1:TT]
            s.op("dve", lambda e, c=c, init=init, HSc=HSc: e.tensor_tensor_scan(out=HSc[:, c, :], data0=AA[:, c, :], data1=BB[:, c, :],
                                                                              initial=init, op0=ALU.mult, op1=ALU.add),
                 reads=["AA", "BB", HSpn], writes=[(HSn, c)])
        s.op("dve", lambda e, O=O, HSc=HSc: e.tensor_tensor(out=O[:], in0=HSc[:], in1=GY[:], op=ALU.mult), reads=[HSn, "GY"], writes=[On])
        evs.append(s.dma("sp", [(oT[:, it * TT:(it + 1) * TT].rearrange("(c p) t -> p c t", p=128), O[:])], "st", reads=[On]))
    s.finish("sp", evs[-1:])
    s.emit()
    return nc


_NC_CACHE = {}


def _get(name, fn):
    if name not in _NC_CACHE:
        _NC_CACHE[name] = fn()
    return _NC_CACHE[name]


def _split_cols(hc):
    aq = np.arange(hc * 128, (hc + 1) * 128)
    base = 4096
    return {"aq": aq, "af": 1024 + aq, "ai": 2048 + aq, "ag": 3072 + aq,
            "bq": base + aq, "bk": base + 1024 + aq, "bv": base + 2048 + aq, "bz": base + 3072 + aq,
            "ba": np.array([base + 4096 + hc]), "bb": np.array([base + 4096 + 8 + hc])}


def _f32(a):
    return np.ascontiguousarray(np.asarray(a, dtype=np.float32))


def kernel(x, p, ln_mix, ln_ffn, ln_ple, ln_final, lb_table, ab_w_in, ab_conv, b_a_log, b_dt_bias, a_gnorm, b_gnorm,
           ab_w_out, c_w_in, c_conv_w, c_conv_b, c_w_r, c_b_r, c_w_i, c_b_i, c_lambda, c_w_out, ffn_w_gate, ffn_w_up,
           ffn_w_down, moe_router, moe_w_gate, moe_w_up, moe_w_down, ple_w_proj, ple_w_gate):
    NCORE = 8
    x = np.asarray(x, np.float32)
    B, S, _ = x.shape
    T = B * S
    NT = T // NCORE
    cores = list(range(NCORE))
    xT = np.ascontiguousarray(x.reshape(T, D).T)
    p = np.asarray(p, np.float32)
    pT = [np.ascontiguousarray(p[l].reshape(T, PLE).T) for l in range(2)]

    w_in = np.asarray(ab_w_in[0], np.float32)
    conv = np.asarray(ab_conv[0], np.float32)
    consts1 = k1_consts()
    gn = _f32(np.broadcast_to(np.stack([np.asarray(a_gnorm[0]), np.asarray(b_gnorm[0])], 0)[None], (64, 2, 128)))
    g_mix0 = vec_layout(ln_mix[0])
    maps = []
    for hc in cores:
        c = _split_cols(hc)
        hs = slice(hc * 128, (hc + 1) * 128)
        cw = np.stack([conv[:, hs].T, conv[:, 1024 + hc * 128:1024 + (hc + 1) * 128].T,
                       conv[:, 2048 + hc * 128:2048 + (hc + 1) * 128].T], 1)
        m = {"xT": xT, "g_mix": g_mix0,
             "w_fm": _f32(w_in[:, np.concatenate([c["aq"], c["af"], c["bq"], c["bk"], c["bv"]])]),
             "w_tok": _f32(w_in[:, np.concatenate([c["ai"], c["ag"], c["bz"]])]),
             "w_ab": _f32(w_in[:, np.concatenate([c["ba"], c["bb"]])]),
             "lbt": _f32(np.asarray(lb_table)[:, hs].T), "convw": _f32(cw),
             "sc2": np.array([[np.asarray(b_a_log)[0, hc], np.asarray(b_dt_bias)[0, hc]]], np.float32), "gn": gn}
        m.update(consts1)
        maps.append(m)
    nc1 = _get(("k1", T, S), lambda: build_k1(T, S))
    r1 = run_bass_kernel_spmd(nc1, maps, core_ids=cores).results
    mixed = np.empty((T, D), np.float32)
    for hc in cores:
        mixed[:, hc * 128:(hc + 1) * 128] = r1[hc]["o"][:, 0:128]
        mixed[:, 1024 + hc * 128:1024 + (hc + 1) * 128] = r1[hc]["o"][:, 128:256]
    mT = np.ascontiguousarray(mixed.T)
    del mixed, r1

    shared2 = {"w_out": _f32(ab_w_out[0]), "wg": _f32(ffn_w_gate[0]), "wu": _f32(ffn_w_up[0]), "wd": _f32(ffn_w_down[0]),
               "wpg": _f32(ple_w_gate[0]), "wpp": _f32(ple_w_proj[0]),
               "g_ffn": vec_layout(ln_ffn[0]), "g_ple": vec_layout(ln_ple[0])}
    maps = []
    for c in cores:
        sl = slice(c * NT, (c + 1) * NT)
        m = {"hT": _f32(xT[:, sl]), "mT": _f32(mT[:, sl]), "pT": _f32(pT[0][:, sl])}
        m.update(shared2)
        maps.append(m)
    nc2 = _get(("k2", NT), lambda: build_k2(NT))
    r2 = run_bass_kernel_spmd(nc2, maps, core_ids=cores).results
    h1T = np.ascontiguousarray(np.concatenate([r2[c]["oT"] for c in cores], axis=1))
    del r2, mT, maps

    cw_in = np.asarray(c_w_in[0], np.float32)
    g_mix1 = vec_layout(ln_mix[1])
    maps = []
    for c in cores:
        sl = slice(c * 256, (c + 1) * 256)
        cv = np.zeros((128, 2, 8), np.float32)

        def pc(v):
            return np.asarray(v, np.float32)[sl].reshape(2, 128).T
        for k in range(4):
            cv[:, :, k] = pc(np.asarray(c_conv_w[0])[k])
        cv[:, :, 4] = pc(c_conv_b[0]); cv[:, :, 5] = pc(c_b_r[0]); cv[:, :, 6] = pc(c_b_i[0]); cv[:, :, 7] = pc(c_lambda[0])
        maps.append({"hT": h1T, "g_mix": g_mix1, "w_y": _f32(cw_in[:, sl]), "w_x": _f32(cw_in[:, D + c * 256:D + (c + 1) * 256]),
                     "w_r": _f32(np.asarray(c_w_r[0])[c]), "w_i": _f32(np.asarray(c_w_i[0])[c]), "cvec": cv})
    nc3 = _get(("k3", T, S), lambda: build_k3(T, S))
    r3 = run_bass_kernel_spmd(nc3, maps, core_ids=cores).results
    gT = np.ascontiguousarray(np.concatenate([r3[c]["oT"] for c in cores], axis=0))
    del r3, maps

    shared4 = {"w_out": _f32(c_w_out[0]), "wg": _f32(moe_w_gate[0]), "wu": _f32(moe_w_up[0]), "wd": _f32(moe_w_down[0]),
               "wr": _f32(np.asarray(moe_router[0], np.float32).reshape(16, 128, 8).transpose(1, 0, 2)),
               "wpg": _f32(ple_w_gate[1]), "wpp": _f32(ple_w_proj[1]),
               "g_ffn": vec_layout(ln_ffn[1]), "g_ple": vec_layout(ln_ple[1]), "g_fin": vec_layout(ln_final)}
    shared4.update(k4_consts())
    maps = []
    for c in cores:
        sl = slice(c * NT, (c + 1) * NT)
        m = {"hT": _f32(h1T[:, sl]), "mT": _f32(gT[:, sl]), "pT": _f32(pT[1][:, sl])}
        m.update(shared4)
        maps.append(m)
    nc4 = _get(("k4", NT), lambda: build_k4(NT))
    r4 = run_bass_kernel_spmd(nc4, maps, core_ids=cores).results
    outT = np.concatenate([r4[c]["oT"] for c in cores], axis=1)
    return np.ascontiguousarray(outT.T).reshape(B, S, D)
```

```python
import contextlib
import numpy as np
import concourse.bass as bass
import concourse.mybir as mybir
from concourse.bass_utils import run_bass_kernel_spmd

F32 = mybir.dt.float32
BF16 = mybir.dt.bfloat16
ALU = mybir.AluOpType
AF = mybir.ActivationFunctionType
AX = mybir.AxisListType


class Sched:
    ENG = ("pe", "act", "dve", "pool", "sp")

    def __init__(self, nc, selfsync=True):
        self.nc = nc
        self.stack = contextlib.ExitStack()
        self.ops = {e: [] for e in self.ENG}
        self.sems = {}
        self.cnt = {}
        self.waited = {e: {} for e in self.ENG}
        self.state = {}
        self.selfsync = selfsync
        self.nwaits = 0
        for e in ("pe", "act", "dve", "pool"):
            self._sem("e_" + e)

    def _sem(self, name):
        if name not in self.sems:
            self.sems[name] = self.stack.enter_context(self.nc.semaphore(name))
            self.cnt[name] = 0
        return self.sems[name]

    def sbuf(self, name, shape, dtype):
        return self.stack.enter_context(self.nc.sbuf_tensor(name, list(shape), dtype))

    def psum(self, name, shape, dtype=F32):
        return self.stack.enter_context(self.nc.psum_tensor(name, list(shape), dtype))

    def _st(self, t, r):
        d = self.state.setdefault(t, {})
        if r not in d:
            d[r] = [None, {}]
        return d[r]

    def _overl(self, t, r):
        d = self.state.get(t, {})
        if r is None:
            return list(d.values())
        return [d[k] for k in (r, None) if k in d]

    def _deps(self, reads, writes):
        ev = {}

        def add(e):
            if e is not None and ev.get(e[0], 0) < e[1]:
                ev[e[0]] = e[1]
        for (t, r) in reads:
            for st in self._overl(t, r):
                add(st[0])
        for (t, r) in writes:
            for st in self._overl(t, r):
                add(st[0])
                for s, v in st[1].items():
                    add((s, v))
        return ev

    def _commit(self, reads, writes, e):
        for (t, r) in reads:
            st = self._st(t, r)
            if st[1].get(e[0], 0) < e[1]:
                st[1][e[0]] = e[1]
        for (t, r) in writes:
            if r is None:
                self.state[t] = {None: [e, {}]}
            else:
                st = self._st(t, r)
                st[0] = e
                st[1] = {}

    def _waits(self, eng, ev):
        w = []
        for s, v in ev.items():
            if s == "e_" + eng and (eng == "pe" or not self.selfsync):
                continue
            if self.waited[eng].get(s, 0) < v:
                self.waited[eng][s] = v
                w.append((s, v))
        self.nwaits += len(w)
        return w

    @staticmethod
    def _norm(keys):
        out = []
        for k in keys:
            if isinstance(k, tuple):
                if k[0].startswith("PS"):
                    out.append((k[0], None))
                else:
                    out.append((k[0], k[1]))
            else:
                out.append((k, None))
        return out

    def op(self, eng, fn, reads=(), writes=()):
        reads, writes = self._norm(reads), self._norm(writes)
        w = self._waits(eng, self._deps(reads, writes))
        s = "e_" + eng
        self.cnt[s] += 1
        e = (s, self.cnt[s])
        self.ops[eng].append((w, fn, [(s, 1)]))
        self._commit(reads, writes, e)
        return e

    def dma(self, q, pairs, chan, reads=(), writes=()):
        reads, writes = self._norm(reads), self._norm(writes)
        w = self._waits(q, self._deps(reads, writes))
        s = "d_" + chan
        self._sem(s)
        for i, (o, a) in enumerate(pairs):
            self.cnt[s] += 16
            self.ops[q].append((w if i == 0 else [], ("dma", o, a), [(s, 16)]))
        e = (s, self.cnt[s])
        self._commit(reads, writes, e)
        return e

    def finish(self, eng, events):
        ev = {}
        for (s, v) in events:
            ev[s] = max(ev.get(s, 0), v)
        self.ops[eng].append((list(ev.items()), None, []))

    def emit(self):
        nc = self.nc
        sems = self.sems

        def run(eng, lst):
            for (w, fn, incs) in lst:
                for (s, v) in w:
                    eng.wait_ge(sems[s], v)
                if fn is None:
                    continue
                if isinstance(fn, tuple):
                    ins = eng.dma_start(out=fn[1], in_=fn[2])
                else:
                    ins = fn(eng)
                for (s, n) in incs:
                    ins.then_inc(sems[s], n)
        with nc.Block() as block:
            @block.tensor
            def _(e):
                run(e, self.ops["pe"])

            @block.scalar
            def _(e):
                run(e, self.ops["act"])

            @block.vector
            def _(e):
                run(e, self.ops["dve"])

            @block.gpsimd
            def _(e):
                run(e, self.ops["pool"])

            @block.sync
            def _(e):
                run(e, self.ops["sp"])
        self.stack.close()


D = 2048
KC = 16
TT = 512
FF = 5632
FC = FF // 128
PLE = 256
EPS = 1e-6


class DenseCore:
    def __init__(self, s):
        self.s = s
        s_ = s
        self.H = s_.sbuf("H", [128, KC, TT], F32)
        self.MT = s_.sbuf("MT", [128, KC, TT], BF16)
        self.UT = s_.sbuf("UT", [128, KC, TT], BF16)
        self.HT = s_.sbuf("HT", [128, FC, TT], BF16)
        self.PT = s_.sbuf("PT", [128, 2, TT], BF16)
        self.WB = [s_.sbuf(f"WB{i}", [128, KC, 256], BF16) for i in range(4)]
        self.WD = [s_.sbuf(f"WD{i}", [128, 11, 512], BF16) for i in range(2)]
        self.WP = [s_.sbuf(f"WP{i}", [128, 2, 256], BF16) for i in range(2)]
        self.RSTD = s_.sbuf("RSTD", [128, TT], F32)
        self.SG = [s_.sbuf(f"SG{i}", [128, TT], F32) for i in range(2)]
        self.TMP = [s_.sbuf(f"TMP{i}", [128, TT], F32) for i in range(2)]
        self.ONES = s_.sbuf("ONES", [128, 128], BF16)
        self.PB = s_.psum("PB", [128, 8, TT])
        s_.op("pool", lambda e: e.memset(self.ONES[:], 1.0), writes=["ONES"])
        self.wb_i = 0
        self.wd_i = 0
        self.wp_i = 0
        self.pb_i = 0
        self.sg_i = 0

    def bank(self):
        b = self.pb_i % 8
        self.pb_i += 1
        return b

    def load_vec(self, name, ap):
        t = self.s.sbuf(name, list(ap.shape), F32)
        self.s.dma("sp", [(t[:], ap)], "c_" + name, writes=[name])
        return t

    def load_tile(self, hT_ap, tok):
        self.s.dma("sp", [(self.H[:], hT_ap[:, tok].rearrange("(kc p) t -> p kc t", p=128))], "h", writes=["H"])

    def load_bf(self, dst, name, src_ap, tok, chan):
        self.s.dma("pool", [(dst[:], src_ap[:, tok].rearrange("(kc p) t -> p kc t", p=128))], chan, writes=[name])

    def store_tile(self, out_ap, tok):
        return self.s.dma("sp", [(out_ap[:, tok].rearrange("(kc p) t -> p kc t", p=128), self.H[:])], "st", reads=["H"])

    def rmsnorm(self, Gname, G):
        s = self.s
        H, SQ, UT, RSTD, ONES, PB = self.H, self.MT, self.UT, self.RSTD, self.ONES, self.PB
        s.op("act", lambda e: e.activation(out=SQ[:], in_=H[:], func=AF.Square), reads=["H"], writes=["MT"])
        b = self.bank()
        for kc in range(KC):
            s.op("pe", lambda e, kc=kc: e.matmul(PB[:, b, :], lhsT=ONES[:], rhs=SQ[:, kc, :], start=(kc == 0), stop=(kc == KC - 1)),
                 reads=["ONES", "MT"], writes=[("PB", b)])
        s.op("act", lambda e: e.activation(out=RSTD[:], in_=PB[:, b, :], func=AF.Sqrt, bias=EPS, scale=1.0 / D),
             reads=[("PB", b)], writes=["RSTD"])
        s.op("dve", lambda e: e.reciprocal(out=RSTD[:], in_=RSTD[:]), reads=["RSTD"], writes=["RSTD"])
        for kc in range(KC):
            s.op("dve", lambda e, kc=kc: e.scalar_tensor_tensor(out=UT[:, kc, :], in0=H[:, kc, :], scalar=G[:, kc:kc + 1],
                                                              in1=RSTD[:], op0=ALU.mult, op1=ALU.mult),
                 reads=[("H", kc), Gname, "RSTD"], writes=[("UT", kc)])

    def load_wb(self, w_ap, cb, chan):
        i = self.wb_i % 4
        self.wb_i += 1
        self.s.dma("pool", [(self.WB[i][:], w_ap[:, cb * 256:(cb + 1) * 256].rearrange("(kc p) c -> p kc c", p=128))],
                   f"wb{i}", writes=[f"WB{i}"])
        return i

    def mm2048(self, bank, wi, j, rhs, rhsname):
        s = self.s
        W, PB = self.WB[wi], self.PB
        for kc in range(KC):
            s.op("pe", lambda e, kc=kc: e.matmul(PB[:, bank, :], lhsT=W[:, kc, j * 128:(j + 1) * 128], rhs=rhs[:, kc, :],
                                               start=(kc == 0), stop=(kc == KC - 1)),
                 reads=[f"WB{wi}", (rhsname, kc)], writes=[("PB", bank)])

    def proj_add(self, w_ap, rhs, rhsname):
        s = self.s
        H, PB = self.H, self.PB
        for cb in range(D // 256):
            wi = self.load_wb(w_ap, cb, "w")
            for j in range(2):
                dc = cb * 2 + j
                b = self.bank()
                self.mm2048(b, wi, j, rhs, rhsname)
                s.op("dve", lambda e, dc=dc, b=b: e.tensor_tensor(out=H[:, dc, :], in0=H[:, dc, :], in1=PB[:, b, :], op=ALU.add),
                     reads=[("H", dc), ("PB", b)], writes=[("H", dc)])

    def ffn(self, wg_ap, wu_ap, wd_ap, gate_bc=None, gate_name=None):
        s = self.s
        H, PB, HT, UT = self.H, self.PB, self.HT, self.UT
        for cb in range(FF // 256):
            wg = self.load_wb(wg_ap, cb, "w")
            wu = self.load_wb(wu_ap, cb, "w")
            for j in range(2):
                fc = cb * 2 + j
                ba, bb = self.bank(), self.bank()
                self.mm2048(ba, wg, j, UT, "UT")
                self.mm2048(bb, wu, j, UT, "UT")
                sg = self.SG[self.sg_i % 2]
                sgn = f"SG{self.sg_i % 2}"
                self.sg_i += 1
                s.op("act", lambda e, sg=sg, ba=ba: e.activation(out=sg[:], in_=PB[:, ba, :], func=AF.Silu),
                     reads=[("PB", ba)], writes=[sgn])
                s.op("dve", lambda e, sg=sg, bb=bb, fc=fc: e.tensor_tensor(out=HT[:, fc, :], in0=sg[:], in1=PB[:, bb, :], op=ALU.mult),
                     reads=[sgn, ("PB", bb)], writes=[("HT", fc)])
        for cb in range(D // 512):
            banks = [self.bank() for _ in range(4)]
            for fg in range(FC // 11):
                i = self.wd_i % 2
                self.wd_i += 1
                WD = self.WD[i]
                s.dma("pool", [(WD[:], wd_ap[fg * 11 * 128:(fg + 1) * 11 * 128, cb * 512:(cb + 1) * 512]
                                .rearrange("(fc p) c -> p fc c", p=128))], f"wd{i}", writes=[f"WD{i}"])
                for jf in range(11):
                    fc = fg * 11 + jf
                    for d4 in range(4):
                        s.op("pe", lambda e, WD=WD, jf=jf, d4=d4, fc=fc, b=banks[d4]: e.matmul(
                            PB[:, b, :], lhsT=WD[:, jf, d4 * 128:(d4 + 1) * 128], rhs=HT[:, fc, :],
                            start=(fc == 0), stop=(fc == FC - 1)),
                            reads=[f"WD{i}", ("HT", fc)], writes=[("PB", banks[d4])])
            for d4 in range(4):
                dc = cb * 4 + d4
                b = banks[d4]
                if gate_bc is None:
                    s.op("dve", lambda e, dc=dc, b=b: e.tensor_tensor(out=H[:, dc, :], in0=H[:, dc, :], in1=PB[:, b, :], op=ALU.add),
                         reads=[("H", dc), ("PB", b)], writes=[("H", dc)])
                else:
                    tmp = self.TMP[d4 % 2]
                    tn = f"TMP{d4 % 2}"
                    s.op("dve", lambda e, tmp=tmp, b=b: e.tensor_tensor(out=tmp[:], in0=PB[:, b, :], in1=gate_bc, op=ALU.mult),
                         reads=[("PB", b), gate_name], writes=[tn])
                    s.op("dve", lambda e, tmp=tmp, dc=dc: e.tensor_tensor(out=H[:, dc, :], in0=H[:, dc, :], in1=tmp[:], op=ALU.add),
                         reads=[("H", dc), tn], writes=[("H", dc)])

    def ple(self, wpg_ap, wpp_ap):
        s = self.s
        H, PB, UT, PT = self.H, self.PB, self.UT, self.PT
        for cb in range(D // 256):
            wi = self.load_wb(wpg_ap, cb, "w")
            ip = self.wp_i % 2
            self.wp_i += 1
            WP = self.WP[ip]
            s.dma("pool", [(WP[:], wpp_ap[:, cb * 256:(cb + 1) * 256].rearrange("(kc p) c -> p kc c", p=128))],
                  f"wp{ip}", writes=[f"WP{ip}"])
            for j in range(2):
                dc = cb * 2 + j
                ba, bb = self.bank(), self.bank()
                self.mm2048(ba, wi, j, UT, "UT")
                for kc in range(2):
                    s.op("pe", lambda e, kc=kc, WP=WP, j=j, bb=bb: e.matmul(PB[:, bb, :], lhsT=WP[:, kc, j * 128:(j + 1) * 128],
                                                                         rhs=PT[:, kc, :], start=(kc == 0), stop=(kc == 1)),
                         reads=[f"WP{ip}", "PT"], writes=[("PB", bb)])
                sg = self.SG[self.sg_i % 2]
                sgn = f"SG{self.sg_i % 2}"
                self.sg_i += 1
                s.op("act", lambda e, sg=sg, ba=ba: e.activation(out=sg[:], in_=PB[:, ba, :], func=AF.Sigmoid),
                     reads=[("PB", ba)], writes=[sgn])
                tmp = self.TMP[j]
                tn = f"TMP{j}"
                s.op("dve", lambda e, tmp=tmp, sg=sg, bb=bb: e.tensor_tensor(out=tmp[:], in0=sg[:], in1=PB[:, bb, :], op=ALU.mult),
                     reads=[sgn, ("PB", bb)], writes=[tn])
                s.op("dve", lambda e, tmp=tmp, dc=dc: e.tensor_tensor(out=H[:, dc, :], in0=H[:, dc, :], in1=tmp[:], op=ALU.add),
                     reads=[("H", dc), tn], writes=[("H", dc)])


def vec_layout(v):
    return np.ascontiguousarray(np.asarray(v, np.float32).reshape(-1, 128).T)


def build_k2(NT):
    nc = bass.Bass("TRN2", target_bir_lowering=False)
    dt = lambda n, sh, kind="ExternalInput": nc.dram_tensor(n, sh, F32, kind=kind).ap()
    hT = dt("hT", [D, NT]); mT = dt("mT", [D, NT]); pT = dt("pT", [PLE, NT])
    w_out = dt("w_out", [D, D]); wg = dt("wg", [D, FF]); wu = dt("wu", [D, FF]); wd = dt("wd", [FF, D])
    wpg = dt("wpg", [D, D]); wpp = dt("wpp", [PLE, D])
    g_ffn = dt("g_ffn", [128, KC]); g_ple = dt("g_ple", [128, KC])
    oT = dt("oT", [D, NT], "ExternalOutput")
    s = Sched(nc)
    c = DenseCore(s)
    Gf = c.load_vec("Gf", g_ffn)
    Gp = c.load_vec("Gp", g_ple)
    evs = []
    for it in range(NT // TT):
        tok = slice(it * TT, (it + 1) * TT)
        c.load_tile(hT, tok)
        c.load_bf(c.MT, "MT", mT, tok, "m")
        c.load_bf(c.PT, "PT", pT, tok, "p")
        c.proj_add(w_out, c.MT, "MT")
        c.rmsnorm("Gf", Gf)
        c.ffn(wg, wu, wd)
        c.rmsnorm("Gp", Gp)
        c.ple(wpg, wpp)
        evs.append(c.store_tile(oT, tok))
    s.finish("sp", evs[-1:])
    s.emit()
    return nc


class MoECore(DenseCore):
    def __init__(self, s, ident_ap, sel_ap, wr_ap):
        super().__init__(s)
        sb = s.sbuf
        self.IDF = sb("IDF", [128, 128], F32)
        self.SEL = sb("SEL", [8, 8, 128], F32)
        self.WR32 = sb("WR32", [128, KC, 8], F32)
        self.WRH = sb("WRH", [128, KC, 8], BF16)
        self.WRL = sb("WRL", [128, KC, 8], BF16)
        self.LG = sb("LG", [128, 4, 8], F32)
        self.M8 = sb("M8", [128, 4, 8], F32)
        self.GT = sb("GT", [128, 4, 8], F32)
        self.SM = sb("SM", [128, 4, 4], F32)
        self.GTT = sb("GTT", [8, TT], F32)
        self.GB = [sb(f"GB{i}", [128, TT], F32) for i in range(2)]
        s.dma("sp", [(self.IDF[:], ident_ap)], "c_id", writes=["IDF"])
        s.dma("sp", [(self.SEL[:], sel_ap)], "c_sel", writes=["SEL"])
        s.dma("sp", [(self.WR32[:], wr_ap)], "c_wr", writes=["WR32"])
        s.op("act", lambda e: e.activation(out=self.WRH[:], in_=self.WR32[:], func=AF.Copy), reads=["WR32"], writes=["WRH"])
        s.op("dve", lambda e: e.tensor_tensor(out=self.WRL[:], in0=self.WR32[:], in1=self.WRH[:], op=ALU.subtract),
             reads=["WR32", "WRH"], writes=["WRL"])
        self.gb_i = 0

    def rmsnorm_hilo(self, Gname, G):
        s = self.s
        H, SQ, UT, RSTD, ONES, PB, HT = self.H, self.MT, self.UT, self.RSTD, self.ONES, self.PB, self.HT
        s.op("act", lambda e: e.activation(out=SQ[:], in_=H[:], func=AF.Square), reads=["H"], writes=["MT"])
        b = self.bank()
        for kc in range(KC):
            s.op("pe", lambda e, kc=kc: e.matmul(PB[:, b, :], lhsT=ONES[:], rhs=SQ[:, kc, :], start=(kc == 0), stop=(kc == KC - 1)),
                 reads=["ONES", "MT"], writes=[("PB", b)])
        s.op("act", lambda e: e.activation(out=RSTD[:], in_=PB[:, b, :], func=AF.Sqrt, bias=EPS, scale=1.0 / D),
             reads=[("PB", b)], writes=["RSTD"])
        s.op("dve", lambda e: e.reciprocal(out=RSTD[:], in_=RSTD[:]), reads=["RSTD"], writes=["RSTD"])
        for kc in range(KC):
            tmp = self.TMP[kc % 2]
            tn = f"TMP{kc % 2}"
            s.op("dve", lambda e, kc=kc, tmp=tmp: e.scalar_tensor_tensor(out=tmp[:], in0=H[:, kc, :], scalar=G[:, kc:kc + 1],
                                                                       in1=RSTD[:], op0=ALU.mult, op1=ALU.mult),
                 reads=[("H", kc), Gname, "RSTD"], writes=[tn])
            s.op("act", lambda e, kc=kc, tmp=tmp: e.activation(out=UT[:, kc, :], in_=tmp[:], func=AF.Copy), reads=[tn], writes=[("UT", kc)])
            s.op("dve", lambda e, kc=kc, tmp=tmp: e.tensor_tensor(out=HT[:, kc, :], in0=tmp[:], in1=UT[:, kc, :], op=ALU.subtract),
                 reads=[tn, ("UT", kc)], writes=[("HT", kc)])

    def router(self):
        s = self.s
        UT, HT, PB, LG, M8, GT, SM, GTT, IDF = self.UT, self.HT, self.PB, self.LG, self.M8, self.GT, self.SM, self.GTT, self.IDF
        WRH, WRL = self.WRH, self.WRL
        b = self.bank()
        for q in range(4):
            ts_ = slice(q * 128, (q + 1) * 128)
            n = 0
            for (a, an, w, wn) in ((UT, "UT", WRH, "WRH"), (HT, "HT", WRH, "WRH"), (UT, "UT", WRL, "WRL")):
                for kc in range(KC):
                    s.op("pe", lambda e, a=a, w=w, kc=kc, ts_=ts_, q=q, n=n: e.matmul(
                        PB[:, b, q * 8:(q + 1) * 8], lhsT=a[:, kc, ts_], rhs=w[:, kc, :], start=(n == 0), stop=(n == 3 * KC - 1)),
                        reads=[(an, kc), wn], writes=[("PB", b)])
                    n += 1
        s.op("act", lambda e: e.activation(out=LG[:], in_=PB[:, b, 0:32].rearrange("p (q e) -> p q e", q=4), func=AF.Copy),
             reads=[("PB", b)], writes=["LG"])
        for q in range(4):
            s.op("dve", lambda e, q=q: e.max(out=M8[:, q, :], in_=LG[:, q, :]), reads=["LG"], writes=[("M8", q)])
            s.op("dve", lambda e, q=q: e.tensor_scalar(out=GT[:, q, :], in0=LG[:, q, :], scalar1=M8[:, q, 1:2], scalar2=None, op0=ALU.is_ge),
                 reads=["LG", ("M8", q)], writes=[("GT", q)])
            s.op("dve", lambda e, q=q: e.tensor_scalar_mul(out=SM[:, q, 0:1], in0=M8[:, q, 0:1], scalar1=-1.0), reads=[("M8", q)], writes=[("SM", q)])
            s.op("act", lambda e, q=q: e.activation(out=LG[:, q, :], in_=LG[:, q, :], func=AF.Exp, bias=SM[:, q, 0:1]),
                 reads=["LG", ("SM", q)], writes=["LG"])
            s.op("dve", lambda e, q=q: e.tensor_tensor(out=GT[:, q, :], in0=GT[:, q, :], in1=LG[:, q, :], op=ALU.mult),
                 reads=["LG", ("GT", q)], writes=[("GT", q)])
            s.op("dve", lambda e, q=q: e.reduce_sum(out=SM[:, q, 1:2], in_=GT[:, q, :], axis=AX.X), reads=[("GT", q)], writes=[("SM", q)])
            s.op("dve", lambda e, q=q: e.reciprocal(out=SM[:, q, 1:2], in_=SM[:, q, 1:2]), reads=[("SM", q)], writes=[("SM", q)])
            s.op("dve", lambda e, q=q: e.tensor_scalar(out=GT[:, q, :], in0=GT[:, q, :], scalar1=SM[:, q, 1:2], scalar2=None, op0=ALU.mult),
                 reads=[("GT", q), ("SM", q)], writes=[("GT", q)])
        b2 = self.bank()
        for q in range(4):
            s.op("pe", lambda e, q=q: e.transpose(PB[0:8, b2, q * 128:(q + 1) * 128], GT[:, q, :], IDF[:]), reads=[("GT", q), "IDF"],
                 writes=[("PB", b2)])
        s.op("act", lambda e: e.activation(out=GTT[:], in_=PB[0:8, b2, :], func=AF.Copy), reads=[("PB", b2)], writes=["GTT"])

    def gate_bc(self, ex):
        s = self.s
        i = self.gb_i % 2
        self.gb_i += 1
        GB, PB = self.GB[i], self.PB
        b = self.bank()
        s.op("pe", lambda e: e.matmul(PB[:, b, :], lhsT=self.SEL[:, ex, :], rhs=self.GTT[:], start=True, stop=True),
             reads=["SEL", "GTT"], writes=[("PB", b)])
        s.op("act", lambda e: e.activation(out=GB[:], in_=PB[:, b, :], func=AF.Copy), reads=[("PB", b)], writes=[f"GB{i}"])
        return GB[:], f"GB{i}"

    def final_norm(self, Gname, G):
        s = self.s
        H, SQ, RSTD, ONES, PB = self.H, self.MT, self.RSTD, self.ONES, self.PB
        s.op("act", lambda e: e.activation(out=SQ[:], in_=H[:], func=AF.Square), reads=["H"], writes=["MT"])
        b = self.bank()
        for kc in range(KC):
            s.op("pe", lambda e, kc=kc: e.matmul(PB[:, b, :], lhsT=ONES[:], rhs=SQ[:, kc, :], start=(kc == 0), stop=(kc == KC - 1)),
                 reads=["ONES", "MT"], writes=[("PB", b)])
        s.op("act", lambda e: e.activation(out=RSTD[:], in_=PB[:, b, :], func=AF.Sqrt, bias=EPS, scale=1.0 / D),
             reads=[("PB", b)], writes=["RSTD"])
        s.op("dve", lambda e: e.reciprocal(out=RSTD[:], in_=RSTD[:]), reads=["RSTD"], writes=["RSTD"])
        for kc in range(KC):
            s.op("dve", lambda e, kc=kc: e.scalar_tensor_tensor(out=H[:, kc, :], in0=H[:, kc, :], scalar=G[:, kc:kc + 1],
                                                              in1=RSTD[:], op0=ALU.mult, op1=ALU.mult),
                 reads=[("H", kc), Gname, "RSTD"], writes=[("H", kc)])


def k4_consts():
    sel = np.zeros((8, 8, 128), np.float32)
    for e in range(8):
        sel[e, e, :] = 1.0
    return {"ident": np.eye(128, dtype=np.float32), "sel": sel}


def build_k4(NT, NEXP=8):
    nc = bass.Bass("TRN2", target_bir_lowering=False)
    dt = lambda n, sh, kind="ExternalInput": nc.dram_tensor(n, sh, F32, kind=kind).ap()
    hT = dt("hT", [D, NT]); mT = dt("mT", [D, NT]); pT = dt("pT", [PLE, NT])
    w_out = dt("w_out", [D, D])
    wg = dt("wg", [NEXP, D, FF]); wu = dt("wu", [NEXP, D, FF]); wd = dt("wd", [NEXP, FF, D])
    wr = dt("wr", [128, KC, 8])
    wpg = dt("wpg", [D, D]); wpp = dt("wpp", [PLE, D])
    g_ffn = dt("g_ffn", [128, KC]); g_ple = dt("g_ple", [128, KC]); g_fin = dt("g_fin", [128, KC])
    ident = dt("ident", [128, 128]); sel = dt("sel", [8, 8, 128])
    oT = dt("oT", [D, NT], "ExternalOutput")
    s = Sched(nc)
    c = MoECore(s, ident, sel, wr)
    Gf = c.load_vec("Gf", g_ffn)
    Gp = c.load_vec("Gp", g_ple)
    Gl = c.load_vec("Gl", g_fin)
    evs = []
    for it in range(NT // TT):
        tok = slice(it * TT, (it + 1) * TT)
        c.load_tile(hT, tok)
        c.load_bf(c.MT, "MT", mT, tok, "m")
        c.load_bf(c.PT, "PT", pT, tok, "p")
        c.proj_add(w_out, c.MT, "MT")
        c.rmsnorm_hilo("Gf", Gf)
        c.router()
        for ex in range(NEXP):
            gb, gbn = c.gate_bc(ex)
            c.ffn(wg[ex], wu[ex], wd[ex], gate_bc=gb, gate_name=gbn)
        c.rmsnorm("Gp", Gp)
        c.ple(wpg, wpp)
        c.final_norm("Gl", Gl)
        evs.append(c.store_tile(oT, tok))
    s.finish("sp", evs[-1:])
    s.emit()
    return nc


D = 2048
KC = 16
TT = 512
NCH = 8
C = 64
EPS = 1e-6
DK = 128
QSCALE = DK ** -0.5


def k1_consts():
    ident = np.eye(128, dtype=np.float32)
    s_idx = np.arange(C)[:, None]
    t_idx = np.arange(C)[None, :]
    m_u = (s_idx <= t_idx).astype(np.float32)
    m_su = (s_idx < t_idx).astype(np.float32)
    m_sl = (s_idx > t_idx).astype(np.float32)
    masks = np.stack([m_su, m_sl, m_u], 1)
    rst = np.ones((128, TT), np.float32)
    rst[:, ::C] = 0.0
    return {"ident": ident, "masks": np.ascontiguousarray(masks), "rst": rst}


def build_k1(NTOK, SEQ):
    nc = bass.Bass("TRN2", target_bir_lowering=False)
    dt = lambda n, sh, kind="ExternalInput": nc.dram_tensor(n, sh, F32, kind=kind).ap()
    xT = dt("xT", [D, NTOK])
    g_mix = dt("g_mix", [128, KC])
    w_fm = dt("w_fm", [D, 640])
    w_tok = dt("w_tok", [D, 384])
    w_ab = dt("w_ab", [D, 2])
    lbt = dt("lbt", [128, 3])
    convw = dt("convw", [128, 3, 4])
    sc2 = dt("sc2", [1, 2])
    gn = dt("gn", [C, 2, 128])
    ident_d = dt("ident", [128, 128])
    masks_d = dt("masks", [C, 3, C])
    rst_d = dt("rst", [128, TT])
    o = dt("o", [NTOK, 256], "ExternalOutput")

    s = Sched(nc)
    sb = s.sbuf
    H = sb("H", [128, KC, TT], F32)
    UT = sb("UT", [128, KC, TT], BF16)
    SQ = UT
    WFM = sb("WFM", [128, KC, 640], BF16)
    WTOK = sb("WTOK", [128, KC, 384], BF16)
    WAB = sb("WAB", [128, KC, 2], BF16)
    G = sb("G", [128, KC], F32)
    LBT = sb("LBT", [128, 3], F32)
    LB = sb("LB", [128, 4], F32)
    CW = sb("CW", [128, 3, 4], F32)
    SC2 = sb("SC2", [1, 2], F32)
    NEA = sb("NEA", [1, 1], F32)
    GN = sb("GN", [C, 2, 128], F32)
    IDF = sb("IDF", [128, 128], F32)
    IDB = sb("IDB", [128, 128], BF16)
    MASKS = sb("MASKS", [C, 3, C], F32)
    RST = sb("RST", [128, TT], F32)
    ONESB = sb("ONESB", [128, 128], BF16)
    ONER = sb("ONER", [1, 128], F32)
    RSTD = sb("RSTD", [128, TT], F32)
    TOK = sb("TOK", [C, NCH, 384], F32)
    SGT = sb("SGT", [C, NCH, 256], F32)
    AQ = sb("AQ", [128, TT], F32)
    FF_ = sb("FF", [128, TT], F32)
    LOGF = sb("LOGF", [128, TT], F32)
    KA = sb("KA", [128, TT], F32)
    BC = sb("BC", [128, TT], F32)
    EBP = sb("EBP", [128, TT], F32)
    ENB = sb("ENB", [128, TT], F32)
    E2 = sb("E2", [128, TT], F32)
    QE = sb("QE", [128, TT], BF16)
    KE = sb("KE", [128, TT], BF16)
    KE2T = sb("KE2T", [128, TT], BF16)
    KE2A = sb("KE2A", [C, NCH, 128], BF16)
    VA = sb("VA", [C, NCH, 128], BF16)
    STA = sb("STA", [C, NCH, C], BF16)
    SA = sb("SA", [128, 128], F32)
    SAB = sb("SAB", [128, 128], BF16)
    XR = sb("XR", [128, 3, TT + 3], F32)
    XC = sb("XC", [128, 3, TT], F32)
    CS = XC
    SQ2 = sb("SQ2", [128, 2, TT], BF16)
    RS2 = sb("RS2", [128, 2, TT], F32)
    QN = sb("QN", [128, TT], F32)
    KN = sb("KN", [128, TT], F32)
    KNB = sb("KNB", [128, TT], BF16)
    CVB = sb("CVB", [128, TT], BF16)
    EGB = sb("EGB", [128, TT], F32)
    QEB = sb("QEB", [128, TT], BF16)
    ROW = sb("ROW", [1, 8, TT], F32)
    R_SP, R_GAM, R_L, R_NGAM, R_EG, R_EGP, R_EGL, R_BETA = range(8)
    R_G = R_SP
    R_GAMP = R_L
    EE = [sb(f"EE{i}", [C, 3, C], F32) for i in range(2)]
    M_ = [[sb(f"M{p}_{i}", [C, C], F32) for i in range(2)] for p in range(2)]
    N_ = [[sb(f"N{p}_{i}", [C, C], F32) for i in range(2)] for p in range(2)]
    R_ = [[sb(f"R{p}_{i}", [C, C], F32) for i in range(2)] for p in range(2)]
    TTB = [sb(f"TTB{i}", [C, C], BF16) for i in range(2)]
    QKT = [sb(f"QKT{i}", [C, C], BF16) for i in range(4)]
    COL = [sb(f"COL{i}", [C, 3], F32) for i in range(2)]
    XK = [sb(f"XK{i}", [C, 128], BF16) for i in range(2)]
    BV = [sb(f"BV{i}", [C, 128], BF16) for i in range(2)]
    KE2B = [sb(f"KE2B{i}", [C, 128], BF16) for i in range(4)]
    WTB = [sb(f"WTB{i}", [128, C], BF16) for i in range(4)]
    USB = [sb(f"USB{i}", [C, 128], F32) for i in range(4)]
    VN = sb("VN", [C, 128], BF16)
    SB_ = sb("SB", [128, 128], F32)
    SBB = sb("SBB", [128, 128], BF16)
    OUTT = sb("OUTT", [C, NCH, 256], F32)
    SQO = [sb(f"SQO{i}", [C, 128], F32) for i in range(2)]
    SSO = [sb(f"SSO{i}", [C, 2], F32) for i in range(2)]
    TMPO = [sb(f"TMPO{i}", [C, 128], F32) for i in range(2)]

    PS = [s.psum(f"PS{i}", [128, 512], F32) for i in range(5)] + [s.psum("PS5", [128, 1024], BF16)] + \
         [s.psum(f"PS{i}", [128, 512], F32) for i in (6, 7)]

    op = s.op
    s.dma("sp", [(G[:], g_mix)], "c0", writes=["G"])
    s.dma("sp", [(LBT[:], lbt)], "c1", writes=["LBT"])
    s.dma("sp", [(CW[:], convw)], "c2", writes=["CW"])
    s.dma("sp", [(SC2[:], sc2)], "c3", writes=["SC2"])
    s.dma("sp", [(GN[:], gn)], "c4", writes=["GN"])
    s.dma("sp", [(IDF[:], ident_d)], "c5", writes=["IDF"])
    s.dma("sp", [(MASKS[:], masks_d)], "c6", writes=["MASKS"])
    s.dma("sp", [(RST[:], rst_d)], "c7", writes=["RST"])
    s.dma("pool", [(IDB[:], ident_d)], "c8", writes=["IDB"])
    s.dma("pool", [(WFM[:], w_fm.rearrange("(kc p) c -> p kc c", p=128))], "c9", writes=["WFM"])
    s.dma("pool", [(WTOK[:], w_tok.rearrange("(kc p) c -> p kc c", p=128))], "c10", writes=["WTOK"])
    s.dma("pool", [(WAB[:], w_ab.rearrange("(kc p) c -> p kc c", p=128))], "c11", writes=["WAB"])
    op("pool", lambda e: e.memset(ONESB[:], 1.0), writes=["ONESB"])
    op("pool", lambda e: e.memset(ONER[:], 1.0), writes=["ONER"])
    op("act", lambda e: e.activation(out=LBT[:], in_=LBT[:], func=AF.Exp), reads=["LBT"], writes=["LBT"])
    op("dve", lambda e: e.reduce_sum(out=LB[:, 2:3], in_=LBT[:], axis=AX.X), reads=["LBT"], writes=["LB"])
    op("dve", lambda e: e.reciprocal(out=LB[:, 2:3], in_=LB[:, 2:3]), reads=["LB"], writes=["LB"])
    op("dve", lambda e: e.tensor_tensor(out=LB[:, 0:1], in0=LBT[:, 0:1], in1=LB[:, 2:3], op=ALU.mult), reads=["LB", "LBT"], writes=["LB"])
    op("dve", lambda e: e.tensor_scalar(out=LB[:, 1:2], in0=LB[:, 0:1], scalar1=-1.0, scalar2=1.0, op0=ALU.mult, op1=ALU.add),
       reads=["LB"], writes=["LB"])
    op("act", lambda e: e.activation(out=NEA[:], in_=SC2[:, 0:1], func=AF.Exp), reads=["SC2"], writes=["NEA"])
    op("dve", lambda e: e.tensor_scalar_mul(out=NEA[:], in0=NEA[:], scalar1=-1.0), reads=["NEA"], writes=["NEA"])

    ntile = NTOK // TT
    tpb = SEQ // TT
    big_i = [0]

    def big():
        b = big_i[0] % 2
        big_i[0] += 1
        return b

    evs = []
    for it in range(ntile):
        first = (it % tpb == 0)
        tok0 = it * TT
        s.dma("sp", [(H[:], xT[:, tok0:tok0 + TT].rearrange("(kc p) t -> p kc t", p=128))], "h", writes=["H"])
        op("act", lambda e: e.activation(out=SQ[:], in_=H[:], func=AF.Square), reads=["H"], writes=["UT"])
        b = big()
        for kc in range(KC):
            op("pe", lambda e, kc=kc, b=b: e.matmul(PS[b][:], lhsT=ONESB[:], rhs=SQ[:, kc, :], start=(kc == 0), stop=(kc == KC - 1)),
               reads=["ONESB", "UT"], writes=[f"PS{b}"])
        op("act", lambda e, b=b: e.activation(out=RSTD[:], in_=PS[b][:], func=AF.Sqrt, bias=EPS, scale=1.0 / D),
           reads=[f"PS{b}"], writes=["RSTD"])
        op("dve", lambda e: e.reciprocal(out=RSTD[:], in_=RSTD[:]), reads=["RSTD"], writes=["RSTD"])
        for kc in range(KC):
            op("dve", lambda e, kc=kc: e.scalar_tensor_tensor(out=UT[:, kc, :], in0=H[:, kc, :], scalar=G[:, kc:kc + 1],
                                                            in1=RSTD[:], op0=ALU.mult, op1=ALU.mult),
               reads=["H", "G", "RSTD"], writes=[("UT", kc)])
        if first:
            op("pool", lambda e: e.memset(XR[:, :, 0:3], 0.0), writes=["XR"])
            op("pool", lambda e: e.memset(SA[:], 0.0), writes=["SA"])
            op("pool", lambda e: e.memset(SAB[:], 0.0), writes=["SAB"])
            op("pool", lambda e: e.memset(SB_[:], 0.0), writes=["SB"])
            op("pool", lambda e: e.memset(SBB[:], 0.0), writes=["SBB"])

        def fm_proj(col, M, b):
            for kc in range(KC):
                op("pe", lambda e, kc=kc: e.matmul(PS[b][0:M, :], lhsT=(WFM[:, kc, col * 128:(col + 1) * 128] if M == 128 else WAB[:, kc, col:col + 1]),
                                                 rhs=UT[:, kc, :], start=(kc == 0), stop=(kc == KC - 1)),
                   reads=["WFM", "WAB", ("UT", kc)], writes=[f"PS{b}"])
        b = big(); fm_proj(0, 128, b)
        op("act", lambda e, b=b: e.activation(out=AQ[:], in_=PS[b][:], func=AF.Copy), reads=[f"PS{b}"], writes=["AQ"])
        b = big(); fm_proj(1, 128, b)
        op("act", lambda e, b=b: e.activation(out=FF_[:], in_=PS[b][:], func=AF.Sigmoid), reads=[f"PS{b}"], writes=["FF"])
        for i in range(3):
            b = big(); fm_proj(2 + i, 128, b)
            op("act", lambda e, b=b, i=i: e.activation(out=XR[:, i, 3:TT + 3], in_=PS[b][:], func=AF.Copy), reads=[f"PS{b}"], writes=["XR"])
        b = big(); fm_proj(0, 1, b)
        op("act", lambda e, b=b: e.activation(out=ROW[:, R_SP, :], in_=PS[b][0:1, :], func=AF.Exp, bias=SC2[:, 1:2]),
           reads=[f"PS{b}", "SC2"], writes=[("ROW", R_SP)])
        op("act", lambda e: e.activation(out=ROW[:, R_SP, :], in_=ROW[:, R_SP, :], func=AF.Ln, bias=1.0),
           reads=[("ROW", R_SP)], writes=[("ROW", R_SP)])
        op("dve", lambda e: e.tensor_scalar(out=ROW[:, R_G, :], in0=ROW[:, R_SP, :], scalar1=NEA[:, 0:1], scalar2=None, op0=ALU.mult),
           reads=[("ROW", R_SP), "NEA"], writes=[("ROW", R_G)])
        b = big(); fm_proj(1, 1, b)
        op("act", lambda e, b=b: e.activation(out=ROW[:, R_BETA, :], in_=PS[b][0:1, :], func=AF.Sigmoid),
           reads=[f"PS{b}"], writes=[("ROW", R_BETA)])
        op("act", lambda e, b=b: e.activation(out=ROW[:, R_L, :], in_=PS[b][0:1, :], func=AF.Exp, scale=-1.0),
           reads=[f"PS{b}"], writes=[("ROW", R_L)])
        op("act", lambda e: e.activation(out=ROW[:, R_L, :], in_=ROW[:, R_L, :], func=AF.Ln, bias=1.0),
           reads=[("ROW", R_L)], writes=[("ROW", R_L)])
        for j in range(NCH):
            pb = 2 + (j % 2)
            for kc in range(KC):
                op("pe", lambda e, kc=kc, j=j, pb=pb: e.matmul(PS[pb][0:C, 0:384], lhsT=UT[:, kc, j * C:(j + 1) * C], rhs=WTOK[:, kc, :],
                                                            start=(kc == 0), stop=(kc == KC - 1)),
                   reads=["WTOK", ("UT", kc)], writes=[f"PS{pb}"])
            op("act", lambda e, j=j, pb=pb: e.activation(out=TOK[:, j, :], in_=PS[pb][0:C, 0:384], func=AF.Copy),
               reads=[f"PS{pb}"], writes=[("TOK", j)])
        op("act", lambda e: e.activation(out=SGT[:], in_=TOK[:, :, 128:384], func=AF.Silu), reads=["TOK"], writes=["SGT"])
        op("pool", lambda e: e.tensor_copy(out=VA[:], in_=TOK[:, :, 0:128]), reads=["TOK"], writes=["VA"])

        op("dve", lambda e: e.tensor_scalar(out=FF_[:], in0=FF_[:], scalar1=LB[:, 1:2], scalar2=LB[:, 0:1], op0=ALU.mult, op1=ALU.add),
           reads=["FF", "LB"], writes=["FF"])
        op("act", lambda e: e.activation(out=LOGF[:], in_=FF_[:], func=AF.Ln), reads=["FF"], writes=["LOGF"])
        op("dve", lambda e: e.tensor_scalar(out=KA[:], in0=FF_[:], scalar1=-1.0, scalar2=1.0, op0=ALU.mult, op1=ALU.add),
           reads=["FF"], writes=["KA"])
        op("dve", lambda e: e.tensor_tensor_scan(out=BC[:], data0=RST[:], data1=LOGF[:], initial=0.0, op0=ALU.mult, op1=ALU.add),
           reads=["RST", "LOGF"], writes=["BC"])
        op("act", lambda e: e.activation(out=EBP[:], in_=BC[:], func=AF.Exp), reads=["BC"], writes=["EBP"])
        op("act", lambda e: e.activation(out=ENB[:], in_=BC[:], func=AF.Exp, scale=-1.0), reads=["BC"], writes=["ENB"])
        for j in range(NCH):
            op("act", lambda e, j=j: e.activation(out=E2[:, j * C:(j + 1) * C], in_=BC[:, j * C:(j + 1) * C], func=AF.Exp, scale=-1.0,
                                                bias=BC[:, (j + 1) * C - 1:(j + 1) * C]), reads=["BC"], writes=["E2"])
        op("dve", lambda e: e.scalar_tensor_tensor(out=QE[:], in0=AQ[:], scalar=QSCALE, in1=EBP[:], op0=ALU.mult, op1=ALU.mult),
           reads=["AQ", "EBP"], writes=["QE"])
        op("dve", lambda e: e.tensor_tensor(out=KE[:], in0=KA[:], in1=ENB[:], op=ALU.mult), reads=["KA", "ENB"], writes=["KE"])
        op("dve", lambda e: e.tensor_tensor(out=KE2T[:], in0=KA[:], in1=E2[:], op=ALU.mult), reads=["KA", "E2"], writes=["KE2T"])

        for i in range(3):
            op("dve", lambda e, i=i: e.tensor_scalar(out=XC[:, i, :], in0=XR[:, i, 3:TT + 3], scalar1=CW[:, i, 3:4], scalar2=None, op0=ALU.mult),
               reads=["XR", "CW"], writes=[("XC", i)])
            for k in range(3):
                op("dve", lambda e, i=i, k=k: e.scalar_tensor_tensor(out=XC[:, i, :], in0=XR[:, i, k:TT + k], scalar=CW[:, i, k:k + 1],
                                                                    in1=XC[:, i, :], op0=ALU.mult, op1=ALU.add),
                   reads=["XR", "CW", ("XC", i)], writes=[("XC", i)])
        op("pool", lambda e: e.tensor_copy(out=XR[:, :, 0:3], in_=XR[:, :, TT:TT + 3]), reads=["XR", "XC"], writes=["XR"])
        op("act", lambda e: e.activation(out=CS[:], in_=XC[:], func=AF.Silu), reads=["XC"], writes=["XC"])
        op("act", lambda e: e.activation(out=SQ2[:], in_=CS[:, 0:2, :], func=AF.Square), reads=["XC"], writes=["SQ2"])
        for i in range(2):
            b = big()
            op("pe", lambda e, i=i, b=b: e.matmul(PS[b][:], lhsT=ONESB[:], rhs=SQ2[:, i, :], start=True, stop=True),
               reads=["ONESB", "SQ2"], writes=[f"PS{b}"])
            op("act", lambda e, i=i, b=b: e.activation(out=RS2[:, i, :], in_=PS[b][:], func=AF.Sqrt, bias=EPS), reads=[f"PS{b}"], writes=[("RS2", i)])
        op("dve", lambda e: e.reciprocal(out=RS2[:], in_=RS2[:]), reads=["RS2"], writes=["RS2"])
        op("dve", lambda e: e.scalar_tensor_tensor(out=QN[:], in0=CS[:, 0, :], scalar=QSCALE, in1=RS2[:, 0, :], op0=ALU.mult, op1=ALU.mult),
           reads=["XC", "RS2"], writes=["QN"])
        op("dve", lambda e: e.tensor_tensor(out=KN[:], in0=CS[:, 1, :], in1=RS2[:, 1, :], op=ALU.mult), reads=["XC", "RS2"], writes=["KN"])
        op("act", lambda e: e.activation(out=KNB[:], in_=KN[:], func=AF.Copy), reads=["KN"], writes=["KNB"])
        op("act", lambda e: e.activation(out=CVB[:], in_=CS[:, 2, :], func=AF.Copy), reads=["XC"], writes=["CVB"])
        op("dve", lambda e: e.tensor_tensor_scan(out=ROW[:, R_GAM, :], data0=RST[0:1, :], data1=ROW[:, R_G, :], initial=0.0,
                                                 op0=ALU.mult, op1=ALU.add), reads=["RST", ("ROW", R_G)], writes=[("ROW", R_GAM)])
        op("dve", lambda e: e.tensor_tensor(out=ROW[:, R_GAMP, :], in0=ROW[:, R_GAM, :], in1=ROW[:, R_L, :], op=ALU.subtract),
           reads=[("ROW", R_GAM), ("ROW", R_L)], writes=[("ROW", R_GAMP)])
        op("dve", lambda e: e.tensor_scalar_mul(out=ROW[:, R_NGAM, :], in0=ROW[:, R_GAM, :], scalar1=-1.0),
           reads=[("ROW", R_GAM)], writes=[("ROW", R_NGAM)])
        op("act", lambda e: e.activation(out=ROW[:, R_EG, :], in_=ROW[:, R_GAM, :], func=AF.Exp), reads=[("ROW", R_GAM)], writes=[("ROW", R_EG)])
        op("act", lambda e: e.activation(out=ROW[:, R_EGP, :], in_=ROW[:, R_GAMP, :], func=AF.Exp), reads=[("ROW", R_GAMP)], writes=[("ROW", R_EGP)])
        for j in range(NCH):
            op("act", lambda e, j=j: e.activation(out=ROW[:, R_EGL, j * C:(j + 1) * C], in_=ROW[:, R_GAM, j * C:(j + 1) * C], func=AF.Exp,
                                                scale=-1.0, bias=ROW[:, R_GAM, (j + 1) * C - 1:(j + 1) * C]),
               reads=[("ROW", R_GAM)], writes=[("ROW", R_EGL)])
        b = big()
        op("pe", lambda e, b=b: e.matmul(PS[b][:], lhsT=ONER[0:1, :], rhs=ROW[:, R_EG, :], start=True, stop=True),
           reads=["ONER", ("ROW", R_EG)], writes=[f"PS{b}"])
        op("act", lambda e, b=b: e.activation(out=EGB[:], in_=PS[b][:], func=AF.Copy), reads=[f"PS{b}"], writes=["EGB"])
        op("dve", lambda e: e.tensor_tensor(out=QEB[:], in0=QN[:], in1=EGB[:], op=ALU.mult), reads=["QN", "EGB"], writes=["QEB"])

        P2, P3, P4, P5, P6, P7 = PS[2], PS[3], PS[4], PS[5], PS[6], PS[7]

        def pre_ops(j):
            L = []
            add = lambda eng, fn, r=(), w=(): L.append((eng, fn, r, w))
            cs = slice(j * C, (j + 1) * C)
            jb = j % 4
            pb = j % 2
            PX = P4 if pb == 0 else PS[1]
            PXn = 'PS4' if pb == 0 else 'PS1'
            P5o = pb * 384
            P6o = pb * 256
            add("pe", lambda e: e.transpose(P5[0:C, P5o:P5o + 128], KE2T[:, cs], IDB[:]), ["KE2T", "IDB"], ["PS5"])
            add("act", lambda e: e.activation(out=KE2A[:, j, :], in_=P5[0:C, P5o:P5o + 128], func=AF.Copy), ["PS5"], [("KE2A", j)])
            add("pe", lambda e: e.matmul(P6[0:C, P6o + 192:P6o + 256], lhsT=KE[:, cs], rhs=QE[:, cs], start=True, stop=True), ["KE", "QE"], ["PS6"])
            add("dve", lambda e: e.tensor_tensor(out=STA[:, j, :], in0=P6[0:C, P6o + 192:P6o + 256], in1=MASKS[:, 2, :], op=ALU.mult),
                ["PS6", "MASKS"], [("STA", j)])
            add("pe", lambda e: e.matmul(PX[0:C, 0:64], lhsT=KN[:, cs], rhs=KN[:, cs], start=True, stop=True), ["KN"], [PXn])
            add("pe", lambda e: e.matmul(PX[0:C, 64:128], lhsT=KN[:, cs], rhs=QN[:, cs], start=True, stop=True), ["KN", "QN"], [PXn])
            add("pe", lambda e: e.matmul(PX[0:C, 128:192], lhsT=ONER[0:1, 0:C], rhs=ROW[:, R_GAMP, cs], start=True, stop=False),
                ["ONER", ("ROW", R_GAMP)], [PXn])
            add("pe", lambda e: e.matmul(PX[0:C, 128:192], lhsT=ROW[:, R_NGAM, cs], rhs=ONER[0:1, 0:C], start=False, stop=True),
                ["ONER", ("ROW", R_NGAM)], [PXn])
            add("pe", lambda e: e.matmul(PX[0:C, 192:256], lhsT=ROW[:, R_GAMP, cs], rhs=ONER[0:1, 0:C], start=True, stop=False),
                ["ONER", ("ROW", R_GAMP)], [PXn])
            add("pe", lambda e: e.matmul(PX[0:C, 192:256], lhsT=ONER[0:1, 0:C], rhs=ROW[:, R_NGAM, cs], start=False, stop=True),
                ["ONER", ("ROW", R_NGAM)], [PXn])
            add("pe", lambda e: e.matmul(PX[0:C, 256:320], lhsT=ONER[0:1, 0:C], rhs=ROW[:, R_GAM, cs], start=True, stop=False),
                ["ONER", ("ROW", R_GAM)], [PXn])
            add("pe", lambda e: e.matmul(PX[0:C, 256:320], lhsT=ROW[:, R_NGAM, cs], rhs=ONER[0:1, 0:C], start=False, stop=True),
                ["ONER", ("ROW", R_NGAM)], [PXn])
            add("dve", lambda e: e.tensor_scalar_min(out=EE[pb][:], in0=PX[0:C, 128:320].rearrange("p (a b) -> p a b", a=3), scalar1=0.0),
                [PXn], [f"EE{pb}"])
            add("act", lambda e: e.activation(out=EE[pb][:], in_=EE[pb][:], func=AF.Exp), [f"EE{pb}"], [f"EE{pb}"])
            add("dve", lambda e: e.tensor_tensor(out=EE[pb][:], in0=EE[pb][:], in1=MASKS[:], op=ALU.mult), [f"EE{pb}", "MASKS"], [f"EE{pb}"])
            add("dve", lambda e: e.scalar_tensor_tensor(out=M_[pb][0][:], in0=PX[0:C, 0:64], scalar=-1.0, in1=EE[pb][:, 0, :], op0=ALU.mult, op1=ALU.mult),
                [PXn, f"EE{pb}"], [f"M{pb}_0"])
            add("dve", lambda e: e.scalar_tensor_tensor(out=N_[pb][0][:], in0=PX[0:C, 0:64], scalar=-1.0, in1=EE[pb][:, 1, :], op0=ALU.mult, op1=ALU.mult),
                [PXn, f"EE{pb}"], [f"N{pb}_0"])
            add("dve", lambda e: e.tensor_tensor(out=QKT[jb][:], in0=PX[0:C, 64:128], in1=EE[pb][:, 2, :], op=ALU.mult), [PXn, f"EE{pb}"], [f"QKT{jb}"])
            add("dve", lambda e: e.tensor_tensor(out=R_[pb][0][:], in0=M_[pb][0][:], in1=IDF[0:C, 0:C], op=ALU.add), [f"M{pb}_0", "IDF"], [f"R{pb}_0"])
            cur = 0
            for lvl in range(1, 6):
                nx = 1 - cur
                lastl = (lvl == 5)
                if not lastl:
                    add("pe", lambda e, cur=cur: e.matmul(PX[0:C, 320:384], lhsT=N_[pb][cur][:], rhs=M_[pb][cur][:], start=True, stop=True),
                        [f"N{pb}_{cur}", f"M{pb}_{cur}"], [PXn])
                add("pe", lambda e, cur=cur: e.matmul(PX[0:C, 384:448], lhsT=M_[pb][cur][:], rhs=N_[pb][cur][:], start=True, stop=True),
                    [f"N{pb}_{cur}", f"M{pb}_{cur}"], [PXn])
                if not lastl:
                    add("act", lambda e, nx=nx: e.activation(out=M_[pb][nx][:], in_=PX[0:C, 320:384], func=AF.Copy), [PXn], [f"M{pb}_{nx}"])
                add("act", lambda e, nx=nx: e.activation(out=N_[pb][nx][:], in_=PX[0:C, 384:448], func=AF.Copy), [PXn], [f"N{pb}_{nx}"])
                add("pe", lambda e, cur=cur, nx=nx: e.matmul(PX[0:C, 448:512], lhsT=N_[pb][nx][:], rhs=R_[pb][cur][:], start=True, stop=True),
                    [f"N{pb}_{nx}", f"R{pb}_{cur}"], [PXn])
                if lastl:
                    add("dve", lambda e, cur=cur: e.tensor_tensor(out=TTB[pb][:], in0=R_[pb][cur][:], in1=PX[0:C, 448:512], op=ALU.add),
                        [f"R{pb}_{cur}", PXn], [f"TTB{pb}"])
                else:
                    add("dve", lambda e, cur=cur, nx=nx: e.tensor_tensor(out=R_[pb][nx][:], in0=R_[pb][cur][:], in1=PX[0:C, 448:512], op=ALU.add),
                        [f"R{pb}_{cur}", PXn], [f"R{pb}_{nx}"])
                cur = nx
            add("pe", lambda e: e.transpose(P5[0:C, P5o + 128:P5o + 256], KNB[:, cs], IDB[:]), ["KNB", "IDB"], ["PS5"])
            add("pe", lambda e: e.transpose(P5[0:C, P5o + 256:P5o + 384], CVB[:, cs], IDB[:]), ["CVB", "IDB"], ["PS5"])
            for ci, rr in enumerate((R_EGP, R_BETA, R_EGL)):
                add("pe", lambda e, ci=ci, rr=rr: e.matmul(P3[0:C, 128 + pb * 4 + ci:129 + pb * 4 + ci], lhsT=ROW[:, rr, cs], rhs=ONER[0:1, 0:1], start=True, stop=True),
                    [("ROW", rr), "ONER"], ["PS3"])
            add("act", lambda e: e.activation(out=COL[pb][:], in_=P3[0:C, 128 + pb * 4:131 + pb * 4], func=AF.Copy), ["PS3"], [f"COL{pb}"])
            add("dve", lambda e: e.tensor_scalar(out=XK[pb][:], in0=P5[0:C, P5o + 128:P5o + 256], scalar1=COL[pb][:, 0:1], scalar2=None, op0=ALU.mult),
                ["PS5", f"COL{pb}"], [f"XK{pb}"])
            add("dve", lambda e: e.tensor_scalar(out=KE2B[jb][:], in0=P5[0:C, P5o + 128:P5o + 256], scalar1=COL[pb][:, 2:3], scalar2=None, op0=ALU.mult),
                ["PS5", f"COL{pb}"], [f"KE2B{jb}"])
            add("dve", lambda e: e.tensor_scalar(out=BV[pb][:], in0=P5[0:C, P5o + 256:P5o + 384], scalar1=COL[pb][:, 1:2], scalar2=None, op0=ALU.mult),
                ["PS5", f"COL{pb}"], [f"BV{pb}"])
            add("pe", lambda e: e.matmul(P6[:, P6o:P6o + 64], lhsT=XK[pb][:], rhs=TTB[pb][:], start=True, stop=True), [f"XK{pb}", f"TTB{pb}"], ["PS6"])
            add("act", lambda e: e.activation(out=WTB[jb][:], in_=P6[:, P6o:P6o + 64], func=AF.Copy), ["PS6"], [f"WTB{jb}"])
            add("pe", lambda e: e.matmul(P6[0:C, P6o + 64:P6o + 192], lhsT=TTB[pb][:], rhs=BV[pb][:], start=True, stop=True), [f"BV{pb}", f"TTB{pb}"], ["PS6"])
            add("act", lambda e: e.activation(out=USB[jb][:], in_=P6[0:C, P6o + 64:P6o + 192], func=AF.Copy), ["PS6"], [f"USB{jb}"])
            return L

        def chain_ops(j):
            L = []
            add = lambda eng, fn, r=(), w=(): L.append((eng, fn, r, w))
            cs = slice(j * C, (j + 1) * C)
            last = slice((j + 1) * C - 1, (j + 1) * C)
            jb = j % 4

            def out_norm(ps_ap, pskey, gi):
                add("act", lambda e: e.activation(out=SQO[gi][:], in_=ps_ap, func=AF.Square), [pskey], [f"SQO{gi}"])
                add("dve", lambda e: e.reduce_sum(out=SSO[gi][:, 0:1], in_=SQO[gi][:], axis=AX.X), [f"SQO{gi}"], [f"SSO{gi}"])
                add("act", lambda e: e.activation(out=SSO[gi][:, 1:2], in_=SSO[gi][:, 0:1], func=AF.Sqrt, bias=EPS, scale=1.0 / 128),
                    [f"SSO{gi}"], [f"SSO{gi}"])
                add("dve", lambda e: e.reciprocal(out=SSO[gi][:, 1:2], in_=SSO[gi][:, 1:2]), [f"SSO{gi}"], [f"SSO{gi}"])
                add("dve", lambda e: e.scalar_tensor_tensor(out=TMPO[gi][:], in0=ps_ap, scalar=SSO[gi][:, 1:2], in1=GN[:, gi, :],
                                                            op0=ALU.mult, op1=ALU.mult), [pskey, f"SSO{gi}", "GN"], [f"TMPO{gi}"])
                add("dve", lambda e: e.tensor_tensor(out=OUTT[:, j, gi * 128:(gi + 1) * 128], in0=TMPO[gi][:], in1=SGT[:, j, gi * 128:(gi + 1) * 128],
                                                     op=ALU.mult), [f"TMPO{gi}", "SGT"], [("OUTT", (j, gi))])
            add("pe", lambda e: e.matmul(P7[0:C, 0:128], lhsT=WTB[jb][:], rhs=SBB[:], start=True, stop=True), [f"WTB{jb}", "SBB"], ["PS7"])
            add("dve", lambda e: e.tensor_tensor(out=VN[:], in0=USB[jb][:], in1=P7[0:C, 0:128], op=ALU.subtract), [f"USB{jb}", "PS7"], ["VN"])
            add("pe", lambda e: e.matmul(P3[:, 0:128], lhsT=KE2A[:, j, :], rhs=VA[:, j, :], start=True, stop=True), [("KE2A", j), "VA"], ["PS3"])
            add("pe", lambda e: e.matmul(P2[0:C, 0:128], lhsT=STA[:, j, :], rhs=VA[:, j, :], start=True, stop=False), [("STA", j), "VA"], ["PS2"])
            add("pe", lambda e: e.matmul(P2[0:C, 0:128], lhsT=QE[:, cs], rhs=SAB[:], start=False, stop=True), ["QE", "SAB"], ["PS2"])
            add("dve", lambda e: e.scalar_tensor_tensor(out=SA[:], in0=SA[:], scalar=EBP[:, last], in1=P3[:, 0:128], op0=ALU.mult, op1=ALU.add),
                ["SA", "EBP", "PS3"], ["SA"])
            add("act", lambda e: e.activation(out=SAB[:], in_=SA[:], func=AF.Copy), ["SA"], ["SAB"])
            add("pe", lambda e: e.matmul(P7[0:C, 128:256], lhsT=QEB[:, cs], rhs=SBB[:], start=True, stop=False), ["QEB", "SBB"], ["PS7"])
            add("pe", lambda e: e.matmul(P7[0:C, 128:256], lhsT=QKT[jb][:], rhs=VN[:], start=False, stop=True), [f"QKT{jb}", "VN"], ["PS7"])
            add("pe", lambda e: e.matmul(P7[:, 256:384], lhsT=KE2B[jb][:], rhs=VN[:], start=True, stop=True), [f"KE2B{jb}", "VN"], ["PS7"])
            add("dve", lambda e: e.scalar_tensor_tensor(out=SB_[:], in0=SB_[:], scalar=EGB[:, last], in1=P7[:, 256:384], op0=ALU.mult, op1=ALU.add),
                ["SB", "EGB", "PS7"], ["SB"])
            add("act", lambda e: e.activation(out=SBB[:], in_=SB_[:], func=AF.Copy), ["SB"], ["SBB"])
            out_norm(P2[0:C, 0:128], "PS2", 0)
            out_norm(P7[0:C, 128:256], "PS7", 1)
            return L

        def submit(lst):
            for (eng, fn, r, w) in lst:
                op(eng, fn, reads=r, writes=w)

        def merge(a, b):
            out = []
            ia = ib = 0
            na, nb = len(a), len(b)
            while ia < na or ib < nb:
                if ib >= nb or (ia < na and ia * nb <= ib * na):
                    out.append(a[ia]); ia += 1
                else:
                    out.append(b[ib]); ib += 1
            return out

        submit(merge(pre_ops(0), pre_ops(1)))
        for j in range(0, NCH, 2):
            nxt = merge(pre_ops(j + 2), pre_ops(j + 3)) if j + 3 < NCH else []
            submit(merge(nxt, chain_ops(j) + chain_ops(j + 1)))
        evs.append(s.dma("sp", [(o[tok0:tok0 + TT, :].rearrange("(j p) c -> p j c", p=C), OUTT[:])], "st", reads=["OUTT"]))
    s.finish("sp", evs[-1:])
    s.emit()
    return nc


D = 2048
KC = 16
TT = 512
EPS = 1e-6
CB = 256


def build_k3(NTOK, SEQ):
    nc = bass.Bass("TRN2", target_bir_lowering=False)
    dt = lambda n, sh, kind="ExternalInput": nc.dram_tensor(n, sh, F32, kind=kind).ap()
    hT = dt("hT", [D, NTOK])
    g_mix = dt("g_mix", [128, KC])
    w_y = dt("w_y", [D, CB]); w_x = dt("w_x", [D, CB])
    w_r = dt("w_r", [CB, CB]); w_i = dt("w_i", [CB, CB])
    cvec = dt("cvec", [128, 2, 8])
    oT = dt("oT", [CB, NTOK], "ExternalOutput")
    s = Sched(nc)
    H = [s.sbuf(f"H{i}", [128, KC, TT], F32) for i in range(2)]
    SQ = s.sbuf("SQ", [128, KC, TT], BF16)
    UT = s.sbuf("UT", [128, KC, TT], BF16)
    WY = s.sbuf("WY", [128, KC, CB], BF16)
    WX = s.sbuf("WX", [128, KC, CB], BF16)
    WR = s.sbuf("WR", [128, 2, CB], BF16)
    WI = s.sbuf("WI", [128, 2, CB], BF16)
    CV = s.sbuf("CV", [128, 2, 8], F32)
    C8 = s.sbuf("C8", [128, 2], F32)
    G = s.sbuf("G", [128, KC], F32)
    ONES = s.sbuf("ONES", [128, 128], BF16)
    RSTD = s.sbuf("RSTD", [128, TT], F32)
    YF = s.sbuf("YF", [128, 2, TT], F32)
    T1 = s.sbuf("T1", [128, 2, TT], F32)
    GY = s.sbuf("GY", [128, 2, TT], F32)
    XR = s.sbuf("XR", [128, 2, TT + 3], F32)
    XC = s.sbuf("XC", [128, 2, TT], F32)
    XCB = s.sbuf("XCB", [128, 2, TT], BF16)
    RG = s.sbuf("RG", [128, 2, TT], F32)
    IG = s.sbuf("IG", [128, 2, TT], F32)
    AA = s.sbuf("AA", [128, 2, TT], F32)
    MM = s.sbuf("MM", [128, 2, TT], F32)
    BB = s.sbuf("BB", [128, 2, TT], F32)
    HS = [s.sbuf(f"HS{i}", [128, 2, TT], F32) for i in range(2)]
    OUT = [s.sbuf(f"OUT{i}", [128, 2, TT], F32) for i in range(2)]
    PB = s.psum("PB", [128, 8, TT])
    pbi = [0]

    def bank():
        b = pbi[0] % 8
        pbi[0] += 1
        return b

    s.op("pool", lambda e: e.memset(ONES[:], 1.0), writes=["ONES"])
    s.dma("sp", [(G[:], g_mix)], "c0", writes=["G"])
    s.dma("sp", [(CV[:], cvec)], "c1", writes=["CV"])
    s.dma("pool", [(WY[:], w_y.rearrange("(kc p) c -> p kc c", p=128))], "c2", writes=["WY"])
    s.dma("pool", [(WX[:], w_x.rearrange("(kc p) c -> p kc c", p=128))], "c3", writes=["WX"])
    s.dma("pool", [(WR[:], w_r.rearrange("(kc p) c -> p kc c", p=128))], "c4", writes=["WR"])
    s.dma("pool", [(WI[:], w_i.rearrange("(kc p) c -> p kc c", p=128))], "c5", writes=["WI"])
    s.op("act", lambda e: e.activation(out=C8[:], in_=CV[:, :, 7], func=AF.Exp, scale=-1.0), reads=["CV"], writes=["C8"])
    s.op("act", lambda e: e.activation(out=C8[:], in_=C8[:], func=AF.Ln, bias=1.0), reads=["C8"], writes=["C8"])
    s.op("dve", lambda e: e.tensor_scalar_mul(out=C8[:], in0=C8[:], scalar1=-8.0), reads=["C8"], writes=["C8"])

    ntile = NTOK // TT
    tpb = SEQ // TT
    evs = []
    s.dma("sp", [(H[0][:], hT[:, 0:TT].rearrange("(kc p) t -> p kc t", p=128))], "h0", writes=["H0"])
    for it in range(ntile):
        Hc, Hn = H[it % 2], f"H{it % 2}"
        if it + 1 < ntile:
            nx = (it + 1) % 2
            s.dma("sp", [(H[nx][:], hT[:, (it + 1) * TT:(it + 2) * TT].rearrange("(kc p) t -> p kc t", p=128))], f"h{nx}",
                  writes=[f"H{nx}"])
        first = (it % tpb == 0)
        HSc, HSn = HS[it % 2], f"HS{it % 2}"
        HSp = HS[(it + 1) % 2]
        HSpn = f"HS{(it + 1) % 2}"
        O, On = OUT[it % 2], f"OUT{it % 2}"
        s.op("act", lambda e, Hc=Hc: e.activation(out=SQ[:], in_=Hc[:], func=AF.Square), reads=[Hn], writes=["SQ"])
        b = bank()
        for kc in range(KC):
            s.op("pe", lambda e, kc=kc, b=b: e.matmul(PB[:, b, :], lhsT=ONES[:], rhs=SQ[:, kc, :], start=(kc == 0), stop=(kc == KC - 1)),
                 reads=["ONES", "SQ"], writes=[("PB", b)])
        s.op("act", lambda e, b=b: e.activation(out=RSTD[:], in_=PB[:, b, :], func=AF.Sqrt, bias=EPS, scale=1.0 / D),
             reads=[("PB", b)], writes=["RSTD"])
        s.op("dve", lambda e: e.reciprocal(out=RSTD[:], in_=RSTD[:]), reads=["RSTD"], writes=["RSTD"])
        for kc in range(KC):
            s.op("dve", lambda e, kc=kc, Hc=Hc: e.scalar_tensor_tensor(out=UT[:, kc, :], in0=Hc[:, kc, :], scalar=G[:, kc:kc + 1],
                                                                    in1=RSTD[:], op0=ALU.mult, op1=ALU.mult),
                 reads=[Hn, "G", "RSTD"], writes=[("UT", kc)])
        by = [bank(), bank()]
        bx = [bank(), bank()]
        for c in range(2):
            for kc in range(KC):
                s.op("pe", lambda e, kc=kc, c=c: e.matmul(PB[:, by[c], :], lhsT=WY[:, kc, c * 128:(c + 1) * 128], rhs=UT[:, kc, :],
                                                       start=(kc == 0), stop=(kc == KC - 1)),
                     reads=["WY", ("UT", kc)], writes=[("PB", by[c])])
            for kc in range(KC):
                s.op("pe", lambda e, kc=kc, c=c: e.matmul(PB[:, bx[c], :], lhsT=WX[:, kc, c * 128:(c + 1) * 128], rhs=UT[:, kc, :],
                                                       start=(kc == 0), stop=(kc == KC - 1)),
                     reads=["WX", ("UT", kc)], writes=[("PB", bx[c])])
        if first:
            s.op("pool", lambda e: e.memset(XR[:, :, 0:3], 0.0), writes=["XR"])
        for c in range(2):
            s.op("act", lambda e, c=c: e.activation(out=YF[:, c, :], in_=PB[:, by[c], :], func=AF.Copy),
                 reads=[("PB", by[c])], writes=[("YF", c)])
            s.op("act", lambda e, c=c: e.activation(out=XR[:, c, 3:TT + 3], in_=PB[:, bx[c], :], func=AF.Copy),
                 reads=[("PB", bx[c])], writes=["XR"])
        s.op("dve", lambda e: e.tensor_tensor(out=T1[:], in0=YF[:], in1=YF[:], op=ALU.mult), reads=["YF"], writes=["T1"])
        s.op("dve", lambda e: e.tensor_scalar(out=T1[:], in0=T1[:], scalar1=0.044715, scalar2=1.0, op0=ALU.mult, op1=ALU.add),
             reads=["T1"], writes=["T1"])
        s.op("dve", lambda e: e.tensor_tensor(out=T1[:], in0=T1[:], in1=YF[:], op=ALU.mult), reads=["T1", "YF"], writes=["T1"])
        s.op("act", lambda e: e.activation(out=T1[:], in_=T1[:], func=AF.Sigmoid, scale=1.5957691216), reads=["T1"], writes=["T1"])
        s.op("dve", lambda e: e.tensor_tensor(out=GY[:], in0=T1[:], in1=YF[:], op=ALU.mult), reads=["T1", "YF"], writes=["GY"])
        for c in range(2):
            s.op("dve", lambda e, c=c: e.tensor_scalar(out=XC[:, c, :], in0=XR[:, c, 3:TT + 3], scalar1=CV[:, c, 3:4], scalar2=CV[:, c, 4:5],
                                                      op0=ALU.mult, op1=ALU.add), reads=["XR", "CV"], writes=[("XC", c)])
            for k in range(3):
                s.op("dve", lambda e, c=c, k=k: e.scalar_tensor_tensor(out=XC[:, c, :], in0=XR[:, c, k:TT + k], scalar=CV[:, c, k:k + 1],
                                                                      in1=XC[:, c, :], op0=ALU.mult, op1=ALU.add),
                     reads=["XR", "CV", ("XC", c)], writes=[("XC", c)])
        s.op("act", lambda e: e.activation(out=XCB[:], in_=XC[:], func=AF.Copy), reads=["XC"], writes=["XCB"])
        s.op("pool", lambda e: e.tensor_copy(out=XR[:, :, 0:3], in_=XR[:, :, TT:TT + 3]), reads=["XR"], writes=["XR"])
        br = [bank(), bank()]
        bi = [bank(), bank()]
        for c in range(2):
            for kc in range(2):
                s.op("pe", lambda e, kc=kc, c=c: e.matmul(PB[:, br[c], :], lhsT=WR[:, kc, c * 128:(c + 1) * 128], rhs=XCB[:, kc, :],
                                                       start=(kc == 0), stop=(kc == 1)), reads=["WR", "XCB"], writes=[("PB", br[c])])
            for kc in range(2):
                s.op("pe", lambda e, kc=kc, c=c: e.matmul(PB[:, bi[c], :], lhsT=WI[:, kc, c * 128:(c + 1) * 128], rhs=XCB[:, kc, :],
                                                       start=(kc == 0), stop=(kc == 1)), reads=["WI", "XCB"], writes=[("PB", bi[c])])
        for c in range(2):
            s.op("act", lambda e, c=c: e.activation(out=RG[:, c, :], in_=PB[:, br[c], :], func=AF.Sigmoid, bias=CV[:, c, 5:6]),
                 reads=[("PB", br[c]), "CV"], writes=[("RG", c)])
            s.op("act", lambda e, c=c: e.activation(out=IG[:, c, :], in_=PB[:, bi[c], :], func=AF.Sigmoid, bias=CV[:, c, 6:7]),
                 reads=[("PB", bi[c]), "CV"], writes=[("IG", c)])
        for c in range(2):
            s.op("act", lambda e, c=c: e.activation(out=AA[:, c, :], in_=RG[:, c, :], func=AF.Exp, scale=C8[:, c:c + 1]),
                 reads=[("RG", c), "C8"], writes=[("AA", c)])
        s.op("dve", lambda e: e.tensor_tensor(out=MM[:], in0=AA[:], in1=AA[:], op=ALU.mult), reads=["AA"], writes=["MM"])
        s.op("dve", lambda e: e.tensor_scalar(out=MM[:], in0=MM[:], scalar1=-1.0, scalar2=1.0, op0=ALU.mult, op1=ALU.add),
             reads=["MM"], writes=["MM"])
        s.op("dve", lambda e: e.tensor_scalar_max(out=MM[:], in0=MM[:], scalar1=0.0), reads=["MM"], writes=["MM"])
        s.op("act", lambda e: e.activation(out=MM[:], in_=MM[:], func=AF.Sqrt), reads=["MM"], writes=["MM"])
        if first:
            s.op("dve", lambda e: e.memset(MM[:, :, 0:1], 1.0), reads=["MM"], writes=["MM"])
        s.op("dve", lambda e: e.tensor_tensor(out=BB[:], in0=IG[:], in1=XC[:], op=ALU.mult), reads=["IG", "XC"], writes=["BB"])
        s.op("dve", lambda e: e.tensor_tensor(out=BB[:], in0=BB[:], in1=MM[:], op=ALU.mult), reads=["BB", "MM"], writes=["BB"])
        for c in range(2):
            init = 0.0 if first else HSp[:, c, TT - 1:TT]
            s.op("dve", lambda e, c=c, init=init, HSc=HSc: e.tensor_tensor_scan(out=HSc[:, c, :], data0=AA[:, c, :], data1=BB[:, c, :],
                                                                              initial=init, op0=ALU.mult, op1=ALU.add),
                 reads=["AA", "BB", HSpn], writes=[(HSn, c)])
        s.op("dve", lambda e, O=O, HSc=HSc: e.tensor_tensor(out=O[:], in0=HSc[:], in1=GY[:], op=ALU.mult), reads=[HSn, "GY"], writes=[On])
        evs.append(s.dma("sp", [(oT[:, it * TT:(it + 1) * TT].rearrange("(c p) t -> p c t", p=128), O[:])], "st", reads=[On]))
    s.finish("sp", evs[-1:])
    s.emit()
    return nc


_NC_CACHE = {}


def _get(name, fn):
    if name not in _NC_CACHE:
        _NC_CACHE[name] = fn()
    return _NC_CACHE[name]


def _split_cols(hc):
    aq = np.arange(hc * 128, (hc + 1) * 128)
    base = 4096
    return {"aq": aq, "af": 1024 + aq, "ai": 2048 + aq, "ag": 3072 + aq,
            "bq": base + aq, "bk": base + 1024 + aq, "bv": base + 2048 + aq, "bz": base + 3072 + aq,
            "ba": np.array([base + 4096 + hc]), "bb": np.array([base + 4096 + 8 + hc])}


def _f32(a):
    return np.ascontiguousarray(np.asarray(a, dtype=np.float32))


def kernel(x, p, ln_mix, ln_ffn, ln_ple, ln_final, lb_table, ab_w_in, ab_conv, b_a_log, b_dt_bias, a_gnorm, b_gnorm,
           ab_w_out, c_w_in, c_conv_w, c_conv_b, c_w_r, c_b_r, c_w_i, c_b_i, c_lambda, c_w_out, ffn_w_gate, ffn_w_up,
           ffn_w_down, moe_router, moe_w_gate, moe_w_up, moe_w_down, ple_w_proj, ple_w_gate):
    NCORE = 8
    x = np.asarray(x, np.float32)
    B, S, _ = x.shape
    T = B * S
    NT = T // NCORE
    cores = list(range(NCORE))
    xT = np.ascontiguousarray(x.reshape(T, D).T)
    p = np.asarray(p, np.float32)
    pT = [np.ascontiguousarray(p[l].reshape(T, PLE).T) for l in range(2)]

    w_in = np.asarray(ab_w_in[0], np.float32)
    conv = np.asarray(ab_conv[0], np.float32)
    consts1 = k1_consts()
    gn = _f32(np.broadcast_to(np.stack([np.asarray(a_gnorm[0]), np.asarray(b_gnorm[0])], 0)[None], (64, 2, 128)))
    g_mix0 = vec_layout(ln_mix[0])
    maps = []
    for hc in cores:
        c = _split_cols(hc)
        hs = slice(hc * 128, (hc + 1) * 128)
        cw = np.stack([conv[:, hs].T, conv[:, 1024 + hc * 128:1024 + (hc + 1) * 128].T,
                       conv[:, 2048 + hc * 128:2048 + (hc + 1) * 128].T], 1)
        m = {"xT": xT, "g_mix": g_mix0,
             "w_fm": _f32(w_in[:, np.concatenate([c["aq"], c["af"], c["bq"], c["bk"], c["bv"]])]),
             "w_tok": _f32(w_in[:, np.concatenate([c["ai"], c["ag"], c["bz"]])]),
             "w_ab": _f32(w_in[:, np.concatenate([c["ba"], c["bb"]])]),
             "lbt": _f32(np.asarray(lb_table)[:, hs].T), "convw": _f32(cw),
             "sc2": np.array([[np.asarray(b_a_log)[0, hc], np.asarray(b_dt_bias)[0, hc]]], np.float32), "gn": gn}
        m.update(consts1)
        maps.append(m)
    nc1 = _get(("k1", T, S), lambda: build_k1(T, S))
    r1 = run_bass_kernel_spmd(nc1, maps, core_ids=cores).results
    mixed = np.empty((T, D), np.float32)
    for hc in cores:
        mixed[:, hc * 128:(hc + 1) * 128] = r1[hc]["o"][:, 0:128]
        mixed[:, 1024 + hc * 128:1024 + (hc + 1) * 128] = r1[hc]["o"][:, 128:256]
    mT = np.ascontiguousarray(mixed.T)
    del mixed, r1

    shared2 = {"w_out": _f32(ab_w_out[0]), "wg": _f32(ffn_w_gate[0]), "wu": _f32(ffn_w_up[0]), "wd": _f32(ffn_w_down[0]),
               "wpg": _f32(ple_w_gate[0]), "wpp": _f32(ple_w_proj[0]),
               "g_ffn": vec_layout(ln_ffn[0]), "g_ple": vec_layout(ln_ple[0])}
    maps = []
    for c in cores:
        sl = slice(c * NT, (c + 1) * NT)
        m = {"hT": _f32(xT[:, sl]), "mT": _f32(mT[:, sl]), "pT": _f32(pT[0][:, sl])}
        m.update(shared2)
        maps.append(m)
    nc2 = _get(("k2", NT), lambda: build_k2(NT))
    r2 = run_bass_kernel_spmd(nc2, maps, core_ids=cores).results
    h1T = np.ascontiguousarray(np.concatenate([r2[c]["oT"] for c in cores], axis=1))
    del r2, mT, maps

    cw_in = np.asarray(c_w_in[0], np.float32)
    g_mix1 = vec_layout(ln_mix[1])
    maps = []
    for c in cores:
        sl = slice(c * 256, (c + 1) * 256)
        cv = np.zeros((128, 2, 8), np.float32)

        def pc(v):
            return np.asarray(v, np.float32)[sl].reshape(2, 128).T
        for k in range(4):
            cv[:, :, k] = pc(np.asarray(c_conv_w[0])[k])
        cv[:, :, 4] = pc(c_conv_b[0]); cv[:, :, 5] = pc(c_b_r[0]); cv[:, :, 6] = pc(c_b_i[0]); cv[:, :, 7] = pc(c_lambda[0])
        maps.append({"hT": h1T, "g_mix": g_mix1, "w_y": _f32(cw_in[:, sl]), "w_x": _f32(cw_in[:, D + c * 256:D + (c + 1) * 256]),
                     "w_r": _f32(np.asarray(c_w_r[0])[c]), "w_i": _f32(np.asarray(c_w_i[0])[c]), "cvec": cv})
    nc3 = _get(("k3", T, S), lambda: build_k3(T, S))
    r3 = run_bass_kernel_spmd(nc3, maps, core_ids=cores).results
    gT = np.ascontiguousarray(np.concatenate([r3[c]["oT"] for c in cores], axis=0))
    del r3, maps

    shared4 = {"w_out": _f32(c_w_out[0]), "wg": _f32(moe_w_gate[0]), "wu": _f32(moe_w_up[0]), "wd": _f32(moe_w_down[0]),
               "wr": _f32(np.asarray(moe_router[0], np.float32).reshape(16, 128, 8).transpose(1, 0, 2)),
               "wpg": _f32(ple_w_gate[1]), "wpp": _f32(ple_w_proj[1]),
               "g_ffn": vec_layout(ln_ffn[1]), "g_ple": vec_layout(ln_ple[1]), "g_fin": vec_layout(ln_final)}
    shared4.update(k4_consts())
    maps = []
    for c in cores:
        sl = slice(c * NT, (c + 1) * NT)
        m = {"hT": _f32(h1T[:, sl]), "mT": _f32(gT[:, sl]), "pT": _f32(pT[1][:, sl])}
        m.update(shared4)
        maps.append(m)
    nc4 = _get(("k4", NT), lambda: build_k4(NT))
    r4 = run_bass_kernel_spmd(nc4, maps, core_ids=cores).results
    outT = np.concatenate([r4[c]["oT"] for c in cores], axis=1)
    return np.ascontiguousarray(outT.T).reshape(B, S, D)
```

```python
import contextlib
import numpy as np
import concourse.bass as bass
import concourse.mybir as mybir
from concourse.bass_utils import run_bass_kernel_spmd

F32 = mybir.dt.float32
BF16 = mybir.dt.bfloat16
ALU = mybir.AluOpType
AF = mybir.ActivationFunctionType
AX = mybir.AxisListType


class Sched:
    ENG = ("pe", "act", "dve", "pool", "sp")

    def __init__(self, nc, selfsync=True):
        self.nc = nc
        self.stack = contextlib.ExitStack()
        self.ops = {e: [] for e in self.ENG}
        self.sems = {}
        self.cnt = {}
        self.waited = {e: {} for e in self.ENG}
        self.state = {}
        self.selfsync = selfsync
        self.nwaits = 0
        for e in ("pe", "act", "dve", "pool"):
            self._sem("e_" + e)

    def _sem(self, name):
        if name not in self.sems:
            self.sems[name] = self.stack.enter_context(self.nc.semaphore(name))
            self.cnt[name] = 0
        return self.sems[name]

    def sbuf(self, name, shape, dtype):
        return self.stack.enter_context(self.nc.sbuf_tensor(name, list(shape), dtype))

    def psum(self, name, shape, dtype=F32):
        return self.stack.enter_context(self.nc.psum_tensor(name, list(shape), dtype))

    def _st(self, t, r):
        d = self.state.setdefault(t, {})
        if r not in d:
            d[r] = [None, {}]
        return d[r]

    def _overl(self, t, r):
        d = self.state.get(t, {})
        if r is None:
            return list(d.values())
        return [d[k] for k in (r, None) if k in d]

    def _deps(self, reads, writes):
        ev = {}

        def add(e):
            if e is not None and ev.get(e[0], 0) < e[1]:
                ev[e[0]] = e[1]
        for (t, r) in reads:
            for st in self._overl(t, r):
                add(st[0])
        for (t, r) in writes:
            for st in self._overl(t, r):
                add(st[0])
                for s, v in st[1].items():
                    add((s, v))
        return ev

    def _commit(self, reads, writes, e):
        for (t, r) in reads:
            st = self._st(t, r)
            if st[1].get(e[0], 0) < e[1]:
                st[1][e[0]] = e[1]
        for (t, r) in writes:
            if r is None:
                self.state[t] = {None: [e, {}]}
            else:
                st = self._st(t, r)
                st[0] = e
                st[1] = {}

    def _waits(self, eng, ev):
        w = []
        for s, v in ev.items():
            if s == "e_" + eng and (eng == "pe" or not self.selfsync):
                continue
            if self.waited[eng].get(s, 0) < v:
                self.waited[eng][s] = v
                w.append((s, v))
        self.nwaits += len(w)
        return w

    @staticmethod
    def _norm(keys):
        out = []
        for k in keys:
            if isinstance(k, tuple):
                if k[0].startswith("PS"):
                    out.append((k[0], None))
                else:
                    out.append((k[0], k[1]))
            else:
                out.append((k, None))
        return out

    def op(self, eng, fn, reads=(), writes=()):
        reads, writes = self._norm(reads), self._norm(writes)
        w = self._waits(eng, self._deps(reads, writes))
        s = "e_" + eng
        self.cnt[s] += 1
        e = (s, self.cnt[s])
        self.ops[eng].append((w, fn, [(s, 1)]))
        self._commit(reads, writes, e)
        return e

    def dma(self, q, pairs, chan, reads=(), writes=()):
        reads, writes = self._norm(reads), self._norm(writes)
        w = self._waits(q, self._deps(reads, writes))
        s = "d_" + chan
        self._sem(s)
        for i, (o, a) in enumerate(pairs):
            self.cnt[s] += 16
            self.ops[q].append((w if i == 0 else [], ("dma", o, a), [(s, 16)]))
        e = (s, self.cnt[s])
        self._commit(reads, writes, e)
        return e

    def finish(self, eng, events):
        ev = {}
        for (s, v) in events:
            ev[s] = max(ev.get(s, 0), v)
        self.ops[eng].append((list(ev.items()), None, []))

    def emit(self):
        nc = self.nc
        sems = self.sems

        def run(eng, lst):
            for (w, fn, incs) in lst:
                for (s, v) in w:
                    eng.wait_ge(sems[s], v)
                if fn is None:
                    continue
                if isinstance(fn, tuple):
                    ins = eng.dma_start(out=fn[1], in_=fn[2])
                else:
                    ins = fn(eng)
                for (s, n) in incs:
                    ins.then_inc(sems[s], n)
        with nc.Block() as block:
            @block.tensor
            def _(e):
                run(e, self.ops["pe"])

            @block.scalar
            def _(e):
                run(e, self.ops["act"])

            @block.vector
            def _(e):
                run(e, self.ops["dve"])

            @block.gpsimd
            def _(e):
                run(e, self.ops["pool"])

            @block.sync
            def _(e):
                run(e, self.ops["sp"])
        self.stack.close()


D = 2048
KC = 16
TT = 512
FF = 5632
FC = FF // 128
PLE = 256
EPS = 1e-6


class DenseCore:
    def __init__(self, s):
        self.s = s
        s_ = s
        self.H = s_.sbuf("H", [128, KC, TT], F32)
        self.MT = s_.sbuf("MT", [128, KC, TT], BF16)
        self.UT = s_.sbuf("UT", [128, KC, TT], BF16)
        self.HT = s_.sbuf("HT", [128, FC, TT], BF16)
        self.PT = s_.sbuf("PT", [128, 2, TT], BF16)
        self.WB = [s_.sbuf(f"WB{i}", [128, KC, 256], BF16) for i in range(4)]
        self.WD = [s_.sbuf(f"WD{i}", [128, 11, 512], BF16) for i in range(2)]
        self.WP = [s_.sbuf(f"WP{i}", [128, 2, 256], BF16) for i in range(2)]
        self.RSTD = s_.sbuf("RSTD", [128, TT], F32)
        self.SG = [s_.sbuf(f"SG{i}", [128, TT], F32) for i in range(2)]
        self.TMP = [s_.sbuf(f"TMP{i}", [128, TT], F32) for i in range(2)]
        self.ONES = s_.sbuf("ONES", [128, 128], BF16)
        self.PB = s_.psum("PB", [128, 8, TT])
        s_.op("pool", lambda e: e.memset(self.ONES[:], 1.0), writes=["ONES"])
        self.wb_i = 0
        self.wd_i = 0
        self.wp_i = 0
        self.pb_i = 0
        self.sg_i = 0

    def bank(self):
        b = self.pb_i % 8
        self.pb_i += 1
        return b

    def load_vec(self, name, ap):
        t = self.s.sbuf(name, list(ap.shape), F32)
        self.s.dma("sp", [(t[:], ap)], "c_" + name, writes=[name])
        return t

    def load_tile(self, hT_ap, tok):
        self.s.dma("sp", [(self.H[:], hT_ap[:, tok].rearrange("(kc p) t -> p kc t", p=128))], "h", writes=["H"])

    def load_bf(self, dst, name, src_ap, tok, chan):
        self.s.dma("pool", [(dst[:], src_ap[:, tok].rearrange("(kc p) t -> p kc t", p=128))], chan, writes=[name])

    def store_tile(self, out_ap, tok):
        return self.s.dma("sp", [(out_ap[:, tok].rearrange("(kc p) t -> p kc t", p=128), self.H[:])], "st", reads=["H"])

    def rmsnorm(self, Gname, G):
        s = self.s
        H, SQ, UT, RSTD, ONES, PB = self.H, self.MT, self.UT, self.RSTD, self.ONES, self.PB
        s.op("act", lambda e: e.activation(out=SQ[:], in_=H[:], func=AF.Square), reads=["H"], writes=["MT"])
        b = self.bank()
        for kc in range(KC):
            s.op("pe", lambda e, kc=kc: e.matmul(PB[:, b, :], lhsT=ONES[:], rhs=SQ[:, kc, :], start=(kc == 0), stop=(kc == KC - 1)),
                 reads=["ONES", "MT"], writes=[("PB", b)])
        s.op("act", lambda e: e.activation(out=RSTD[:], in_=PB[:, b, :], func=AF.Sqrt, bias=EPS, scale=1.0 / D),
             reads=[("PB", b)], writes=["RSTD"])
        s.op("dve", lambda e: e.reciprocal(out=RSTD[:], in_=RSTD[:]), reads=["RSTD"], writes=["RSTD"])
        for kc in range(KC):
            s.op("dve", lambda e, kc=kc: e.scalar_tensor_tensor(out=UT[:, kc, :], in0=H[:, kc, :], scalar=G[:, kc:kc + 1],
                                                              in1=RSTD[:], op0=ALU.mult, op1=ALU.mult),
                 reads=[("H", kc), Gname, "RSTD"], writes=[("UT", kc)])

    def load_wb(self, w_ap, cb, chan):
        i = self.wb_i % 4
        self.wb_i += 1
        self.s.dma("pool", [(self.WB[i][:], w_ap[:, cb * 256:(cb + 1) * 256].rearrange("(kc p) c -> p kc c", p=128))],
                   f"wb{i}", writes=[f"WB{i}"])
        return i

    def mm2048(self, bank, wi, j, rhs, rhsname):
        s = self.s
        W, PB = self.WB[wi], self.PB
        for kc in range(KC):
            s.op("pe", lambda e, kc=kc: e.matmul(PB[:, bank, :], lhsT=W[:, kc, j * 128:(j + 1) * 128], rhs=rhs[:, kc, :],
                                               start=(kc == 0), stop=(kc == KC - 1)),
                 reads=[f"WB{wi}", (rhsname, kc)], writes=[("PB", bank)])

    def proj_add(self, w_ap, rhs, rhsname):
        s = self.s
        H, PB = self.H, self.PB
        for cb in range(D // 256):
            wi = self.load_wb(w_ap, cb, "w")
            for j in range(2):
                dc = cb * 2 + j
                b = self.bank()
                self.mm2048(b, wi, j, rhs, rhsname)
                s.op("dve", lambda e, dc=dc, b=b: e.tensor_tensor(out=H[:, dc, :], in0=H[:, dc, :], in1=PB[:, b, :], op=ALU.add),
                     reads=[("H", dc), ("PB", b)], writes=[("H", dc)])

    def ffn(self, wg_ap, wu_ap, wd_ap, gate_bc=None, gate_name=None):
        s = self.s
        H, PB, HT, UT = self.H, self.PB, self.HT, self.UT
        for cb in range(FF // 256):
            wg = self.load_wb(wg_ap, cb, "w")
            wu = self.load_wb(wu_ap, cb, "w")
            for j in range(2):
                fc = cb * 2 + j
                ba, bb = self.bank(), self.bank()
                self.mm2048(ba, wg, j, UT, "UT")
                self.mm2048(bb, wu, j, UT, "UT")
                sg = self.SG[self.sg_i % 2]
                sgn = f"SG{self.sg_i % 2}"
                self.sg_i += 1
                s.op("act", lambda e, sg=sg, ba=ba: e.activation(out=sg[:], in_=PB[:, ba, :], func=AF.Silu),
                     reads=[("PB", ba)], writes=[sgn])
                s.op("dve", lambda e, sg=sg, bb=bb, fc=fc: e.tensor_tensor(out=HT[:, fc, :], in0=sg[:], in1=PB[:, bb, :], op=ALU.mult),
                     reads=[sgn, ("PB", bb)], writes=[("HT", fc)])
        for cb in range(D // 512):
            banks = [self.bank() for _ in range(4)]
            for fg in range(FC // 11):
                i = self.wd_i % 2
                self.wd_i += 1
                WD = self.WD[i]
                s.dma("pool", [(WD[:], wd_ap[fg * 11 * 128:(fg + 1) * 11 * 128, cb * 512:(cb + 1) * 512]
                                .rearrange("(fc p) c -> p fc c", p=128))], f"wd{i}", writes=[f"WD{i}"])
                for jf in range(11):
                    fc = fg * 11 + jf
                    for d4 in range(4):
                        s.op("pe", lambda e, WD=WD, jf=jf, d4=d4, fc=fc, b=banks[d4]: e.matmul(
                            PB[:, b, :], lhsT=WD[:, jf, d4 * 128:(d4 + 1) * 128], rhs=HT[:, fc, :],
                            start=(fc == 0), stop=(fc == FC - 1)),
                            reads=[f"WD{i}", ("HT", fc)], writes=[("PB", banks[d4])])
            for d4 in range(4):
                dc = cb * 4 + d4
                b = banks[d4]
                if gate_bc is None:
                    s.op("dve", lambda e, dc=dc, b=b: e.tensor_tensor(out=H[:, dc, :], in0=H[:, dc, :], in1=PB[:, b, :], op=ALU.add),
                         reads=[("H", dc), ("PB", b)], writes=[("H", dc)])
                else:
                    tmp = self.TMP[d4 % 2]
                    tn = f"TMP{d4 % 2}"
                    s.op("dve", lambda e, tmp=tmp, b=b: e.tensor_tensor(out=tmp[:], in0=PB[:, b, :], in1=gate_bc, op=ALU.mult),
                         reads=[("PB", b), gate_name], writes=[tn])
                    s.op("dve", lambda e, tmp=tmp, dc=dc: e.tensor_tensor(out=H[:, dc, :], in0=H[:, dc, :], in1=tmp[:], op=ALU.add),
                         reads=[("H", dc), tn], writes=[("H", dc)])

    def ple(self, wpg_ap, wpp_ap):
        s = self.s
        H, PB, UT, PT = self.H, self.PB, self.UT, self.PT
        for cb in range(D // 256):
            wi = self.load_wb(wpg_ap, cb, "w")
            ip = self.wp_i % 2
            self.wp_i += 1
            WP = self.WP[ip]
            s.dma("pool", [(WP[:], wpp_ap[:, cb * 256:(cb + 1) * 256].rearrange("(kc p) c -> p kc c", p=128))],
                  f"wp{ip}", writes=[f"WP{ip}"])
            for j in range(2):
                dc = cb * 2 + j
                ba, bb = self.bank(), self.bank()
                self.mm2048(ba, wi, j, UT, "UT")
                for kc in range(2):
                    s.op("pe", lambda e, kc=kc, WP=WP, j=j, bb=bb: e.matmul(PB[:, bb, :], lhsT=WP[:, kc, j * 128:(j + 1) * 128],
                                                                         rhs=PT[:, kc, :], start=(kc == 0), stop=(kc == 1)),
                         reads=[f"WP{ip}", "PT"], writes=[("PB", bb)])
                sg = self.SG[self.sg_i % 2]
                sgn = f"SG{self.sg_i % 2}"
                self.sg_i += 1
                s.op("act", lambda e, sg=sg, ba=ba: e.activation(out=sg[:], in_=PB[:, ba, :], func=AF.Sigmoid),
                     reads=[("PB", ba)], writes=[sgn])
                tmp = self.TMP[j]
                tn = f"TMP{j}"
                s.op("dve", lambda e, tmp=tmp, sg=sg, bb=bb: e.tensor_tensor(out=tmp[:], in0=sg[:], in1=PB[:, bb, :], op=ALU.mult),
                     reads=[sgn, ("PB", bb)], writes=[tn])
                s.op("dve", lambda e, tmp=tmp, dc=dc: e.tensor_tensor(out=H[:, dc, :], in0=H[:, dc, :], in1=tmp[:], op=ALU.add),
                     reads=[("H", dc), tn], writes=[("H", dc)])


def vec_layout(v):
    return np.ascontiguousarray(np.asarray(v, np.float32).reshape(-1, 128).T)


def build_k2(NT):
    nc = bass.Bass("TRN2", target_bir_lowering=False)
    dt = lambda n, sh, kind="ExternalInput": nc.dram_tensor(n, sh, F32, kind=kind).ap()
    hT = dt("hT", [D, NT]); mT = dt("mT", [D, NT]); pT = dt("pT", [PLE, NT])
    w_out = dt("w_out", [D, D]); wg = dt("wg", [D, FF]); wu = dt("wu", [D, FF]); wd = dt("wd", [FF, D])
    wpg = dt("wpg", [D, D]); wpp = dt("wpp", [PLE, D])
    g_ffn = dt("g_ffn", [128, KC]); g_ple = dt("g_ple", [128, KC])
    oT = dt("oT", [D, NT], "ExternalOutput")
    s = Sched(nc)
    c = DenseCore(s)
    Gf = c.load_vec("Gf", g_ffn)
    Gp = c.load_vec("Gp", g_ple)
    evs = []
    for it in range(NT // TT):
        tok = slice(it * TT, (it + 1) * TT)
        c.load_tile(hT, tok)
        c.load_bf(c.MT, "MT", mT, tok, "m")
        c.load_bf(c.PT, "PT", pT, tok, "p")
        c.proj_add(w_out, c.MT, "MT")
        c.rmsnorm("Gf", Gf)
        c.ffn(wg, wu, wd)
        c.rmsnorm("Gp", Gp)
        c.ple(wpg, wpp)
        evs.append(c.store_tile(oT, tok))
    s.finish("sp", evs[-1:])
    s.emit()
    return nc


class MoECore(DenseCore):
    def __init__(self, s, ident_ap, sel_ap, wr_ap):
        super().__init__(s)
        sb = s.sbuf
        self.IDF = sb("IDF", [128, 128], F32)
        self.SEL = sb("SEL", [8, 8, 128], F32)
        self.WR32 = sb("WR32", [128, KC, 8], F32)
        self.WRH = sb("WRH", [128, KC, 8], BF16)
        self.WRL = sb("WRL", [128, KC, 8], BF16)
        self.LG = sb("LG", [128, 4, 8], F32)
        self.M8 = sb("M8", [128, 4, 8], F32)
        self.GT = sb("GT", [128, 4, 8], F32)
        self.SM = sb("SM", [128, 4, 4], F32)
        self.GTT = sb("GTT", [8, TT], F32)
        self.GB = [sb(f"GB{i}", [128, TT], F32) for i in range(2)]
        s.dma("sp", [(self.IDF[:], ident_ap)], "c_id", writes=["IDF"])
        s.dma("sp", [(self.SEL[:], sel_ap)], "c_sel", writes=["SEL"])
        s.dma("sp", [(self.WR32[:], wr_ap)], "c_wr", writes=["WR32"])
        s.op("act", lambda e: e.activation(out=self.WRH[:], in_=self.WR32[:], func=AF.Copy), reads=["WR32"], writes=["WRH"])
        s.op("dve", lambda e: e.tensor_tensor(out=self.WRL[:], in0=self.WR32[:], in1=self.WRH[:], op=ALU.subtract),
             reads=["WR32", "WRH"], writes=["WRL"])
        self.gb_i = 0

    def rmsnorm_hilo(self, Gname, G):
        s = self.s
        H, SQ, UT, RSTD, ONES, PB, HT = self.H, self.MT, self.UT, self.RSTD, self.ONES, self.PB, self.HT
        s.op("act", lambda e: e.activation(out=SQ[:], in_=H[:], func=AF.Square), reads=["H"], writes=["MT"])
        b = self.bank()
        for kc in range(KC):
            s.op("pe", lambda e, kc=kc: e.matmul(PB[:, b, :], lhsT=ONES[:], rhs=SQ[:, kc, :], start=(kc == 0), stop=(kc == KC - 1)),
                 reads=["ONES", "MT"], writes=[("PB", b)])
        s.op("act", lambda e: e.activation(out=RSTD[:], in_=PB[:, b, :], func=AF.Sqrt, bias=EPS, scale=1.0 / D),
             reads=[("PB", b)], writes=["RSTD"])
        s.op("dve", lambda e: e.reciprocal(out=RSTD[:], in_=RSTD[:]), reads=["RSTD"], writes=["RSTD"])
        for kc in range(KC):
            tmp = self.TMP[kc % 2]
            tn = f"TMP{kc % 2}"
            s.op("dve", lambda e, kc=kc, tmp=tmp: e.scalar_tensor_tensor(out=tmp[:], in0=H[:, kc, :], scalar=G[:, kc:kc + 1],
                                                                       in1=RSTD[:], op0=ALU.mult, op1=ALU.mult),
                 reads=[("H", kc), Gname, "RSTD"], writes=[tn])
            s.op("act", lambda e, kc=kc, tmp=tmp: e.activation(out=UT[:, kc, :], in_=tmp[:], func=AF.Copy), reads=[tn], writes=[("UT", kc)])
            s.op("dve", lambda e, kc=kc, tmp=tmp: e.tensor_tensor(out=HT[:, kc, :], in0=tmp[:], in1=UT[:, kc, :], op=ALU.subtract),
                 reads=[tn, ("UT", kc)], writes=[("HT", kc)])

    def router(self):
        s = self.s
        UT, HT, PB, LG, M8, GT, SM, GTT, IDF = self.UT, self.HT, self.PB, self.LG, self.M8, self.GT, self.SM, self.GTT, self.IDF
        WRH, WRL = self.WRH, self.WRL
        b = self.bank()
        for q in range(4):
            ts_ = slice(q * 128, (q + 1) * 128)
            n = 0
            for (a, an, w, wn) in ((UT, "UT", WRH, "WRH"), (HT, "HT", WRH, "WRH"), (UT, "UT", WRL, "WRL")):
                for kc in range(KC):
                    s.op("pe", lambda e, a=a, w=w, kc=kc, ts_=ts_, q=q, n=n: e.matmul(
                        PB[:, b, q * 8:(q + 1) * 8], lhsT=a[:, kc, ts_], rhs=w[:, kc, :], start=(n == 0), stop=(n == 3 * KC - 1)),
                        reads=[(an, kc), wn], writes=[("PB", b)])
                    n += 1
        s.op("act", lambda e: e.activation(out=LG[:], in_=PB[:, b, 0:32].rearrange("p (q e) -> p q e", q=4), func=AF.Copy),
             reads=[("PB", b)], writes=["LG"])
        for q in range(4):
            s.op("dve", lambda e, q=q: e.max(out=M8[:, q, :], in_=LG[:, q, :]), reads=["LG"], writes=[("M8", q)])
            s.op("dve", lambda e, q=q: e.tensor_scalar(out=GT[:, q, :], in0=LG[:, q, :], scalar1=M8[:, q, 1:2], scalar2=None, op0=ALU.is_ge),
                 reads=["LG", ("M8", q)], writes=[("GT", q)])
            s.op("dve", lambda e, q=q: e.tensor_scalar_mul(out=SM[:, q, 0:1], in0=M8[:, q, 0:1], scalar1=-1.0), reads=[("M8", q)], writes=[("SM", q)])
            s.op("act", lambda e, q=q: e.activation(out=LG[:, q, :], in_=LG[:, q, :], func=AF.Exp, bias=SM[:, q, 0:1]),
                 reads=["LG", ("SM", q)], writes=["LG"])
            s.op("dve", lambda e, q=q: e.tensor_tensor(out=GT[:, q, :], in0=GT[:, q, :], in1=LG[:, q, :], op=ALU.mult),
                 reads=["LG", ("GT", q)], writes=[("GT", q)])
            s.op("dve", lambda e, q=q: e.reduce_sum(out=SM[:, q, 1:2], in_=GT[:, q, :], axis=AX.X), reads=[("GT", q)], writes=[("SM", q)])
            s.op("dve", lambda e, q=q: e.reciprocal(out=SM[:, q, 1:2], in_=SM[:, q, 1:2]), reads=[("SM", q)], writes=[("SM", q)])
            s.op("dve", lambda e, q=q: e.tensor_scalar(out=GT[:, q, :], in0=GT[:, q, :], scalar1=SM[:, q, 1:2], scalar2=None, op0=ALU.mult),
                 reads=[("GT", q), ("SM", q)], writes=[("GT", q)])
        b2 = self.bank()
        for q in range(4):
            s.op("pe", lambda e, q=q: e.transpose(PB[0:8, b2, q * 128:(q + 1) * 128], GT[:, q, :], IDF[:]), reads=[("GT", q), "IDF"],
                 writes=[("PB", b2)])
        s.op("act", lambda e: e.activation(out=GTT[:], in_=PB[0:8, b2, :], func=AF.Copy), reads=[("PB", b2)], writes=["GTT"])

    def gate_bc(self, ex):
        s = self.s
        i = self.gb_i % 2
        self.gb_i += 1
        GB, PB = self.GB[i], self.PB
        b = self.bank()
        s.op("pe", lambda e: e.matmul(PB[:, b, :], lhsT=self.SEL[:, ex, :], rhs=self.GTT[:], start=True, stop=True),
             reads=["SEL", "GTT"], writes=[("PB", b)])
        s.op("act", lambda e: e.activation(out=GB[:], in_=PB[:, b, :], func=AF.Copy), reads=[("PB", b)], writes=[f"GB{i}"])
        return GB[:], f"GB{i}"

    def final_norm(self, Gname, G):
        s = self.s
        H, SQ, RSTD, ONES, PB = self.H, self.MT, self.RSTD, self.ONES, self.PB
        s.op("act", lambda e: e.activation(out=SQ[:], in_=H[:], func=AF.Square), reads=["H"], writes=["MT"])
        b = self.bank()
        for kc in range(KC):
            s.op("pe", lambda e, kc=kc: e.matmul(PB[:, b, :], lhsT=ONES[:], rhs=SQ[:, kc, :], start=(kc == 0), stop=(kc == KC - 1)),
                 reads=["ONES", "MT"], writes=[("PB", b)])
        s.op("act", lambda e: e.activation(out=RSTD[:], in_=PB[:, b, :], func=AF.Sqrt, bias=EPS, scale=1.0 / D),
             reads=[("PB", b)], writes=["RSTD"])
        s.op("dve", lambda e: e.reciprocal(out=RSTD[:], in_=RSTD[:]), reads=["RSTD"], writes=["RSTD"])
        for kc in range(KC):
            s.op("dve", lambda e, kc=kc: e.scalar_tensor_tensor(out=H[:, kc, :], in0=H[:, kc, :], scalar=G[:, kc:kc + 1],
                                                              in1=RSTD[:], op0=ALU.mult, op1=ALU.mult),
                 reads=[("H", kc), Gname, "RSTD"], writes=[("H", kc)])


def k4_consts():
    sel = np.zeros((8, 8, 128), np.float32)
    for e in range(8):
        sel[e, e, :] = 1.0
    return {"ident": np.eye(128, dtype=np.float32), "sel": sel}


def build_k4(NT, NEXP=8):
    nc = bass.Bass("TRN2", target_bir_lowering=False)
    dt = lambda n, sh, kind="ExternalInput": nc.dram_tensor(n, sh, F32, kind=kind).ap()
    hT = dt("hT", [D, NT]); mT = dt("mT", [D, NT]); pT = dt("pT", [PLE, NT])
    w_out = dt("w_out", [D, D])
    wg = dt("wg", [NEXP, D, FF]); wu = dt("wu", [NEXP, D, FF]); wd = dt("wd", [NEXP, FF, D])
    wr = dt("wr", [128, KC, 8])
    wpg = dt("wpg", [D, D]); wpp = dt("wpp", [PLE, D])
    g_ffn = dt("g_ffn", [128, KC]); g_ple = dt("g_ple", [128, KC]); g_fin = dt("g_fin", [128, KC])
    ident = dt("ident", [128, 128]); sel = dt("sel", [8, 8, 128])
    oT = dt("oT", [D, NT], "ExternalOutput")
    s = Sched(nc)
    c = MoECore(s, ident, sel, wr)
    Gf = c.load_vec("Gf", g_ffn)
    Gp = c.load_vec("Gp", g_ple)
    Gl = c.load_vec("Gl", g_fin)
    evs = []
    for it in range(NT // TT):
        tok = slice(it * TT, (it + 1) * TT)
        c.load_tile(hT, tok)
        c.load_bf(c.MT, "MT", mT, tok, "m")
        c.load_bf(c.PT, "PT", pT, tok, "p")
        c.proj_add(w_out, c.MT, "MT")
        c.rmsnorm_hilo("Gf", Gf)
        c.router()
        for ex in range(NEXP):
            gb, gbn = c.gate_bc(ex)
            c.ffn(wg[ex], wu[ex], wd[ex], gate_bc=gb, gate_name=gbn)
        c.rmsnorm("Gp", Gp)
        c.ple(wpg, wpp)
        c.final_norm("Gl", Gl)
        evs.append(c.store_tile(oT, tok))
    s.finish("sp", evs[-1:])
    s.emit()
    return nc


D = 2048
KC = 16
TT = 512
NCH = 8
C = 64
EPS = 1e-6
DK = 128
QSCALE = DK ** -0.5


def k1_consts():
    ident = np.eye(128, dtype=np.float32)
    s_idx = np.arange(C)[:, None]
    t_idx = np.arange(C)[None, :]
    m_u = (s_idx <= t_idx).astype(np.float32)
    m_su = (s_idx < t_idx).astype(np.float32)
    m_sl = (s_idx > t_idx).astype(np.float32)
    masks = np.stack([m_su, m_sl, m_u], 1)
    rst = np.ones((128, TT), np.float32)
    rst[:, ::C] = 0.0
    return {"ident": ident, "masks": np.ascontiguousarray(masks), "rst": rst}


def build_k1(NTOK, SEQ):
    nc = bass.Bass("TRN2", target_bir_lowering=False)
    dt = lambda n, sh, kind="ExternalInput": nc.dram_tensor(n, sh, F32, kind=kind).ap()
    xT = dt("xT", [D, NTOK])
    g_mix = dt("g_mix", [128, KC])
    w_fm = dt("w_fm", [D, 640])
    w_tok = dt("w_tok", [D, 384])
    w_ab = dt("w_ab", [D, 2])
    lbt = dt("lbt", [128, 3])
    convw = dt("convw", [128, 3, 4])
    sc2 = dt("sc2", [1, 2])
    gn = dt("gn", [C, 2, 128])
    ident_d = dt("ident", [128, 128])
    masks_d = dt("masks", [C, 3, C])
    rst_d = dt("rst", [128, TT])
    o = dt("o", [NTOK, 256], "ExternalOutput")

    s = Sched(nc)
    sb = s.sbuf
    H = sb("H", [128, KC, TT], F32)
    UT = sb("UT", [128, KC, TT], BF16)
    SQ = UT
    WFM = sb("WFM", [128, KC, 640], BF16)
    WTOK = sb("WTOK", [128, KC, 384], BF16)
    WAB = sb("WAB", [128, KC, 2], BF16)
    G = sb("G", [128, KC], F32)
    LBT = sb("LBT", [128, 3], F32)
    LB = sb("LB", [128, 4], F32)
    CW = sb("CW", [128, 3, 4], F32)
    SC2 = sb("SC2", [1, 2], F32)
    NEA = sb("NEA", [1, 1], F32)
    GN = sb("GN", [C, 2, 128], F32)
    IDF = sb("IDF", [128, 128], F32)
    IDB = sb("IDB", [128, 128], BF16)
    MASKS = sb("MASKS", [C, 3, C], F32)
    RST = sb("RST", [128, TT], F32)
    ONESB = sb("ONESB", [128, 128], BF16)
    ONER = sb("ONER", [1, 128], F32)
    RSTD = sb("RSTD", [128, TT], F32)
    TOK = sb("TOK", [C, NCH, 384], F32)
    TOKP = sb("TOKP", [128, NCH // 2, 384], F32)
    SGT = sb("SGT", [C, NCH, 256], F32)
    AQ = sb("AQ", [128, TT], F32)
    FF_ = sb("FF", [128, TT], F32)
    LOGF = sb("LOGF", [128, TT], F32)
    KA = sb("KA", [128, TT], F32)
    BC = sb("BC", [128, TT], F32)
    EBP = sb("EBP", [128, TT], F32)
    ENB = sb("ENB", [128, TT], F32)
    E2 = sb("E2", [128, TT], F32)
    QE = sb("QE", [128, TT], BF16)
    KE = sb("KE", [128, TT], BF16)
    KE2T = sb("KE2T", [128, TT], BF16)
    KE2A = sb("KE2A", [C, NCH, 128], BF16)
    VA = sb("VA", [C, NCH, 128], BF16)
    STA = sb("STA", [C, NCH, C], BF16)
    SA = sb("SA", [128, 128], F32)
    SAB = sb("SAB", [128, 128], BF16)
    XR = sb("XR", [128, 3, TT + 3], F32)
    XC = sb("XC", [128, 3, TT], F32)
    CS = XC
    SQ2 = sb("SQ2", [128, 2, TT], BF16)
    RS2 = sb("RS2", [128, 2, TT], F32)
    QN = sb("QN", [128, TT], F32)
    KN = sb("KN", [128, TT], F32)
    KNB = sb("KNB", [128, TT], BF16)
    CVB = sb("CVB", [128, TT], BF16)
    EGB = sb("EGB", [128, TT], F32)
    QEB = sb("QEB", [128, TT], BF16)
    QNB = sb("QNB", [128, TT], BF16)
    ROW = sb("ROW", [1, 8, TT], F32)
    R_SP, R_GAM, R_L, R_NGAM, R_EG, R_EGP, R_EGL, R_BETA = range(8)
    R_G = R_SP
    R_GAMP = R_L
    EE = [sb(f"EE{i}", [C, 3, C], F32) for i in range(2)]
    M_ = [[sb(f"M{p}_{i}", [C, C], BF16) for i in range(2)] for p in range(2)]
    N_ = [[sb(f"N{p}_{i}", [C, C], BF16) for i in range(2)] for p in range(2)]
    R_ = [[sb(f"R{p}_{i}", [C, C], BF16) for i in range(2)] for p in range(2)]
    TTB = [sb(f"TTB{i}", [C, C], BF16) for i in range(2)]
    QKT = [sb(f"QKT{i}", [C, C], BF16) for i in range(4)]
    COL = [sb(f"COL{i}", [C, 3], F32) for i in range(2)]
    XK = [sb(f"XK{i}", [C, 128], BF16) for i in range(2)]
    BV = [sb(f"BV{i}", [C, 128], BF16) for i in range(2)]
    KE2B = [sb(f"KE2B{i}", [C, 128], BF16) for i in range(4)]
    WTB = [sb(f"WTB{i}", [128, C], BF16) for i in range(4)]
    USB = [sb(f"USB{i}", [C, 128], F32) for i in range(4)]
    VN = sb("VN", [C, 128], BF16)
    SB_ = sb("SB", [128, 128], F32)
    SBB = sb("SBB", [128, 128], BF16)
    OUTT = sb("OUTT", [C, NCH, 256], F32)
    SQO = [sb(f"SQO{i}", [C, 128], F32) for i in range(2)]
    SSO = [sb(f"SSO{i}", [C, 2], F32) for i in range(2)]
    TMPO = [sb(f"TMPO{i}", [C, 128], F32) for i in range(2)]

    PS = [s.psum(f"PS{i}", [128, 512], F32) for i in range(5)] + [s.psum("PS5", [128, 1024], BF16)] + \
         [s.psum(f"PS{i}", [128, 512], F32) for i in (6, 7)]

    op = s.op
    s.dma("sp", [(G[:], g_mix)], "c0", writes=["G"])
    s.dma("sp", [(LBT[:], lbt)], "c1", writes=["LBT"])
    s.dma("sp", [(CW[:], convw)], "c2", writes=["CW"])
    s.dma("sp", [(SC2[:], sc2)], "c3", writes=["SC2"])
    s.dma("sp", [(GN[:], gn)], "c4", writes=["GN"])
    s.dma("sp", [(IDF[:], ident_d)], "c5", writes=["IDF"])
    s.dma("sp", [(MASKS[:], masks_d)], "c6", writes=["MASKS"])
    s.dma("sp", [(RST[:], rst_d)], "c7", writes=["RST"])
    s.dma("pool", [(IDB[:], ident_d)], "c8", writes=["IDB"])
    s.dma("pool", [(WFM[:], w_fm.rearrange("(kc p) c -> p kc c", p=128))], "c9", writes=["WFM"])
    s.dma("pool", [(WTOK[:], w_tok.rearrange("(kc p) c -> p kc c", p=128))], "c10", writes=["WTOK"])
    s.dma("pool", [(WAB[:], w_ab.rearrange("(kc p) c -> p kc c", p=128))], "c11", writes=["WAB"])
    op("pool", lambda e: e.memset(ONESB[:], 1.0), writes=["ONESB"])
    op("pool", lambda e: e.memset(ONER[:], 1.0), writes=["ONER"])
    op("act", lambda e: e.activation(out=LBT[:], in_=LBT[:], func=AF.Exp), reads=["LBT"], writes=["LBT"])
    op("dve", lambda e: e.reduce_sum(out=LB[:, 2:3], in_=LBT[:], axis=AX.X), reads=["LBT"], writes=["LB"])
    op("dve", lambda e: e.reciprocal(out=LB[:, 2:3], in_=LB[:, 2:3]), reads=["LB"], writes=["LB"])
    op("dve", lambda e: e.tensor_tensor(out=LB[:, 0:1], in0=LBT[:, 0:1], in1=LB[:, 2:3], op=ALU.mult), reads=["LB", "LBT"], writes=["LB"])
    op("dve", lambda e: e.tensor_scalar(out=LB[:, 1:2], in0=LB[:, 0:1], scalar1=-1.0, scalar2=1.0, op0=ALU.mult, op1=ALU.add),
       reads=["LB"], writes=["LB"])
    op("act", lambda e: e.activation(out=NEA[:], in_=SC2[:, 0:1], func=AF.Exp), reads=["SC2"], writes=["NEA"])
    op("dve", lambda e: e.tensor_scalar_mul(out=NEA[:], in0=NEA[:], scalar1=-1.0), reads=["NEA"], writes=["NEA"])

    ntile = NTOK // TT
    tpb = SEQ // TT
    big_i = [0]

    def big():
        b = big_i[0] % 2
        big_i[0] += 1
        return b

    evs = []
    for it in range(ntile):
        first = (it % tpb == 0)
        tok0 = it * TT
        s.dma("sp", [(H[:], xT[:, tok0:tok0 + TT].rearrange("(kc p) t -> p kc t", p=128))], "h", writes=["H"])
        op("act", lambda e: e.activation(out=SQ[:], in_=H[:], func=AF.Square), reads=["H"], writes=["UT"])
        b = big()
        for kc in range(KC):
            op("pe", lambda e, kc=kc, b=b: e.matmul(PS[b][:], lhsT=ONESB[:], rhs=SQ[:, kc, :], start=(kc == 0), stop=(kc == KC - 1)),
               reads=["ONESB", "UT"], writes=[f"PS{b}"])
        op("act", lambda e, b=b: e.activation(out=RSTD[:], in_=PS[b][:], func=AF.Sqrt, bias=EPS, scale=1.0 / D),
           reads=[f"PS{b}"], writes=["RSTD"])
        op("dve", lambda e: e.reciprocal(out=RSTD[:], in_=RSTD[:]), reads=["RSTD"], writes=["RSTD"])
        for kc in range(KC):
            op("dve", lambda e, kc=kc: e.scalar_tensor_tensor(out=UT[:, kc, :], in0=H[:, kc, :], scalar=G[:, kc:kc + 1],
                                                            in1=RSTD[:], op0=ALU.mult, op1=ALU.mult),
               reads=["H", "G", "RSTD"], writes=[("UT", kc)])
        if first:
            op("pool", lambda e: e.memset(XR[:, :, 0:3], 0.0), writes=["XR"])
            op("pool", lambda e: e.memset(SA[:], 0.0), writes=["SA"])
            op("pool", lambda e: e.memset(SAB[:], 0.0), writes=["SAB"])
            op("pool", lambda e: e.memset(SB_[:], 0.0), writes=["SB"])
            op("pool", lambda e: e.memset(SBB[:], 0.0), writes=["SBB"])

        def fm_proj(col, M, b):
            for kc in range(KC):
                op("pe", lambda e, kc=kc: e.matmul(PS[b][0:M, :], lhsT=(WFM[:, kc, col * 128:(col + 1) * 128] if M == 128 else WAB[:, kc, col:col + 1]),
                                                 rhs=UT[:, kc, :], start=(kc == 0), stop=(kc == KC - 1)),
                   reads=["WFM", "WAB", ("UT", kc)], writes=[f"PS{b}"])
        b = big(); fm_proj(0, 128, b)
        op("act", lambda e, b=b: e.activation(out=AQ[:], in_=PS[b][:], func=AF.Copy), reads=[f"PS{b}"], writes=["AQ"])
        b = big(); fm_proj(1, 128, b)
        op("act", lambda e, b=b: e.activation(out=FF_[:], in_=PS[b][:], func=AF.Sigmoid), reads=[f"PS{b}"], writes=["FF"])
        for i in range(3):
            b = big(); fm_proj(2 + i, 128, b)
            op("act", lambda e, b=b, i=i: e.activation(out=XR[:, i, 3:TT + 3], in_=PS[b][:], func=AF.Copy), reads=[f"PS{b}"], writes=["XR"])
        b = big(); fm_proj(0, 1, b)
        op("act", lambda e, b=b: e.activation(out=ROW[:, R_SP, :], in_=PS[b][0:1, :], func=AF.Exp, bias=SC2[:, 1:2]),
           reads=[f"PS{b}", "SC2"], writes=[("ROW", R_SP)])
        op("act", lambda e: e.activation(out=ROW[:, R_SP, :], in_=ROW[:, R_SP, :], func=AF.Ln, bias=1.0),
           reads=[("ROW", R_SP)], writes=[("ROW", R_SP)])
        op("dve", lambda e: e.tensor_scalar(out=ROW[:, R_G, :], in0=ROW[:, R_SP, :], scalar1=NEA[:, 0:1], scalar2=None, op0=ALU.mult),
           reads=[("ROW", R_SP), "NEA"], writes=[("ROW", R_G)])
        b = big(); fm_proj(1, 1, b)
        op("act", lambda e, b=b: e.activation(out=ROW[:, R_BETA, :], in_=PS[b][0:1, :], func=AF.Sigmoid),
           reads=[f"PS{b}"], writes=[("ROW", R_BETA)])
        op("act", lambda e, b=b: e.activation(out=ROW[:, R_L, :], in_=PS[b][0:1, :], func=AF.Exp, scale=-1.0),
           reads=[f"PS{b}"], writes=[("ROW", R_L)])
        op("act", lambda e: e.activation(out=ROW[:, R_L, :], in_=ROW[:, R_L, :], func=AF.Ln, bias=1.0),
           reads=[("ROW", R_L)], writes=[("ROW", R_L)])
        for pr in range(NCH // 2):
            pb = 2 + (pr % 2)
            for kc in range(KC):
                op("pe", lambda e, kc=kc, pr=pr, pb=pb: e.matmul(PS[pb][:, 0:384], lhsT=UT[:, kc, pr * 128:(pr + 1) * 128], rhs=WTOK[:, kc, :],
                                                              start=(kc == 0), stop=(kc == KC - 1)),
                   reads=["WTOK", ("UT", kc)], writes=[f"PS{pb}"])
            op("act", lambda e, pr=pr, pb=pb: e.activation(out=TOKP[:, pr, :], in_=PS[pb][:, 0:384], func=AF.Copy),
               reads=[f"PS{pb}"], writes=[("TOKP", pr)])
        tokv = TOK[:].rearrange("p (a b) c -> p a b c", b=2)
        op("act", lambda e: e.activation(out=tokv[:, :, 0, :], in_=TOKP[0:C, :, :], func=AF.Copy), reads=["TOKP"], writes=[("TOK", "even")])
        s.dma("sp", [(tokv[:, :, 1, :], TOKP[C:128, :, :])], "tk", reads=["TOKP"], writes=[("TOK", "odd")])
        op("act", lambda e: e.activation(out=SGT[:], in_=TOK[:, :, 128:384], func=AF.Silu), reads=["TOK"], writes=["SGT"])
        op("pool", lambda e: e.tensor_copy(out=VA[:], in_=TOK[:, :, 0:128]), reads=["TOK"], writes=["VA"])

        op("dve", lambda e: e.tensor_scalar(out=FF_[:], in0=FF_[:], scalar1=LB[:, 1:2], scalar2=LB[:, 0:1], op0=ALU.mult, op1=ALU.add),
           reads=["FF", "LB"], writes=["FF"])
        op("act", lambda e: e.activation(out=LOGF[:], in_=FF_[:], func=AF.Ln), reads=["FF"], writes=["LOGF"])
        op("dve", lambda e: e.tensor_scalar(out=KA[:], in0=FF_[:], scalar1=-1.0, scalar2=1.0, op0=ALU.mult, op1=ALU.add),
           reads=["FF"], writes=["KA"])
        op("dve", lambda e: e.tensor_tensor_scan(out=BC[:], data0=RST[:], data1=LOGF[:], initial=0.0, op0=ALU.mult, op1=ALU.add),
           reads=["RST", "LOGF"], writes=["BC"])
        op("act", lambda e: e.activation(out=EBP[:], in_=BC[:], func=AF.Exp), reads=["BC"], writes=["EBP"])
        op("act", lambda e: e.activation(out=ENB[:], in_=BC[:], func=AF.Exp, scale=-1.0), reads=["BC"], writes=["ENB"])
        for j in range(NCH):
            op("act", lambda e, j=j: e.activation(out=E2[:, j * C:(j + 1) * C], in_=BC[:, j * C:(j + 1) * C], func=AF.Exp, scale=-1.0,
                                                bias=BC[:, (j + 1) * C - 1:(j + 1) * C]), reads=["BC"], writes=["E2"])
        op("dve", lambda e: e.scalar_tensor_tensor(out=QE[:], in0=AQ[:], scalar=QSCALE, in1=EBP[:], op0=ALU.mult, op1=ALU.mult),
           reads=["AQ", "EBP"], writes=["QE"])
        op("dve", lambda e: e.tensor_tensor(out=KE[:], in0=KA[:], in1=ENB[:], op=ALU.mult), reads=["KA", "ENB"], writes=["KE"])
        op("dve", lambda e: e.tensor_tensor(out=KE2T[:], in0=KA[:], in1=E2[:], op=ALU.mult), reads=["KA", "E2"], writes=["KE2T"])

        for i in range(3):
            op("dve", lambda e, i=i: e.tensor_scalar(out=XC[:, i, :], in0=XR[:, i, 3:TT + 3], scalar1=CW[:, i, 3:4], scalar2=None, op0=ALU.mult),
               reads=["XR", "CW"], writes=[("XC", i)])
            for k in range(3):
                op("dve", lambda e, i=i, k=k: e.scalar_tensor_tensor(out=XC[:, i, :], in0=XR[:, i, k:TT + k], scalar=CW[:, i, k:k + 1],
                                                                    in1=XC[:, i, :], op0=ALU.mult, op1=ALU.add),
                   reads=["XR", "CW", ("XC", i)], writes=[("XC", i)])
        op("pool", lambda e: e.tensor_copy(out=XR[:, :, 0:3], in_=XR[:, :, TT:TT + 3]), reads=["XR", "XC"], writes=["XR"])
        op("act", lambda e: e.activation(out=CS[:], in_=XC[:], func=AF.Silu), reads=["XC"], writes=["XC"])
        op("act", lambda e: e.activation(out=SQ2[:], in_=CS[:, 0:2, :], func=AF.Square), reads=["XC"], writes=["SQ2"])
        for i in range(2):
            b = big()
            op("pe", lambda e, i=i, b=b: e.matmul(PS[b][:], lhsT=ONESB[:], rhs=SQ2[:, i, :], start=True, stop=True),
               reads=["ONESB", "SQ2"], writes=[f"PS{b}"])
            op("act", lambda e, i=i, b=b: e.activation(out=RS2[:, i, :], in_=PS[b][:], func=AF.Sqrt, bias=EPS), reads=[f"PS{b}"], writes=[("RS2", i)])
        op("dve", lambda e: e.reciprocal(out=RS2[:], in_=RS2[:]), reads=["RS2"], writes=["RS2"])
        op("dve", lambda e: e.scalar_tensor_tensor(out=QN[:], in0=CS[:, 0, :], scalar=QSCALE, in1=RS2[:, 0, :], op0=ALU.mult, op1=ALU.mult),
           reads=["XC", "RS2"], writes=["QN"])
        op("dve", lambda e: e.tensor_tensor(out=KN[:], in0=CS[:, 1, :], in1=RS2[:, 1, :], op=ALU.mult), reads=["XC", "RS2"], writes=["KN"])
        op("act", lambda e: e.activation(out=KNB[:], in_=KN[:], func=AF.Copy), reads=["KN"], writes=["KNB"])
        op("act", lambda e: e.activation(out=QNB[:], in_=QN[:], func=AF.Copy), reads=["QN"], writes=["QNB"])
        op("act", lambda e: e.activation(out=CVB[:], in_=CS[:, 2, :], func=AF.Copy), reads=["XC"], writes=["CVB"])
        op("dve", lambda e: e.tensor_tensor_scan(out=ROW[:, R_GAM, :], data0=RST[0:1, :], data1=ROW[:, R_G, :], initial=0.0,
                                                 op0=ALU.mult, op1=ALU.add), reads=["RST", ("ROW", R_G)], writes=[("ROW", R_GAM)])
        op("dve", lambda e: e.tensor_tensor(out=ROW[:, R_GAMP, :], in0=ROW[:, R_GAM, :], in1=ROW[:, R_L, :], op=ALU.subtract),
           reads=[("ROW", R_GAM), ("ROW", R_L)], writes=[("ROW", R_GAMP)])
        op("dve", lambda e: e.tensor_scalar_mul(out=ROW[:, R_NGAM, :], in0=ROW[:, R_GAM, :], scalar1=-1.0),
           reads=[("ROW", R_GAM)], writes=[("ROW", R_NGAM)])
        op("act", lambda e: e.activation(out=ROW[:, R_EG, :], in_=ROW[:, R_GAM, :], func=AF.Exp), reads=[("ROW", R_GAM)], writes=[("ROW", R_EG)])
        op("act", lambda e: e.activation(out=ROW[:, R_EGP, :], in_=ROW[:, R_GAMP, :], func=AF.Exp), reads=[("ROW", R_GAMP)], writes=[("ROW", R_EGP)])
        for j in range(NCH):
            op("act", lambda e, j=j: e.activation(out=ROW[:, R_EGL, j * C:(j + 1) * C], in_=ROW[:, R_GAM, j * C:(j + 1) * C], func=AF.Exp,
                                                scale=-1.0, bias=ROW[:, R_GAM, (j + 1) * C - 1:(j + 1) * C]),
               reads=[("ROW", R_GAM)], writes=[("ROW", R_EGL)])
        b = big()
        op("pe", lambda e, b=b: e.matmul(PS[b][:], lhsT=ONER[0:1, :], rhs=ROW[:, R_EG, :], start=True, stop=True),
           reads=["ONER", ("ROW", R_EG)], writes=[f"PS{b}"])
        op("act", lambda e, b=b: e.activation(out=EGB[:], in_=PS[b][:], func=AF.Copy), reads=[f"PS{b}"], writes=["EGB"])
        op("dve", lambda e: e.tensor_tensor(out=QEB[:], in0=QN[:], in1=EGB[:], op=ALU.mult), reads=["QN", "EGB"], writes=["QEB"])

        P2, P3, P4, P5, P6, P7 = PS[2], PS[3], PS[4], PS[5], PS[6], PS[7]

        def pre_ops(j):
            L = []
            add = lambda eng, fn, r=(), w=(): L.append((eng, fn, r, w))
            cs = slice(j * C, (j + 1) * C)
            jb = j % 4
            pb = j % 2
            PX = P4 if pb == 0 else PS[1]
            PXn = 'PS4' if pb == 0 else 'PS1'
            P5o = pb * 384
            P6o = pb * 256
            add("pe", lambda e: e.transpose(P5[0:C, P5o:P5o + 128], KE2T[:, cs], IDB[:]), ["KE2T", "IDB"], ["PS5"])
            add("act", lambda e: e.activation(out=KE2A[:, j, :], in_=P5[0:C, P5o:P5o + 128], func=AF.Copy), ["PS5"], [("KE2A", j)])
            add("pe", lambda e: e.matmul(P6[0:C, P6o + 192:P6o + 256], lhsT=KE[:, cs], rhs=QE[:, cs], start=True, stop=True), ["KE", "QE"], ["PS6"])
            add("dve", lambda e: e.tensor_tensor(out=STA[:, j, :], in0=P6[0:C, P6o + 192:P6o + 256], in1=MASKS[:, 2, :], op=ALU.mult),
                ["PS6", "MASKS"], [("STA", j)])
            add("pe", lambda e: e.matmul(PX[0:C, 0:64], lhsT=KNB[:, cs], rhs=KNB[:, cs], start=True, stop=True), ["KNB"], [PXn])
            add("pe", lambda e: e.matmul(PX[0:C, 64:128], lhsT=KNB[:, cs], rhs=QNB[:, cs], start=True, stop=True), ["KNB", "QNB"], [PXn])
            add("pe", lambda e: e.matmul(PX[0:C, 128:192], lhsT=ONER[0:1, 0:C], rhs=ROW[:, R_GAMP, cs], start=True, stop=False),
                ["ONER", ("ROW", R_GAMP)], [PXn])
            add("pe", lambda e: e.matmul(PX[0:C, 128:192], lhsT=ROW[:, R_NGAM, cs], rhs=ONER[0:1, 0:C], start=False, stop=True),
                ["ONER", ("ROW", R_NGAM)], [PXn])
            add("pe", lambda e: e.matmul(PX[0:C, 192:256], lhsT=ROW[:, R_GAMP, cs], rhs=ONER[0:1, 0:C], start=True, stop=False),
                ["ONER", ("ROW", R_GAMP)], [PXn])
            add("pe", lambda e: e.matmul(PX[0:C, 192:256], lhsT=ONER[0:1, 0:C], rhs=ROW[:, R_NGAM, cs], start=False, stop=True),
                ["ONER", ("ROW", R_NGAM)], [PXn])
            add("pe", lambda e: e.matmul(PX[0:C, 256:320], lhsT=ONER[0:1, 0:C], rhs=ROW[:, R_GAM, cs], start=True, stop=False),
                ["ONER", ("ROW", R_GAM)], [PXn])
            add("pe", lambda e: e.matmul(PX[0:C, 256:320], lhsT=ROW[:, R_NGAM, cs], rhs=ONER[0:1, 0:C], start=False, stop=True),
                ["ONER", ("ROW", R_NGAM)], [PXn])
            add("dve", lambda e: e.tensor_scalar_min(out=EE[pb][:], in0=PX[0:C, 128:320].rearrange("p (a b) -> p a b", a=3), scalar1=0.0),
                [PXn], [f"EE{pb}"])
            add("act", lambda e: e.activation(out=EE[pb][:], in_=EE[pb][:], func=AF.Exp), [f"EE{pb}"], [f"EE{pb}"])
            add("dve", lambda e: e.tensor_tensor(out=EE[pb][:], in0=EE[pb][:], in1=MASKS[:], op=ALU.mult), [f"EE{pb}", "MASKS"], [f"EE{pb}"])
            add("dve", lambda e: e.scalar_tensor_tensor(out=M_[pb][0][:], in0=PX[0:C, 0:64], scalar=-1.0, in1=EE[pb][:, 0, :], op0=ALU.mult, op1=ALU.mult),
                [PXn, f"EE{pb}"], [f"M{pb}_0"])
            add("dve", lambda e: e.scalar_tensor_tensor(out=N_[pb][0][:], in0=PX[0:C, 0:64], scalar=-1.0, in1=EE[pb][:, 1, :], op0=ALU.mult, op1=ALU.mult),
                [PXn, f"EE{pb}"], [f"N{pb}_0"])
            add("dve", lambda e: e.tensor_tensor(out=QKT[jb][:], in0=PX[0:C, 64:128], in1=EE[pb][:, 2, :], op=ALU.mult), [PXn, f"EE{pb}"], [f"QKT{jb}"])
            add("dve", lambda e: e.tensor_tensor(out=R_[pb][0][:], in0=M_[pb][0][:], in1=IDB[0:C, 0:C], op=ALU.add), [f"M{pb}_0", "IDB"], [f"R{pb}_0"])
            cur = 0
            for lvl in range(1, 6):
                nx = 1 - cur
                lastl = (lvl == 5)
                if not lastl:
                    add("pe", lambda e, cur=cur: e.matmul(PX[0:C, 320:384], lhsT=N_[pb][cur][:], rhs=M_[pb][cur][:], start=True, stop=True),
                        [f"N{pb}_{cur}", f"M{pb}_{cur}"], [PXn])
                add("pe", lambda e, cur=cur: e.matmul(PX[0:C, 384:448], lhsT=M_[pb][cur][:], rhs=N_[pb][cur][:], start=True, stop=True),
                    [f"N{pb}_{cur}", f"M{pb}_{cur}"], [PXn])
                if not lastl:
                    add("act", lambda e, nx=nx: e.activation(out=M_[pb][nx][:], in_=PX[0:C, 320:384], func=AF.Copy), [PXn], [f"M{pb}_{nx}"])
                add("act", lambda e, nx=nx: e.activation(out=N_[pb][nx][:], in_=PX[0:C, 384:448], func=AF.Copy), [PXn], [f"N{pb}_{nx}"])
                add("pe", lambda e, cur=cur, nx=nx: e.matmul(PX[0:C, 448:512], lhsT=N_[pb][nx][:], rhs=R_[pb][cur][:], start=True, stop=True),
                    [f"N{pb}_{nx}", f"R{pb}_{cur}"], [PXn])
                if lastl:
                    add("dve", lambda e, cur=cur: e.tensor_tensor(out=TTB[pb][:], in0=R_[pb][cur][:], in1=PX[0:C, 448:512], op=ALU.add),
                        [f"R{pb}_{cur}", PXn], [f"TTB{pb}"])
                else:
                    add("dve", lambda e, cur=cur, nx=nx: e.tensor_tensor(out=R_[pb][nx][:], in0=R_[pb][cur][:], in1=PX[0:C, 448:512], op=ALU.add),
                        [f"R{pb}_{cur}", PXn], [f"R{pb}_{nx}"])
                cur = nx
            add("pe", lambda e: e.transpose(P5[0:C, P5o + 128:P5o + 256], KNB[:, cs], IDB[:]), ["KNB", "IDB"], ["PS5"])
            add("pe", lambda e: e.transpose(P5[0:C, P5o + 256:P5o + 384], CVB[:, cs], IDB[:]), ["CVB", "IDB"], ["PS5"])
            for ci, rr in enumerate((R_EGP, R_BETA, R_EGL)):
                add("pe", lambda e, ci=ci, rr=rr: e.matmul(P3[0:C, 128 + pb * 4 + ci:129 + pb * 4 + ci], lhsT=ROW[:, rr, cs], rhs=ONER[0:1, 0:1], start=True, stop=True),
                    [("ROW", rr), "ONER"], ["PS3"])
            add("act", lambda e: e.activation(out=COL[pb][:], in_=P3[0:C, 128 + pb * 4:131 + pb * 4], func=AF.Copy), ["PS3"], [f"COL{pb}"])
            add("dve", lambda e: e.tensor_scalar(out=XK[pb][:], in0=P5[0:C, P5o + 128:P5o + 256], scalar1=COL[pb][:, 0:1], scalar2=None, op0=ALU.mult),
                ["PS5", f"COL{pb}"], [f"XK{pb}"])
            add("dve", lambda e: e.tensor_scalar(out=KE2B[jb][:], in0=P5[0:C, P5o + 128:P5o + 256], scalar1=COL[pb][:, 2:3], scalar2=None, op0=ALU.mult),
                ["PS5", f"COL{pb}"], [f"KE2B{jb}"])
            add("dve", lambda e: e.tensor_scalar(out=BV[pb][:], in0=P5[0:C, P5o + 256:P5o + 384], scalar1=COL[pb][:, 1:2], scalar2=None, op0=ALU.mult),
                ["PS5", f"COL{pb}"], [f"BV{pb}"])
            add("pe", lambda e: e.matmul(P6[:, P6o:P6o + 64], lhsT=XK[pb][:], rhs=TTB[pb][:], start=True, stop=True), [f"XK{pb}", f"TTB{pb}"], ["PS6"])
            add("act", lambda e: e.activation(out=WTB[jb][:], in_=P6[:, P6o:P6o + 64], func=AF.Copy), ["PS6"], [f"WTB{jb}"])
            add("pe", lambda e: e.matmul(P6[0:C, P6o + 64:P6o + 192], lhsT=TTB[pb][:], rhs=BV[pb][:], start=True, stop=True), [f"BV{pb}", f"TTB{pb}"], ["PS6"])
            add("act", lambda e: e.activation(out=USB[jb][:], in_=P6[0:C, P6o + 64:P6o + 192], func=AF.Copy), ["PS6"], [f"USB{jb}"])
            return L

        def chain_ops(j):
            L = []
            add = lambda eng, fn, r=(), w=(): L.append((eng, fn, r, w))
            cs = slice(j * C, (j + 1) * C)
            last = slice((j + 1) * C - 1, (j + 1) * C)
            jb = j % 4

            def out_norm(ps_ap, pskey, gi):
                add("act", lambda e: e.activation(out=SQO[gi][:], in_=ps_ap, func=AF.Square), [pskey], [f"SQO{gi}"])
                add("dve", lambda e: e.reduce_sum(out=SSO[gi][:, 0:1], in_=SQO[gi][:], axis=AX.X), [f"SQO{gi}"], [f"SSO{gi}"])
                add("act", lambda e: e.activation(out=SSO[gi][:, 1:2], in_=SSO[gi][:, 0:1], func=AF.Sqrt, bias=EPS, scale=1.0 / 128),
                    [f"SSO{gi}"], [f"SSO{gi}"])
                add("dve", lambda e: e.reciprocal(out=SSO[gi][:, 1:2], in_=SSO[gi][:, 1:2]), [f"SSO{gi}"], [f"SSO{gi}"])
                add("dve", lambda e: e.scalar_tensor_tensor(out=TMPO[gi][:], in0=ps_ap, scalar=SSO[gi][:, 1:2], in1=GN[:, gi, :],
                                                            op0=ALU.mult, op1=ALU.mult), [pskey, f"SSO{gi}", "GN"], [f"TMPO{gi}"])
                add("dve", lambda e: e.tensor_tensor(out=OUTT[:, j, gi * 128:(gi + 1) * 128], in0=TMPO[gi][:], in1=SGT[:, j, gi * 128:(gi + 1) * 128],
                                                     op=ALU.mult), [f"TMPO{gi}", "SGT"], [("OUTT", (j, gi))])
            add("pe", lambda e: e.matmul(P7[0:C, 0:128], lhsT=WTB[jb][:], rhs=SBB[:], start=True, stop=True), [f"WTB{jb}", "SBB"], ["PS7"])
            add("dve", lambda e: e.tensor_tensor(out=VN[:], in0=USB[jb][:], in1=P7[0:C, 0:128], op=ALU.subtract), [f"USB{jb}", "PS7"], ["VN"])
            add("pe", lambda e: e.matmul(P3[:, 0:128], lhsT=KE2A[:, j, :], rhs=VA[:, j, :], start=True, stop=True), [("KE2A", j), "VA"], ["PS3"])
            add("pe", lambda e: e.matmul(P2[0:C, 0:128], lhsT=STA[:, j, :], rhs=VA[:, j, :], start=True, stop=False), [("STA", j), "VA"], ["PS2"])
            add("pe", lambda e: e.matmul(P2[0:C, 0:128], lhsT=QE[:, cs], rhs=SAB[:], start=False, stop=True), ["QE", "SAB"], ["PS2"])
            add("dve", lambda e: e.scalar_tensor_tensor(out=SA[:], in0=SA[:], scalar=EBP[:, last], in1=P3[:, 0:128], op0=ALU.mult, op1=ALU.add),
                ["SA", "EBP", "PS3"], ["SA"])
            add("act", lambda e: e.activation(out=SAB[:], in_=SA[:], func=AF.Copy), ["SA"], ["SAB"])
            add("pe", lambda e: e.matmul(P7[0:C, 128:256], lhsT=QEB[:, cs], rhs=SBB[:], start=True, stop=False), ["QEB", "SBB"], ["PS7"])
            add("pe", lambda e: e.matmul(P7[0:C, 128:256], lhsT=QKT[jb][:], rhs=VN[:], start=False, stop=True), [f"QKT{jb}", "VN"], ["PS7"])
            add("pe", lambda e: e.matmul(P7[:, 256:384], lhsT=KE2B[jb][:], rhs=VN[:], start=True, stop=True), [f"KE2B{jb}", "VN"], ["PS7"])
            add("dve", lambda e: e.scalar_tensor_tensor(out=SB_[:], in0=SB_[:], scalar=EGB[:, last], in1=P7[:, 256:384], op0=ALU.mult, op1=ALU.add),
                ["SB", "EGB", "PS7"], ["SB"])
            add("act", lambda e: e.activation(out=SBB[:], in_=SB_[:], func=AF.Copy), ["SB"], ["SBB"])
            out_norm(P2[0:C, 0:128], "PS2", 0)
            out_norm(P7[0:C, 128:256], "PS7", 1)
            return L

        def submit(lst):
            for (eng, fn, r, w) in lst:
                op(eng, fn, reads=r, writes=w)

        def merge(a, b):
            out = []
            ia = ib = 0
            na, nb = len(a), len(b)
            while ia < na or ib < nb:
                if ib >= nb or (ia < na and ia * nb <= ib * na):
                    out.append(a[ia]); ia += 1
                else:
                    out.append(b[ib]); ib += 1
            return out

        submit(merge(pre_ops(0), pre_ops(1)))
        for j in range(0, NCH, 2):
            nxt = merge(pre_ops(j + 2), pre_ops(j + 3)) if j + 3 < NCH else []
            submit(merge(nxt, chain_ops(j) + chain_ops(j + 1)))
        evs.append(s.dma("sp", [(o[tok0:tok0 + TT, :].rearrange("(j p) c -> p j c", p=C), OUTT[:])], "st", reads=["OUTT"]))
    s.finish("sp", evs[-1:])
    s.emit()
    return nc


D = 2048
KC = 16
TT = 512
EPS = 1e-6
CB = 256


def build_k3(NTOK, SEQ):
    nc = bass.Bass("TRN2", target_bir_lowering=False)
    dt = lambda n, sh, kind="ExternalInput": nc.dram_tensor(n, sh, F32, kind=kind).ap()
    hT = dt("hT", [D, NTOK])
    g_mix = dt("g_mix", [128, KC])
    w_y = dt("w_y", [D, CB]); w_x = dt("w_x", [D, CB])
    w_r = dt("w_r", [CB, CB]); w_i = dt("w_i", [CB, CB])
    cvec = dt("cvec", [128, 2, 8])
    oT = dt("oT", [CB, NTOK], "ExternalOutput")
    s = Sched(nc)
    H = [s.sbuf(f"H{i}", [128, KC, TT], F32) for i in range(2)]
    SQ = s.sbuf("SQ", [128, KC, TT], BF16)
    UT = s.sbuf("UT", [128, KC, TT], BF16)
    WY = s.sbuf("WY", [128, KC, CB], BF16)
    WX = s.sbuf("WX", [128, KC, CB], BF16)
    WR = s.sbuf("WR", [128, 2, CB], BF16)
    WI = s.sbuf("WI", [128, 2, CB], BF16)
    CV = s.sbuf("CV", [128, 2, 8], F32)
    C8 = s.sbuf("C8", [128, 2], F32)
    G = s.sbuf("G", [128, KC], F32)
    ONES = s.sbuf("ONES", [128, 128], BF16)
    RSTD = s.sbuf("RSTD", [128, TT], F32)
    YF = s.sbuf("YF", [128, 2, TT], F32)
    T1 = s.sbuf("T1", [128, 2, TT], F32)
    GY = s.sbuf("GY", [128, 2, TT], F32)
    XR = s.sbuf("XR", [128, 2, TT + 3], F32)
    XC = s.sbuf("XC", [128, 2, TT], F32)
    XCB = s.sbuf("XCB", [128, 2, TT], BF16)
    RG = s.sbuf("RG", [128, 2, TT], F32)
    IG = s.sbuf("IG", [128, 2, TT], F32)
    AA = s.sbuf("AA", [128, 2, TT], F32)
    MM = s.sbuf("MM", [128, 2, TT], F32)
    BB = s.sbuf("BB", [128, 2, TT], F32)
    HS = [s.sbuf(f"HS{i}", [128, 2, TT], F32) for i in range(2)]
    OUT = [s.sbuf(f"OUT{i}", [128, 2, TT], F32) for i in range(2)]
    PB = s.psum("PB", [128, 8, TT])
    pbi = [0]

    def bank():
        b = pbi[0] % 8
        pbi[0] += 1
        return b

    s.op("pool", lambda e: e.memset(ONES[:], 1.0), writes=["ONES"])
    s.dma("sp", [(G[:], g_mix)], "c0", writes=["G"])
    s.dma("sp", [(CV[:], cvec)], "c1", writes=["CV"])
    s.dma("pool", [(WY[:], w_y.rearrange("(kc p) c -> p kc c", p=128))], "c2", writes=["WY"])
    s.dma("pool", [(WX[:], w_x.rearrange("(kc p) c -> p kc c", p=128))], "c3", writes=["WX"])
    s.dma("pool", [(WR[:], w_r.rearrange("(kc p) c -> p kc c", p=128))], "c4", writes=["WR"])
    s.dma("pool", [(WI[:], w_i.rearrange("(kc p) c -> p kc c", p=128))], "c5", writes=["WI"])
    s.op("act", lambda e: e.activation(out=C8[:], in_=CV[:, :, 7], func=AF.Exp, scale=-1.0), reads=["CV"], writes=["C8"])
    s.op("act", lambda e: e.activation(out=C8[:], in_=C8[:], func=AF.Ln, bias=1.0), reads=["C8"], writes=["C8"])
    s.op("dve", lambda e: e.tensor_scalar_mul(out=C8[:], in0=C8[:], scalar1=-8.0), reads=["C8"], writes=["C8"])

    ntile = NTOK // TT
    tpb = SEQ // TT
    evs = []
    s.dma("sp", [(H[0][:], hT[:, 0:TT].rearrange("(kc p) t -> p kc t", p=128))], "h0", writes=["H0"])
    for it in range(ntile):
        Hc, Hn = H[it % 2], f"H{it % 2}"
        if it + 1 < ntile:
            nx = (it + 1) % 2
            s.dma("sp", [(H[nx][:], hT[:, (it + 1) * TT:(it + 2) * TT].rearrange("(kc p) t -> p kc t", p=128))], f"h{nx}",
                  writes=[f"H{nx}"])
        first = (it % tpb == 0)
        HSc, HSn = HS[it % 2], f"HS{it % 2}"
        HSp = HS[(it + 1) % 2]
        HSpn = f"HS{(it + 1) % 2}"
        O, On = OUT[it % 2], f"OUT{it % 2}"
        s.op("act", lambda e, Hc=Hc: e.activation(out=SQ[:], in_=Hc[:], func=AF.Square), reads=[Hn], writes=["SQ"])
        b = bank()
        for kc in range(KC):
            s.op("pe", lambda e, kc=kc, b=b: e.matmul(PB[:, b, :], lhsT=ONES[:], rhs=SQ[:, kc, :], start=(kc == 0), stop=(kc == KC - 1)),
                 reads=["ONES", "SQ"], writes=[("PB", b)])
        s.op("act", lambda e, b=b: e.activation(out=RSTD[:], in_=PB[:, b, :], func=AF.Sqrt, bias=EPS, scale=1.0 / D),
             reads=[("PB", b)], writes=["RSTD"])
        s.op("dve", lambda e: e.reciprocal(out=RSTD[:], in_=RSTD[:]), reads=["RSTD"], writes=["RSTD"])
        for kc in range(KC):
            s.op("dve", lambda e, kc=kc, Hc=Hc: e.scalar_tensor_tensor(out=UT[:, kc, :], in0=Hc[:, kc, :], scalar=G[:, kc:kc + 1],
                                                                    in1=RSTD[:], op0=ALU.mult, op1=ALU.mult),
                 reads=[Hn, "G", "RSTD"], writes=[("UT", kc)])
        by = [bank(), bank()]
        bx = [bank(), bank()]
        for c in range(2):
            for kc in range(KC):
                s.op("pe", lambda e, kc=kc, c=c: e.matmul(PB[:, by[c], :], lhsT=WY[:, kc, c * 128:(c + 1) * 128], rhs=UT[:, kc, :],
                                                       start=(kc == 0), stop=(kc == KC - 1)),
                     reads=["WY", ("UT", kc)], writes=[("PB", by[c])])
            for kc in range(KC):
                s.op("pe", lambda e, kc=kc, c=c: e.matmul(PB[:, bx[c], :], lhsT=WX[:, kc, c * 128:(c + 1) * 128], rhs=UT[:, kc, :],
                                                       start=(kc == 0), stop=(kc == KC - 1)),
                     reads=["WX", ("UT", kc)], writes=[("PB", bx[c])])
        if first:
            s.op("pool", lambda e: e.memset(XR[:, :, 0:3], 0.0), writes=["XR"])
        for c in range(2):
            s.op("act", lambda e, c=c: e.activation(out=YF[:, c, :], in_=PB[:, by[c], :], func=AF.Copy),
                 reads=[("PB", by[c])], writes=[("YF", c)])
            s.op("act", lambda e, c=c: e.activation(out=XR[:, c, 3:TT + 3], in_=PB[:, bx[c], :], func=AF.Copy),
                 reads=[("PB", bx[c])], writes=["XR"])
        s.op("dve", lambda e: e.tensor_tensor(out=T1[:], in0=YF[:], in1=YF[:], op=ALU.mult), reads=["YF"], writes=["T1"])
        s.op("dve", lambda e: e.tensor_scalar(out=T1[:], in0=T1[:], scalar1=0.044715, scalar2=1.0, op0=ALU.mult, op1=ALU.add),
             reads=["T1"], writes=["T1"])
        s.op("dve", lambda e: e.tensor_tensor(out=T1[:], in0=T1[:], in1=YF[:], op=ALU.mult), reads=["T1", "YF"], writes=["T1"])
        s.op("act", lambda e: e.activation(out=T1[:], in_=T1[:], func=AF.Sigmoid, scale=1.5957691216), reads=["T1"], writes=["T1"])
        s.op("dve", lambda e: e.tensor_tensor(out=GY[:], in0=T1[:], in1=YF[:], op=ALU.mult), reads=["T1", "YF"], writes=["GY"])
        for c in range(2):
            s.op("dve", lambda e, c=c: e.tensor_scalar(out=XC[:, c, :], in0=XR[:, c, 3:TT + 3], scalar1=CV[:, c, 3:4], scalar2=CV[:, c, 4:5],
                                                      op0=ALU.mult, op1=ALU.add), reads=["XR", "CV"], writes=[("XC", c)])
            for k in range(3):
                s.op("dve", lambda e, c=c, k=k: e.scalar_tensor_tensor(out=XC[:, c, :], in0=XR[:, c, k:TT + k], scalar=CV[:, c, k:k + 1],
                                                                      in1=XC[:, c, :], op0=ALU.mult, op1=ALU.add),
                     reads=["XR", "CV", ("XC", c)], writes=[("XC", c)])
        s.op("act", lambda e: e.activation(out=XCB[:], in_=XC[:], func=AF.Copy), reads=["XC"], writes=["XCB"])
        s.op("pool", lambda e: e.tensor_copy(out=XR[:, :, 0:3], in_=XR[:, :, TT:TT + 3]), reads=["XR"], writes=["XR"])
        br = [bank(), bank()]
        bi = [bank(), bank()]
        for c in range(2):
            for kc in range(2):
                s.op("pe", lambda e, kc=kc, c=c: e.matmul(PB[:, br[c], :], lhsT=WR[:, kc, c * 128:(c + 1) * 128], rhs=XCB[:, kc, :],
                                                       start=(kc == 0), stop=(kc == 1)), reads=["WR", "XCB"], writes=[("PB", br[c])])
            for kc in range(2):
                s.op("pe", lambda e, kc=kc, c=c: e.matmul(PB[:, bi[c], :], lhsT=WI[:, kc, c * 128:(c + 1) * 128], rhs=XCB[:, kc, :],
                                                       start=(kc == 0), stop=(kc == 1)), reads=["WI", "XCB"], writes=[("PB", bi[c])])
        for c in range(2):
            s.op("act", lambda e, c=c: e.activation(out=RG[:, c, :], in_=PB[:, br[c], :], func=AF.Sigmoid, bias=CV[:, c, 5:6]),
                 reads=[("PB", br[c]), "CV"], writes=[("RG", c)])
            s.op("act", lambda e, c=c: e.activation(out=IG[:, c, :], in_=PB[:, bi[c], :], func=AF.Sigmoid, bias=CV[:, c, 6:7]),
                 reads=[("PB", bi[c]), "CV"], writes=[("IG", c)])
        for c in range(2):
            s.op("act", lambda e, c=c: e.activation(out=AA[:, c, :], in_=RG[:, c, :], func=AF.Exp, scale=C8[:, c:c + 1]),
                 reads=[("RG", c), "C8"], writes=[("AA", c)])
        s.op("dve", lambda e: e.tensor_tensor(out=MM[:], in0=AA[:], in1=AA[:], op=ALU.mult), reads=["AA"], writes=["MM"])
        s.op("dve", lambda e: e.tensor_scalar(out=MM[:], in0=MM[:], scalar1=-1.0, scalar2=1.0, op0=ALU.mult, op1=ALU.add),
             reads=["MM"], writes=["MM"])
        s.op("dve", lambda e: e.tensor_scalar_max(out=MM[:], in0=MM[:], scalar1=0.0), reads=["MM"], writes=["MM"])
        s.op("act", lambda e: e.activation(out=MM[:], in_=MM[:], func=AF.Sqrt), reads=["MM"], writes=["MM"])
        if first:
            s.op("dve", lambda e: e.memset(MM[:, :, 0:1], 1.0), reads=["MM"], writes=["MM"])
        s.op("dve", lambda e: e.tensor_tensor(out=BB[:], in0=IG[:], in1=XC[:], op=ALU.mult), reads=["IG", "XC"], writes=["BB"])
        s.op("dve", lambda e: e.tensor_tensor(out=BB[:], in0=BB[:], in1=MM[:], op=ALU.mult), reads=["BB", "MM"], writes=["BB"])
        for c in range(2):
            init = 0.0 if first else HSp[:, c, TT - 1:TT]
            s.op("dve", lambda e, c=c, init=init, HSc=HSc: e.tensor_tensor_scan(out=HSc[:, c, :], data0=AA[:, c, :], data1=BB[:, c, :],
                                                                              initial=init, op0=ALU.mult, op1=ALU.add),
                 reads=["AA", "BB", HSpn], writes=[(HSn, c)])
        s.op("dve", lambda e, O=O, HSc=HSc: e.tensor_tensor(out=O[:], in0=HSc[:], in1=GY[:], op=ALU.mult), reads=[HSn, "GY"], writes=[On])
        evs.append(s.dma("sp", [(oT[:, it * TT:(it + 1) * TT].rearrange("(c p) t -> p c t", p=128), O[:])], f"st{it % 2}", reads=[On]))
    s.finish("sp", evs[-2:])
    s.emit()
    return nc


_NC_CACHE = {}


def _get(name, fn):
    if name not in _NC_CACHE:
        _NC_CACHE[name] = fn()
    return _NC_CACHE[name]


def _split_cols(hc):
    aq = np.arange(hc * 128, (hc + 1) * 128)
    base = 4096
    return {"aq": aq, "af": 1024 + aq, "ai": 2048 + aq, "ag": 3072 + aq,
            "bq": base + aq, "bk": base + 1024 + aq, "bv": base + 2048 + aq, "bz": base + 3072 + aq,
            "ba": np.array([base + 4096 + hc]), "bb": np.array([base + 4096 + 8 + hc])}


def _f32(a):
    return np.ascontiguousarray(np.asarray(a, dtype=np.float32))


def kernel(x, p, ln_mix, ln_ffn, ln_ple, ln_final, lb_table, ab_w_in, ab_conv, b_a_log, b_dt_bias, a_gnorm, b_gnorm,
           ab_w_out, c_w_in, c_conv_w, c_conv_b, c_w_r, c_b_r, c_w_i, c_b_i, c_lambda, c_w_out, ffn_w_gate, ffn_w_up,
           ffn_w_down, moe_router, moe_w_gate, moe_w_up, moe_w_down, ple_w_proj, ple_w_gate):
    NCORE = 8
    x = np.asarray(x, np.float32)
    B, S, _ = x.shape
    T = B * S
    NT = T // NCORE
    cores = list(range(NCORE))
    xT = np.ascontiguousarray(x.reshape(T, D).T)
    p = np.asarray(p, np.float32)
    pT = [np.ascontiguousarray(p[l].reshape(T, PLE).T) for l in range(2)]

    w_in = np.asarray(ab_w_in[0], np.float32)
    conv = np.asarray(ab_conv[0], np.float32)
    consts1 = k1_consts()
    gn = _f32(np.broadcast_to(np.stack([np.asarray(a_gnorm[0]), np.asarray(b_gnorm[0])], 0)[None], (64, 2, 128)))
    g_mix0 = vec_layout(ln_mix[0])
    maps = []
    for hc in cores:
        c = _split_cols(hc)
        hs = slice(hc * 128, (hc + 1) * 128)
        cw = np.stack([conv[:, hs].T, conv[:, 1024 + hc * 128:1024 + (hc + 1) * 128].T,
                       conv[:, 2048 + hc * 128:2048 + (hc + 1) * 128].T], 1)
        m = {"xT": xT, "g_mix": g_mix0,
             "w_fm": _f32(w_in[:, np.concatenate([c["aq"], c["af"], c["bq"], c["bk"], c["bv"]])]),
             "w_tok": _f32(w_in[:, np.concatenate([c["ai"], c["ag"], c["bz"]])]),
             "w_ab": _f32(w_in[:, np.concatenate([c["ba"], c["bb"]])]),
             "lbt": _f32(np.asarray(lb_table)[:, hs].T), "convw": _f32(cw),
             "sc2": np.array([[np.asarray(b_a_log)[0, hc], np.asarray(b_dt_bias)[0, hc]]], np.float32), "gn": gn}
        m.update(consts1)
        maps.append(m)
    nc1 = _get(("k1", T, S), lambda: build_k1(T, S))
    r1 = run_bass_kernel_spmd(nc1, maps, core_ids=cores).results
    mixed = np.empty((T, D), np.float32)
    for hc in cores:
        mixed[:, hc * 128:(hc + 1) * 128] = r1[hc]["o"][:, 0:128]
        mixed[:, 1024 + hc * 128:1024 + (hc + 1) * 128] = r1[hc]["o"][:, 128:256]
    mT = np.ascontiguousarray(mixed.T)
    del mixed, r1

    shared2 = {"w_out": _f32(ab_w_out[0]), "wg": _f32(ffn_w_gate[0]), "wu": _f32(ffn_w_up[0]), "wd": _f32(ffn_w_down[0]),
               "wpg": _f32(ple_w_gate[0]), "wpp": _f32(ple_w_proj[0]),
               "g_ffn": vec_layout(ln_ffn[0]), "g_ple": vec_layout(ln_ple[0])}
    maps = []
    for c in cores:
        sl = slice(c * NT, (c + 1) * NT)
        m = {"hT": _f32(xT[:, sl]), "mT": _f32(mT[:, sl]), "pT": _f32(pT[0][:, sl])}
        m.update(shared2)
        maps.append(m)
    nc2 = _get(("k2", NT), lambda: build_k2(NT))
    r2 = run_bass_kernel_spmd(nc2, maps, core_ids=cores).results
    h1T = np.ascontiguousarray(np.concatenate([r2[c]["oT"] for c in cores], axis=1))
    del r2, mT, maps

    cw_in = np.asarray(c_w_in[0], np.float32)
    g_mix1 = vec_layout(ln_mix[1])
    maps = []
    for c in cores:
        sl = slice(c * 256, (c + 1) * 256)
        cv = np.zeros((128, 2, 8), np.float32)

        def pc(v):
            return np.asarray(v, np.float32)[sl].reshape(2, 128).T
        for k in range(4):
            cv[:, :, k] = pc(np.asarray(c_conv_w[0])[k])
        cv[:, :, 4] = pc(c_conv_b[0]); cv[:, :, 5] = pc(c_b_r[0]); cv[:, :, 6] = pc(c_b_i[0]); cv[:, :, 7] = pc(c_lambda[0])
        maps.append({"hT": h1T, "g_mix": g_mix1, "w_y": _f32(cw_in[:, sl]), "w_x": _f32(cw_in[:, D + c * 256:D + (c + 1) * 256]),
                     "w_r": _f32(np.asarray(c_w_r[0])[c]), "w_i": _f32(np.asarray(c_w_i[0])[c]), "cvec": cv})
    nc3 = _get(("k3", T, S), lambda: build_k3(T, S))
    r3 = run_bass_kernel_spmd(nc3, maps, core_ids=cores).results
    gT = np.ascontiguousarray(np.concatenate([r3[c]["oT"] for c in cores], axis=0))
    del r3, maps

    shared4 = {"w_out": _f32(c_w_out[0]), "wg": _f32(moe_w_gate[0]), "wu": _f32(moe_w_up[0]), "wd": _f32(moe_w_down[0]),
               "wr": _f32(np.asarray(moe_router[0], np.float32).reshape(16, 128, 8).transpose(1, 0, 2)),
               "wpg": _f32(ple_w_gate[1]), "wpp": _f32(ple_w_proj[1]),
               "g_ffn": vec_layout(ln_ffn[1]), "g_ple": vec_layout(ln_ple[1]), "g_fin": vec_layout(ln_final)}
    shared4.update(k4_consts())
    maps = []
    for c in cores:
        sl = slice(c * NT, (c + 1) * NT)
        m = {"hT": _f32(h1T[:, sl]), "mT": _f32(gT[:, sl]), "pT": _f32(pT[1][:, sl])}
        m.update(shared4)
        maps.append(m)
    nc4 = _get(("k4", NT), lambda: build_k4(NT))
    r4 = run_bass_kernel_spmd(nc4, maps, core_ids=cores).results
    outT = np.concatenate([r4[c]["oT"] for c in cores], axis=1)
    return np.ascontiguousarray(outT.T).reshape(B, S, D)
```
